# Optimizing a Trainium2 kernel written in Bass

```python
import jax, jax.numpy as jnp
from jax import lax
import numpy as np

D_MODEL = 2048
BATCH = 1
SEQ = 8192
DEPTH = 1

N_HEADS_ATTN = 8
HEAD_DIM = 128
N_IDX_HEADS = 16
IDX_DIM = 64
TOPK_MAX = 256
Q_BLOCK = 128
N_RET_HEADS = 8
RET_QK_DIM = 128
RET_V_DIM = 256
RET_CHUNK = 128
N_MEM = 256
N_MEM_HEADS = 4
MEM_HEAD_DIM = 256
D_FF = -(-8 * D_MODEL // (3 * 256)) * 256
ROPE_THETA = 10000.0
EPS = 1e-6
N_BRANCHES = 3

A_WIDTH = N_HEADS_ATTN * HEAD_DIM
IDX_Q_WIDTH = N_IDX_HEADS * IDX_DIM
RET_QK_WIDTH = N_RET_HEADS * RET_QK_DIM
RET_V_WIDTH = N_RET_HEADS * RET_V_DIM
MEM_WIDTH = N_MEM_HEADS * MEM_HEAD_DIM
COL_SIZES = (A_WIDTH, A_WIDTH, A_WIDTH, IDX_Q_WIDTH, IDX_DIM, N_IDX_HEADS,
             RET_QK_WIDTH, RET_QK_WIDTH, RET_V_WIDTH, RET_V_WIDTH, MEM_WIDTH, N_BRANCHES * D_MODEL)
COL_SPLITS = tuple(sum(COL_SIZES[:i + 1]) for i in range(len(COL_SIZES) - 1))
IN_COLS = sum(COL_SIZES)

kernel_name = "hybrid_dsa_retention_memory_gated_block"


def rms_norm(x, g):
    xf = x.astype(jnp.float32)
    y = xf * lax.rsqrt(jnp.mean(xf * xf, axis=-1, keepdims=True) + EPS)
    return (y * g.astype(jnp.float32)).astype(x.dtype)


def rope(x, pos):
    d = x.shape[-1]
    freqs = ROPE_THETA ** (-jnp.arange(0, d, 2, dtype=jnp.float32) / d)
    ang = pos.astype(jnp.float32)[..., None] * freqs
    cos = jnp.cos(ang)[:, :, None, :]
    sin = jnp.sin(ang)[:, :, None, :]
    xf = x.astype(jnp.float32)
    x1, x2 = xf[..., : d // 2], xf[..., d // 2:]
    return jnp.concatenate([x1 * cos - x2 * sin, x2 * cos + x1 * sin], axis=-1).astype(x.dtype)


def dsa_attention(q, k, v, q_idx, k_idx, w_idx):
    B, S, H, dh = q.shape
    k_top = min(TOPK_MAX, S // 4)
    nb = S // Q_BLOCK

    def to_blocks(a):
        return jnp.moveaxis(a.reshape(B, nb, Q_BLOCK, *a.shape[2:]), 1, 0)

    t_blk = jnp.arange(S, dtype=jnp.int32).reshape(nb, Q_BLOCK)
    key_pos = jnp.arange(S, dtype=jnp.int32)
    gather = jax.vmap(lambda a, i: a[i])

    def block(args):
        qb, qib, wb, tb = args
        rel = jax.nn.relu(jnp.einsum('bqhd,bsd->bqhs', qib, k_idx))
        score = jnp.einsum('bqhs,bqh->bqs', rel, wb).astype(jnp.float32)
        causal = key_pos[None, None, :] <= tb[None, :, None]
        score = jnp.where(causal, score, -jnp.inf)
        _, idx = lax.top_k(score, k_top)
        valid = idx <= tb[None, :, None]
        ks = gather(k, idx)
        vs = gather(v, idx)
        logits = jnp.einsum('bqhd,bqkhd->bqhk', qb, ks).astype(jnp.float32)
        logits = jnp.where(valid[:, :, None, :], logits, -jnp.inf)
        p = jax.nn.softmax(logits, axis=-1).astype(v.dtype)
        return jnp.einsum('bqhk,bqkhd->bqhd', p, vs)

    out = lax.map(block, (to_blocks(q), to_blocks(q_idx), to_blocks(w_idx), t_blk))
    return jnp.moveaxis(out, 0, 1).reshape(B, S, H * dh)


def retention(q, k, v, g):
    B, S, H, dk = q.shape
    dv = v.shape[-1]
    C = RET_CHUNK
    n = S // C
    dt = v.dtype
    log_gamma = jnp.log1p(-jnp.exp2(-5.0 - jnp.arange(H, dtype=jnp.float32)))
    i = jnp.arange(C, dtype=jnp.float32)
    diff = i[:, None] - i[None, :]
    inner_decay = jnp.where(diff[None] >= 0,
                            jnp.exp(jnp.maximum(diff, 0.0)[None] * log_gamma[:, None, None]), 0.0)
    k_decay = jnp.exp((C - 1 - i)[None, :] * log_gamma[:, None])
    q_decay = jnp.exp((i + 1)[None, :] * log_gamma[:, None])
    chunk_decay = jnp.exp(C * log_gamma)

    qc = q.reshape(B, n, C, H, dk)
    kc = (k * dk ** -0.5).reshape(B, n, C, H, dk)
    vc = v.reshape(B, n, C, H, dv)

    scores = jnp.einsum('bnihd,bnjhd->bnhij', qc, kc) * inner_decay.astype(dt)
    inner = jnp.einsum('bnhij,bnjhe->bnihe', scores, vc)
    kv = jnp.einsum('bnjhd,bnjhe,hj->nbhde', kc, vc, k_decay.astype(dt)).astype(jnp.float32)

    def step(state, kv_n):
        return state * chunk_decay[None, :, None, None] + kv_n, state

    _, prev = lax.scan(step, jnp.zeros((B, H, dk, dv), jnp.float32), kv)
    cross = jnp.einsum('bnihd,nbhde->bnihe', qc, prev.astype(dt)) * \
        jnp.transpose(q_decay)[None, None, :, :, None].astype(dt)
    y = (inner + cross).reshape(B, S, H, dv).astype(jnp.float32)
    mu = jnp.mean(y, axis=-1, keepdims=True)
    var = jnp.mean(jnp.square(y - mu), axis=-1, keepdims=True)
    yn = ((y - mu) * lax.rsqrt(var + EPS)).astype(dt).reshape(B, S, H * dv)
    return jax.nn.silu(g) * yn


def memory_attention(q, k, v):
    B, S, H, d = q.shape
    logits = jnp.einsum('bshd,bmhd->bhsm', q, k).astype(jnp.float32)
    p = jax.nn.softmax(logits, axis=-1).astype(v.dtype)
    return jnp.einsum('bhsm,bmhd->bshd', p, v).reshape(B, S, H * d)


def setup_inputs(seed: int = 0) -> dict:
    key = jax.random.key(seed)
    ks = jax.random.split(key, 16)

    def w(k, fan_in, fan_out):
        return jax.random.normal(k, (DEPTH, fan_in, fan_out), jnp.float32) * fan_in ** -0.5

    def gain(k):
        return 1.0 + 0.05 * jax.random.normal(k, (DEPTH, D_MODEL), jnp.float32)

    return {
        "x": jax.random.normal(ks[0], (BATCH, SEQ, D_MODEL), jnp.float32),
        "mem": jax.random.normal(ks[1], (BATCH, N_MEM, D_MODEL), jnp.float32),
        "positions": jnp.broadcast_to(jnp.arange(SEQ, dtype=jnp.int32), (BATCH, SEQ)),
        "g_pre_mix": gain(ks[2]),
        "g_mem": gain(ks[3]),
        "w_in": w(ks[4], D_MODEL, IN_COLS),
        "w_mem_kv": w(ks[5], D_MODEL, 2 * MEM_WIDTH),
        "w_branch_a": w(ks[6], A_WIDTH, D_MODEL),
        "w_branch_b": w(ks[7], RET_V_WIDTH, D_MODEL),
        "w_branch_c": w(ks[8], MEM_WIDTH, D_MODEL),
        "w_out": w(ks[9], D_MODEL, D_MODEL),
        "g_post_mix": gain(ks[10]),
        "g_pre_ffn": gain(ks[11]),
        "w_ffn_in": w(ks[12], D_MODEL, 2 * D_FF),
        "w_ffn_out": w(ks[13], D_FF, D_MODEL),
        "g_post_ffn": gain(ks[14]),
    }


def reference(x, mem, positions, g_pre_mix, g_mem, w_in, w_mem_kv, w_branch_a, w_branch_b,
              w_branch_c, w_out, g_post_mix, g_pre_ffn, w_ffn_in, w_ffn_out, g_post_ffn):
    B, S, D = x.shape
    M = mem.shape[1]
    for l in range(DEPTH):
        h = rms_norm(x, g_pre_mix[l])
        proj = h @ w_in[l]
        (a_q, a_k, a_v, i_q, i_k, i_w, r_q, r_k, r_v, r_g, m_q, gate_logits) = \
            jnp.split(proj, COL_SPLITS, axis=-1)

        a_q = rope(a_q.reshape(B, S, N_HEADS_ATTN, HEAD_DIM), positions) * HEAD_DIM ** -0.5
        a_k = rope(a_k.reshape(B, S, N_HEADS_ATTN, HEAD_DIM), positions)
        a_v = a_v.reshape(B, S, N_HEADS_ATTN, HEAD_DIM)
        i_q = rope(i_q.reshape(B, S, N_IDX_HEADS, IDX_DIM), positions) * IDX_DIM ** -0.5
        i_k = rope(i_k.reshape(B, S, 1, IDX_DIM), positions)[:, :, 0, :]
        i_w = i_w * N_IDX_HEADS ** -0.5
        o_a = dsa_attention(a_q, a_k, a_v, i_q, i_k, i_w)

        r_q = rope(r_q.reshape(B, S, N_RET_HEADS, RET_QK_DIM), positions)
        r_k = rope(r_k.reshape(B, S, N_RET_HEADS, RET_QK_DIM), positions)
        r_v = r_v.reshape(B, S, N_RET_HEADS, RET_V_DIM)
        o_b = retention(r_q, r_k, r_v, r_g)

        mem_kv = rms_norm(mem, g_mem[l]) @ w_mem_kv[l]
        m_k, m_v = jnp.split(mem_kv, 2, axis=-1)
        m_k = m_k.reshape(B, M, N_MEM_HEADS, MEM_HEAD_DIM)
        m_v = m_v.reshape(B, M, N_MEM_HEADS, MEM_HEAD_DIM)
        m_q = m_q.reshape(B, S, N_MEM_HEADS, MEM_HEAD_DIM) * MEM_HEAD_DIM ** -0.5
        o_c = memory_attention(m_q, m_k, m_v)

        gates = jax.nn.sigmoid(gate_logits).reshape(B, S, N_BRANCHES, D)
        mixed = (gates[:, :, 0] * (o_a @ w_branch_a[l])
                 + gates[:, :, 1] * (o_b @ w_branch_b[l])
                 + gates[:, :, 2] * (o_c @ w_branch_c[l]))
        x = x + rms_norm(mixed @ w_out[l], g_post_mix[l])

        h2 = rms_norm(x, g_pre_ffn[l])
        f_gate, f_up = jnp.split(h2 @ w_ffn_in[l], 2, axis=-1)
        y2 = (jax.nn.silu(f_gate) * f_up) @ w_ffn_out[l]
        x = x + rms_norm(y2, g_post_ffn[l])
    return x
```

```python
import math
from contextlib import ExitStack

import numpy as np
import ml_dtypes
import concourse.bass as bass
import concourse.mybir as mybir
from concourse.bass_utils import run_bass_kernel_spmd

F32 = mybir.dt.float32
BF16 = mybir.dt.bfloat16
I32 = mybir.dt.int32
ALU = mybir.AluOpType
AF = mybir.ActivationFunctionType
AX = mybir.AxisListType

NCORES = 8
S = 8192
D = 2048
NT = S // NCORES
TT = NT // 128
KC = D // 128
IN_COLS = 17488
DFF = 5632
EPS = 1e-6
NEG = -1000.0
NBIS = 20
TOPK = 256

C_AQ, C_AK, C_AV, C_IQ, C_IK, C_IW, C_RQ, C_RK, C_RV, C_RG, C_MQ, C_GT = (
    0, 1024, 2048, 3072, 4096, 4160, 4176, 5200, 6224, 8272, 10320, 11344)


class Res:
    __slots__ = ("w", "rd")

    def __init__(self):
        self.w = None
        self.rd = {}


class Issuer:
    def __init__(self, eng):
        self.eng = eng
        self.waited = {}


class KB:
    def __init__(self, nc, es):
        self.nc = nc
        self.iss = {n: Issuer(getattr(nc, n)) for n in ("sync", "scalar", "vector", "gpsimd", "tensor")}
        self.csem = {n: es.enter_context(nc.semaphore("c_" + n)) for n in ("scalar", "vector", "gpsimd", "tensor")}
        self.ccount = {n: 0 for n in self.csem}
        self.dsems = {}
        self.dcount = {}
        self.dnext = {}
        for q, n in (("sync", 16), ("gpsimd", 8), ("scalar", 4)):
            self.dsems[q] = [es.enter_context(nc.semaphore("d_%s%d" % (q, i))) for i in range(n)]
            self.dnext[q] = 0
            for s in self.dsems[q]:
                self.dcount[s] = 0

    def _wait(self, iss, need):
        for s, v in need.items():
            if iss.waited.get(s, 0) >= v:
                continue
            iss.eng.wait_ge(s, v)
            iss.waited[s] = v

    def _deps(self, rd, wr, own, raw_same):
        need = {}

        def add(h, same_ok):
            if h is None:
                return
            s, v = h
            if s is own and not same_ok:
                return
            if need.get(s, 0) < v:
                need[s] = v
        for r in rd:
            add(r.w, raw_same)
        for r in wr:
            add(r.w, False)
            for h in r.rd.values():
                add(h, False)
        return need

    def op(self, en, fn, rd=(), wr=()):
        iss = self.iss[en]
        sem = self.csem[en]
        need = self._deps(rd, wr, sem, en != "tensor")
        self._wait(iss, need)
        ins = fn(iss.eng)
        self.ccount[en] += 1
        ins.then_inc(sem, 1)
        h = (sem, self.ccount[en])
        for r in rd:
            r.rd[sem] = h
        for r in wr:
            r.w = h
            r.rd = {}
        return ins

    def act(self, fn, rd=(), wr=()):
        return self.op("scalar", fn, rd, wr)

    def dve(self, fn, rd=(), wr=()):
        return self.op("vector", fn, rd, wr)

    def pe(self, fn, rd=(), wr=()):
        return self.op("tensor", fn, rd, wr)

    def dma(self, q, out, in_, rd=(), wr=(), **kw):
        iss = self.iss[q]
        sems = self.dsems[q]
        s = sems[self.dnext[q] % len(sems)]
        self.dnext[q] += 1
        need = self._deps(rd, wr, None, True)
        if self.dcount[s] > 0 and need.get(s, 0) < self.dcount[s]:
            need[s] = self.dcount[s]
        self._wait(iss, need)
        ins = iss.eng.dma_start(out=out, in_=in_, **kw)
        ins.then_inc(s, 16)
        self.dcount[s] += 16
        h = (s, self.dcount[s])
        for r in rd:
            r.rd[s] = h
        for r in wr:
            r.w = h
            r.rd = {}
        return ins

    def collective(self, kind, ins_, outs_, rd=(), wr=()):
        q = "gpsimd"
        iss = self.iss[q]
        sems = self.dsems[q]
        s = sems[self.dnext[q] % len(sems)]
        self.dnext[q] += 1
        need = self._deps(rd, wr, None, True)
        if self.dcount[s] > 0 and need.get(s, 0) < self.dcount[s]:
            need[s] = self.dcount[s]
        self._wait(iss, need)
        ins = iss.eng.collective_compute(kind, ALU.bypass, replica_groups=[list(range(NCORES))],
                                         ins=ins_, outs=outs_)
        ins.then_inc(s, 16)
        self.dcount[s] += 16
        h = (s, self.dcount[s])
        for r in rd:
            r.rd[s] = h
        for r in wr:
            r.w = h
            r.rd = {}
        return ins

    def barrier(self):
        need = {}
        for n, sem in self.csem.items():
            if self.ccount[n] > 0:
                need[sem] = self.ccount[n]
        for s, v in self.dcount.items():
            if v > 0:
                need[s] = v
        for n, iss in self.iss.items():
            self._wait(iss, dict(need))


_UID = [0]


class Ring:
    def __init__(self, es, nc, name, shape, dt, n, psum=False):
        self.t = []
        self.r = []
        _UID[0] += 1
        name = "r%d_%s" % (_UID[0], name)
        for i in range(n):
            if psum:
                t = es.enter_context(nc.psum_tensor("%s%d" % (name, i), shape, dt))
            else:
                t = es.enter_context(nc.sbuf_tensor("%s%d" % (name, i), shape, dt))
            self.t.append(t)
            self.r.append(Res())
        self.i = 0

    def next(self):
        j = self.i % len(self.t)
        self.i += 1
        return self.t[j], self.r[j]


def build(debug=False, upto=None):
    nc = bass.Bass("TRN2", target_bir_lowering=False)
    okind = "ExternalOutput" if debug else "Internal"

    def din(name, shape, dt):
        return nc.dram_tensor(name, shape, dt, kind="ExternalInput").ap()

    def dscr(name, shape, dt, dbg=True):
        return nc.dram_tensor(name, shape, dt, kind=(okind if dbg else "Internal")).ap()

    x_d = din("x", [NT, D], F32)
    xall_d = din("x_all", [S, D], F32)
    pos_d = din("pos", [128, TT], I32)
    posall_d = din("pos_all", [128, NCORES * TT], I32)
    mem_d = din("mem", [256, D], F32)
    gfm_d = din("gfm", [128, 48], F32)
    gpm_d = din("gpm", [128, D], F32)
    gpf_d = din("gpf", [128, D], F32)
    win_d = din("w_in", [D, IN_COLS], F32)
    wmk_d = din("w_mem_kv", [D, 2048], F32)
    wa_d = din("w_a", [1024, D], F32)
    wb_d = din("w_b", [2048, D], F32)
    wc_d = din("w_c", [1024, D], F32)
    wo_d = din("w_out", [D, D], F32)
    wfi_d = din("w_fi", [D, 2 * DFF], F32)
    wfo_d = din("w_fo", [DFF, D], F32)
    ident_d = din("ident", [128, 128], BF16)
    freqs_d = din("freqs", [128, 64], F32)
    d4_d = din("d4", [128, 512], F32)
    cbg_d = din("cbg", [128, 128], F32)
    dmt_d = din("dmt", [128, 1024], F32)
    qdec_d = din("qdec", [128, 1024], F32)
    kdec_d = din("kdec", [128, 8], F32)
    dch_d = din("dch", [128, 8], F32)
    coef_d = din("coef", [128, 64], F32)
    out_d = nc.dram_tensor("out", [NT, D], F32, kind="ExternalOutput").ap()

    QT = dscr("QT", [1024, NT], BF16)
    KVI_loc = dscr("KVI_loc", [2112, NT], BF16, dbg=False)
    KVI_dbg = dscr("KVI_dbg", [2112, NT], BF16) if debug else None
    KVI_all = dscr("KVI_all", [NCORES * 2112, NT], BF16)
    RK_all = dscr("RK_all", [S, 1024], BF16, dbg=False)
    RV_all = dscr("RV_all", [S, 2048], BF16, dbg=False)
    IQT = dscr("IQT", [1024, NT], BF16)
    IW = dscr("IW", [NT, 16], F32)
    RQT = dscr("RQT", [1024, NT], BF16)
    RKT = dscr("RKT", [1024, NT], BF16)
    RK = dscr("RK", [NT, 1024], BF16)
    RV = dscr("RV", [NT, 2048], BF16)
    RG = dscr("RG", [NT, 2048], BF16)
    MQT = dscr("MQT", [1024, NT], BF16)
    GATES = dscr("GATES", [NT, 3 * D], BF16)
    MT = dscr("MT", [TT, 128, 64, 128], BF16, dbg=False)
    SCORE = dscr("SCORE", [NT, S], F32) if debug else None
    THR = dscr("THR", [128, TT], F32)
    LST_loc = dscr("LST_loc", [1024, 256], F32, dbg=False)
    LST_all = dscr("LST_all", [NCORES * 1024, 256], F32, dbg=False)
    OAT = dscr("OAT", [1024, NT], BF16)
    OBT = dscr("OBT", [2048, NT], BF16)
    OCT = dscr("OCT", [1024, NT], BF16)
    MIX = dscr("MIX", [NT, D], BF16)
    X1 = dscr("X1", [NT, D], F32)
    Y2 = dscr("Y2", [NT, D], F32)

    R = {}
    for nme in ("QT", "KVI_loc", "KVI_all", "IQT", "IW", "RQT", "RKT", "RK", "RV", "RG", "MQT", "GATES", "MT",
                "SCORE", "THR", "LST_loc", "LST_all", "RK_all", "RV_all", "OAT", "OBT", "OCT", "MIX", "X1", "Y2", "out"):
        R[nme] = Res()

    es = ExitStack()
    with es:
        k = KB(nc, es)

        def sb(st, name, shape, dt):
            _UID[0] += 1
            return st.enter_context(nc.sbuf_tensor("s%d_%s" % (_UID[0], name), shape, dt))

        ident = sb(es, "ident", [128, 128], BF16)
        freqs = sb(es, "freqs", [128, 64], F32)
        gfm = sb(es, "gfm", [128, 48], F32)
        posi = sb(es, "posi", [128, TT], I32)
        posf = sb(es, "posf", [128, TT], F32)
        ones = sb(es, "ones", [128, 128], BF16)
        r_const = Res()
        r_cs = Res()
        k.dma("sync", ident[:], ident_d[:, :], wr=[r_const])
        k.dma("sync", freqs[:], freqs_d[:, :], wr=[r_const])
        k.dma("sync", gfm[:], gfm_d[:, :], wr=[r_const])
        k.dma("sync", posi[:], pos_d[:, :], wr=[r_const])
        k.dve(lambda e: e.tensor_copy(out=posf[:], in_=posi[:]), rd=[r_const], wr=[r_cs])
        k.dve(lambda e: e.memset(ones[:], 1.0), wr=[r_cs])

        psf = Ring(es, nc, "psf", [128, 512], F32, 6, psum=True)
        psb = Ring(es, nc, "psb", [128, 1024], BF16, 2, psum=True)

        ang = sb(es, "ang", [128, 64], F32)
        u = sb(es, "u", [128, 64], F32)
        ki = sb(es, "ki", [128, 64], I32)
        kf = sb(es, "kf", [128, 64], F32)
        mm = sb(es, "mm", [128, 64], F32)
        rt = Res()
        posi_all = sb(es, "posi_all", [128, NCORES * TT], I32)
        posf_all = sb(es, "posf_all", [128, NCORES * TT], F32)
        k.dma("sync", posi_all[:], posall_d[:, :], wr=[r_const])
        k.dve(lambda e: e.tensor_copy(out=posf_all[:], in_=posi_all[:]), rd=[r_const], wr=[r_cs])

        def compute_cs(cs, r_cst, pf, col0):
            for t in range(TT):
                k.dve(lambda e: e.tensor_scalar(out=ang[:], in0=freqs[:], scalar1=pf[:, col0 + t:col0 + t + 1], scalar2=None,
                                                op0=ALU.mult), rd=[r_const, r_cs, rt], wr=[rt])
                for which, off in ((1, 0.5), (0, 0.75)):
                    k.dve(lambda e: e.tensor_scalar(out=u[:], in0=ang[:], scalar1=1.0 / (2 * math.pi), scalar2=off,
                                                    op0=ALU.mult, op1=ALU.add), rd=[rt], wr=[rt])
                    k.dve(lambda e: e.tensor_copy(out=ki[:], in_=u[:]), rd=[rt], wr=[rt])
                    k.dve(lambda e: e.tensor_copy(out=kf[:], in_=ki[:]), rd=[rt], wr=[rt])
                    k.dve(lambda e: e.tensor_tensor(out=u[:], in0=u[:], in1=kf[:], op=ALU.subtract), rd=[rt], wr=[rt])
                    k.dve(lambda e: e.tensor_scalar(out=mm[:], in0=u[:], scalar1=0.5, scalar2=None, op0=ALU.is_gt),
                          rd=[rt], wr=[rt])
                    k.dve(lambda e: e.tensor_tensor(out=u[:], in0=u[:], in1=mm[:], op=ALU.subtract), rd=[rt], wr=[rt])
                    k.act(lambda e: e.activation(out=cs[:, t, which, :], in_=u[:], func=AF.Sin,
                                                 scale=-2 * math.pi * (1 - 1e-6)), rd=[rt], wr=[r_cst])

        csr = Ring(es, nc, "cs", [128, TT, 2, 64], F32, 2)
        csh = list(csr.next())
        compute_cs(csh[0], csh[1], posf, 0)

        def norm_T(st_ring, src_ap, r_src_rd, g_col0, dstT, r_dst, col0):
            (xt, r_xt), (xb, r_xb), (st, r_st), (jk, r_jk) = st_ring
            k.dma("sync", xt[:], src_ap, rd=r_src_rd, wr=[r_xt])
            k.act(lambda e: e.activation(out=jk[:], in_=xt[:], func=AF.Square, accum_out=st[:, 0:1]),
                  rd=[r_xt], wr=[r_jk, r_st])
            k.act(lambda e: e.activation(out=st[:, 1:2], in_=st[:, 0:1], func=AF.Sqrt, scale=1.0 / D, bias=EPS),
                  rd=[r_st], wr=[r_st])
            k.dve(lambda e: e.reciprocal(out=st[:, 2:3], in_=st[:, 1:2]), rd=[r_st], wr=[r_st])
            k.dve(lambda e: e.tensor_scalar(out=xb[:], in0=xt[:], scalar1=st[:, 2:3], scalar2=None, op0=ALU.mult),
                  rd=[r_xt, r_st], wr=[r_xb])
            for half in range(2):
                pb, r_pb = psb.next()
                for j in range(8):
                    c = half * 8 + j
                    k.pe(lambda e: e.transpose(out=pb[:, j * 128:(j + 1) * 128], in_=xb[:, c * 128:(c + 1) * 128],
                                               identity=ident[:]), rd=[r_xb, r_const], wr=[r_pb])
                gb = gfm[:, g_col0 + half * 8:g_col0 + half * 8 + 8].unsqueeze(2).to_broadcast([128, 8, 128])
                k.dve(lambda e: e.tensor_tensor(out=dstT[:, half * 8:half * 8 + 8, col0:col0 + 128],
                                                in0=pb[:].rearrange("p (j q) -> p j q", q=128), in1=gb, op=ALU.mult),
                      rd=[r_pb, r_const], wr=[r_dst])

        def load_w(ring, src, rows_kc, c0, n, q="gpsimd", nsplit=4):
            wt, r_w = ring.next()
            step = max(1, rows_kc // nsplit)
            for a in range(0, rows_kc, step):
                b = min(rows_kc, a + step)
                k.dma(q, wt[:, a:b, 0:n],
                      src[a * 128:b * 128, c0:c0 + n].rearrange("(c p) n -> p c n", p=128), wr=[r_w])
            return wt, r_w

        with ExitStack() as ph:
            hTr = Ring(ph, nc, "hT", [128, KC, NT], BF16, 2)
            hT, r_hT = hTr.next()
            nrings = [Ring(ph, nc, "xt", [128, D], F32, 2), Ring(ph, nc, "xb", [128, D], BF16, 2),
                      Ring(ph, nc, "st", [128, 4], F32, 2), Ring(ph, nc, "jk", [128, D], BF16, 1)]
            for t in range(TT):
                norm_T([r.next() for r in nrings], x_d[t * 128:(t + 1) * 128, :], [], 0, hT, r_hT, t * 128)

            wring = Ring(ph, nc, "win", [128, KC, 512], BF16, 2)
            xsr = Ring(ph, nc, "xs", [128, 512], F32, 2)
            xrr = Ring(ph, nc, "xr", [128, 512], BF16, 2)
            tmr = Ring(ph, nc, "tmp", [128, 256], F32, 4)
            stT = Ring(ph, nc, "stT", [128, 4, NT], BF16, 2)
            stM = Ring(ph, nc, "stM", [128, TT, 512], BF16, 2)
            iwst = sb(ph, "iwst", [128, TT, 16], F32)
            r_iwst = Res()

            def rope(xs, r_xs, xr, r_xr, n, hd, t):
                nh = n // hd
                half = hd // 2
                xv = xs[:, 0:n].rearrange("p (h two f) -> p h two f", two=2, f=half)
                ov = xr[:, 0:n].rearrange("p (h two f) -> p h two f", two=2, f=half)
                cs, r_csx = csh
                if hd == 128:
                    cos = cs[:, t, 0, :]
                    sin = cs[:, t, 1, :]
                else:
                    cos = cs[:, t, 0, 0::2]
                    sin = cs[:, t, 1, 0::2]
                cosb = cos.unsqueeze(1).to_broadcast([128, nh, half])
                sinb = sin.unsqueeze(1).to_broadcast([128, nh, half])
                hw = nh * half
                (t1, r1), (t2, r2) = tmr.next(), tmr.next()
                t1v = t1[:, 0:hw].rearrange("p (h f) -> p h f", f=half)
                t2v = t2[:, 0:hw].rearrange("p (h f) -> p h f", f=half)
                k.dve(lambda e: e.tensor_tensor(out=t1v, in0=xv[:, :, 0, :], in1=cosb, op=ALU.mult),
                      rd=[r_xs, r_csx], wr=[r1])
                k.dve(lambda e: e.tensor_tensor(out=t2v, in0=xv[:, :, 1, :], in1=sinb, op=ALU.mult),
                      rd=[r_xs, r_csx], wr=[r2])
                k.dve(lambda e: e.tensor_tensor(out=ov[:, :, 0, :], in0=t1v, in1=t2v, op=ALU.subtract),
                      rd=[r1, r2], wr=[r_xr])
                (t3, r3), (t4, r4) = tmr.next(), tmr.next()
                t3v = t3[:, 0:hw].rearrange("p (h f) -> p h f", f=half)
                t4v = t4[:, 0:hw].rearrange("p (h f) -> p h f", f=half)
                k.dve(lambda e: e.tensor_tensor(out=t3v, in0=xv[:, :, 1, :], in1=cosb, op=ALU.mult),
                      rd=[r_xs, r_csx], wr=[r3])
                k.dve(lambda e: e.tensor_tensor(out=t4v, in0=xv[:, :, 0, :], in1=sinb, op=ALU.mult),
                      rd=[r_xs, r_csx], wr=[r4])
                k.dve(lambda e: e.tensor_tensor(out=ov[:, :, 1, :], in0=t3v, in1=t4v, op=ALU.add),
                      rd=[r3, r4], wr=[r_xr])

            def transposes(xr, r_xr, n, fw, sT, r_sT, t):
                nj = n // 128
                pb, r_pb = psb.next()
                for j in range(nj):
                    k.pe(lambda e: e.transpose(out=pb[:, j * 128:(j + 1) * 128], in_=xr[:, j * 128:(j + 1) * 128],
                                               identity=ident[:]), rd=[r_xr, r_const], wr=[r_pb])
                k.act(lambda e: e.activation(out=sT[:, 0:nj, t * 128:(t + 1) * 128],
                                             in_=pb[:, 0:nj * 128].rearrange("p (j q) -> p j q", q=128), func=AF.Copy),
                      rd=[r_pb], wr=[r_sT])

            st64 = Ring(ph, nc, "st64", [64, 8, NT], BF16, 1)

            def transposes64(xr, r_xr, n, sT, r_sT, t):
                nj = n // 64
                pb, r_pb = psb.next()
                for j in range(nj):
                    k.pe(lambda e: e.transpose(out=pb[0:64, j * 128:(j + 1) * 128], in_=xr[:, j * 64:(j + 1) * 64],
                                               identity=ident[:]), rd=[r_xr, r_const], wr=[r_pb])
                k.act(lambda e: e.activation(out=sT[0:64, 0:nj, t * 128:(t + 1) * 128],
                                             in_=pb[0:64, 0:nj * 128].rearrange("p (j q) -> p j q", q=128),
                                             func=AF.Copy), rd=[r_pb], wr=[r_sT])

            groups = []
            for i in range(2):
                groups.append((C_AQ + 512 * i, 512, "ropeT", dict(hd=128, scale=128 ** -0.5, dst=QT, rn="QT", row0=512 * i)))
            for i in range(2):
                groups.append((C_IQ + 512 * i, 512, "rope64", dict(scale=64 ** -0.5, dst=IQT, rn="IQT", row0=512 * i)))
            groups.append((C_IK, 80, "ikw", dict(iw=True, row0=0, dst=KVI_loc, rn="KVI_loc")))
            for i in range(2):
                groups.append((C_RQ + 512 * i, 512, "ropeT", dict(hd=128, scale=1.0, dst=RQT, rn="RQT", row0=512 * i)))
            for i in range(2):
                groups.append((C_RK + 512 * i, 512, "ropeT", dict(hd=128, scale=1.0, dst=RKT, rn="RKT", row0=512 * i,
                                                                 tm_dst=RK, tm_rn="RK", tm_col0=512 * i)))
            for i in range(4):
                groups.append((C_RV + 512 * i, 512, "tm", dict(func=AF.Copy, scale=1.0, dst=RV, rn="RV", row0=0, col0=512 * i)))
            for i in range(4):
                groups.append((C_RG + 512 * i, 512, "tm", dict(func=AF.Copy, scale=1.0, dst=RG, rn="RG", row0=0, col0=512 * i)))
            for i in range(2):
                groups.append((C_MQ + 512 * i, 512, "T", dict(scale=256 ** -0.5, dst=MQT, rn="MQT", row0=512 * i)))
            for i in range(12):
                groups.append((C_GT + 512 * i, 512, "tm", dict(func=AF.Sigmoid, scale=1.0, dst=GATES, rn="GATES", row0=0, col0=512 * i)))

            def run_groups(groups, hT, r_hT):
                pending = []
                gi_next = [0]

                def prefetch():
                    while gi_next[0] < len(groups) and len(pending) < 1:
                        c0, n, kind, prm = groups[gi_next[0]]
                        pending.append(load_w(wring, win_d, KC, c0, n))
                        gi_next[0] += 1

                prefetch()
                for gi, (c0, n, kind, prm) in enumerate(groups):
                    wt, r_w = pending.pop(0)
                    prefetch()
                    sT = r_sT = sM = r_sM = None
                    if kind in ("ropeT", "T"):
                        sT, r_sT = stT.next()
                    if kind in ("rope64", "ikw"):
                        sT, r_sT = st64.next()
                    if kind in ("tm", "rope_tm") or (kind == "ropeT" and "tm_dst" in prm):
                        sM, r_sM = stM.next()
                    for t in range(TT):
                        ps, r_ps = psf.next()
                        for c in range(KC):
                            k.pe(lambda e: e.matmul(ps[:, 0:n], lhsT=hT[:, c, t * 128:(t + 1) * 128], rhs=wt[:, c, 0:n],
                                                    start=(c == 0), stop=(c == KC - 1)), rd=[r_hT, r_w], wr=[r_ps])
                        if kind == "tm":
                            k.act(lambda e: e.activation(out=sM[:, t, 0:n], in_=ps[:, 0:n], func=prm["func"],
                                                         scale=prm["scale"]), rd=[r_ps], wr=[r_sM])
                        elif kind == "T":
                            xr, r_xr = xrr.next()
                            k.act(lambda e: e.activation(out=xr[:, 0:n], in_=ps[:, 0:n], func=AF.Copy, scale=prm["scale"]),
                                  rd=[r_ps], wr=[r_xr])
                            transposes(xr, r_xr, n, 128, sT, r_sT, t)
                        elif kind == "ropeT":
                            xs, r_xs = xsr.next()
                            xr, r_xr = xrr.next()
                            k.act(lambda e: e.activation(out=xs[:, 0:n], in_=ps[:, 0:n], func=AF.Copy, scale=prm["scale"]),
                                  rd=[r_ps], wr=[r_xs])
                            rope(xs, r_xs, xr, r_xr, n, prm["hd"], t)
                            transposes(xr, r_xr, n, 128, sT, r_sT, t)
                            if sM is not None:
                                k.dve(lambda e: e.tensor_copy(out=sM[:, t, 0:n], in_=xr[:, 0:n]), rd=[r_xr], wr=[r_sM])
                        elif kind == "rope_tm":
                            xs, r_xs = xsr.next()
                            k.act(lambda e: e.activation(out=xs[:, 0:n], in_=ps[:, 0:n], func=AF.Copy, scale=prm["scale"]),
                                  rd=[r_ps], wr=[r_xs])
                            rope(xs, r_xs, sM[:, t, :], r_sM, n, prm["hd"], t)
                        elif kind == "rope64":
                            xs, r_xs = xsr.next()
                            xr, r_xr = xrr.next()
                            k.act(lambda e: e.activation(out=xs[:, 0:n], in_=ps[:, 0:n], func=AF.Copy, scale=prm["scale"]),
                                  rd=[r_ps], wr=[r_xs])
                            rope(xs, r_xs, xr, r_xr, n, 64, t)
                            transposes64(xr, r_xr, n, sT, r_sT, t)
                        elif kind == "ikw":
                            xs, r_xs = xsr.next()
                            xr, r_xr = xrr.next()
                            k.act(lambda e: e.activation(out=xs[:, 0:64], in_=ps[:, 0:64], func=AF.Copy), rd=[r_ps], wr=[r_xs])
                            if prm["iw"]:
                                k.act(lambda e: e.activation(out=iwst[:, t, :], in_=ps[:, 64:80], func=AF.Copy, scale=0.25),
                                      rd=[r_ps], wr=[r_iwst])
                            rope(xs, r_xs, xr, r_xr, 64, 64, t)
                            transposes64(xr, r_xr, 64, sT, r_sT, t)
                    if kind in ("ropeT", "T"):
                        dst = prm["dst"][prm["row0"]:prm["row0"] + n, :].rearrange("(j f) t -> f j t", f=128)
                        k.dma("sync", dst, sT[:, 0:n // 128, :], rd=[r_sT], wr=[R[prm["rn"]]])
                        if sM is not None:
                            dst = prm["tm_dst"][:, prm["tm_col0"]:prm["tm_col0"] + n].rearrange("(t p) n -> p t n", p=128)
                            k.dma("sync", dst, sM[:, :, 0:n], rd=[r_sM], wr=[R[prm["tm_rn"]]])
                    elif kind == "rope64":
                        dst = prm["dst"][prm["row0"]:prm["row0"] + n, :].rearrange("(j f) t -> f j t", f=64)
                        k.dma("sync", dst, sT[0:64, 0:n // 64, :], rd=[r_sT], wr=[R[prm["rn"]]])
                    elif kind == "ikw":
                        if prm["iw"]:
                            k.dma("sync", IW.rearrange("(t p) n -> p t n", p=128), iwst[:], rd=[r_iwst], wr=[R["IW"]])
                        else:
                            k.dma("sync", prm["dst"][prm["row0"]:prm["row0"] + 64, :], sT[0:64, 0, :], rd=[r_sT],
                                  wr=[R[prm["rn"]]])
                    elif kind == "tm":
                        dst = prm["dst"][prm["row0"]:prm["row0"] + NT, prm["col0"]:prm["col0"] + n].rearrange(
                            "(t p) n -> p t n", p=128)
                        k.dma("sync", dst, sM[:, :, 0:n], rd=[r_sM], wr=[R[prm["rn"]]])
                    elif kind == "rope_tm":
                        dst = prm["tm_dst"][prm["tm_row0"]:prm["tm_row0"] + NT, prm["tm_col0"]:prm["tm_col0"] + n].rearrange(
                            "(t p) n -> p t n", p=128)
                        k.dma("sync", dst, sM[:, :, 0:n], rd=[r_sM], wr=[R[prm["tm_rn"]]])

            run_groups(groups, hT, r_hT)
            if upto == "p2":
                k.barrier()
                return nc
            for r in range(NCORES):
                hT, r_hT = hTr.next()
                csh[0], csh[1] = csr.next()
                compute_cs(csh[0], csh[1], posf_all, r * TT)
                for t in range(TT):
                    norm_T([q.next() for q in nrings], xall_d[r * NT + t * 128:r * NT + (t + 1) * 128, :], [], 0, hT, r_hT,
                           t * 128)
                rgl = []
                for i in range(2):
                    rgl.append((C_AK + 512 * i, 512, "ropeT", dict(hd=128, scale=1.0, dst=KVI_all, rn="KVI_all",
                                                                   row0=r * 2112 + 512 * i)))
                for i in range(2):
                    rgl.append((C_AV + 512 * i, 512, "tm", dict(func=AF.Copy, scale=1.0, dst=KVI_all, rn="KVI_all",
                                                                row0=r * 2112 + 1024, col0=512 * i)))
                rgl.append((C_IK, 64, "ikw", dict(iw=False, dst=KVI_all, rn="KVI_all", row0=r * 2112 + 2048)))
                for i in range(2):
                    rgl.append((C_RK + 512 * i, 512, "rope_tm", dict(hd=128, scale=1.0, tm_dst=RK_all, tm_rn="RK_all",
                                                                     tm_row0=r * NT, tm_col0=512 * i)))
                for i in range(4):
                    rgl.append((C_RV + 512 * i, 512, "tm", dict(func=AF.Copy, scale=1.0, dst=RV_all, rn="RV_all",
                                                                row0=r * NT, col0=512 * i)))
                run_groups(rgl, hT, r_hT)
            k.barrier()

        if upto == "p2r":
            return nc
        with ExitStack() as ph:
            iqT = sb(ph, "iqT", [64, 16, NT], BF16)
            ikT = sb(ph, "ikT", [64, S], BF16)
            iw = sb(ph, "iw", [128, TT, 16], F32)
            d4 = sb(ph, "d4", [128, 512], F32)
            cbg = sb(ph, "cbg", [128, 128], F32)
            acc = sb(ph, "acc", [128, S], F32)
            sel = sb(ph, "sel", [128, S], BF16)
            mst = Ring(ph, nc, "mst", [128, 64, 128], BF16, 1)
            rr = Ring(ph, nc, "rr", [128, 512], F32, 3)
            bs = Ring(ph, nc, "bs", [128, 8], F32, 2)
            r_in = Res()
            r_acc = [Res() for _ in range(16)]
            r_sel = Res()
            k.dma("sync", iqT[:], IQT.rearrange("(h d) t -> d h t", d=64), rd=[R["IQT"]], wr=[r_in])
            k.dma("sync", ikT[:].rearrange("d (r t) -> d r t", t=NT),
                  KVI_all.rearrange("(r x) t -> x r t", x=2112)[2048:2112], rd=[R["KVI_all"]], wr=[r_in])
            k.dma("sync", iw[:], IW.rearrange("(t p) n -> p t n", p=128), rd=[R["IW"]], wr=[r_in])
            k.dma("sync", d4[:], d4_d[:, :], wr=[r_in])
            k.dma("sync", cbg[:], cbg_d[:, :], wr=[r_in])
            thr_all = sb(ph, "thr_all", [128, TT], F32)
            r_thr = Res()
            for i in range(TT):
                for g in range(16):
                    a_g = acc[:, g * 512:(g + 1) * 512]
                    k.dve(lambda e: e.tensor_scalar(out=a_g, in0=d4[:], scalar1=cbg[:, i * 16 + g:i * 16 + g + 1],
                                                    scalar2=NEG, op0=ALU.is_gt, op1=ALU.mult),
                          rd=[r_in], wr=[r_acc[g]])
                    for h in range(16):
                        ps, r_ps = psf.next()
                        k.pe(lambda e: e.matmul(ps[:], lhsT=iqT[:, h, i * 128:(i + 1) * 128],
                                                rhs=ikT[:, g * 512:(g + 1) * 512], start=True, stop=True),
                             rd=[r_in], wr=[r_ps])
                        rl, r_rl = rr.next()
                        k.act(lambda e: e.activation(out=rl[:], in_=ps[:], func=AF.Relu), rd=[r_ps], wr=[r_rl])
                        k.dve(lambda e: e.scalar_tensor_tensor(out=a_g, in0=rl[:], scalar=iw[:, i, h:h + 1], in1=a_g,
                                                               op0=ALU.mult, op1=ALU.add),
                              rd=[r_rl, r_in, r_acc[g]], wr=[r_acc[g]])
                if SCORE is not None:
                    k.dma("sync", SCORE[i * 128:(i + 1) * 128, :], acc[:], rd=r_acc, wr=[R["SCORE"]])
                b, r_b = bs.next()
                k.dve(lambda e: e.reduce_max(out=b[:, 0:1], in_=acc[:], axis=AX.X), rd=r_acc, wr=[r_b])
                k.dve(lambda e: e.tensor_scalar(out=b[:, 0:1], in0=b[:, 0:1], scalar1=50.0 + 1e-3, scalar2=None,
                                                op0=ALU.add), rd=[r_b], wr=[r_b])
                k.dve(lambda e: e.memset(b[:, 1:2], -50.0), rd=[r_b], wr=[r_b])
                for it in range(NBIS):
                    k.dve(lambda e: e.tensor_scalar(out=b[:, 2:3], in0=b[:, 0:1], scalar1=2.0 ** -(it + 1), scalar2=None,
                                                    op0=ALU.mult), rd=[r_b], wr=[r_b])
                    k.dve(lambda e: e.tensor_tensor(out=b[:, 3:4], in0=b[:, 2:3], in1=b[:, 1:2], op=ALU.add),
                          rd=[r_b], wr=[r_b])
                    k.dve(lambda e: e.tensor_scalar(out=sel[:], in0=acc[:], scalar1=b[:, 3:4], scalar2=None,
                                                    op0=ALU.is_ge, op1=ALU.add, accum_out=b[:, 4:5]),
                          rd=[r_b] + r_acc, wr=[r_sel, r_b])
                    k.dve(lambda e: e.tensor_scalar(out=b[:, 5:6], in0=b[:, 4:5], scalar1=TOPK - 0.5, scalar2=None,
                                                    op0=ALU.is_ge), rd=[r_b], wr=[r_b])
                    k.dve(lambda e: e.scalar_tensor_tensor(out=b[:, 1:2], in0=b[:, 2:3], scalar=b[:, 5:6],
                                                           in1=b[:, 1:2], op0=ALU.mult, op1=ALU.add),
                          rd=[r_b], wr=[r_b])
                k.dve(lambda e: e.tensor_scalar(out=sel[:], in0=acc[:], scalar1=b[:, 1:2], scalar2=None, op0=ALU.is_ge),
                      rd=[r_b] + r_acc, wr=[r_sel])
                k.dve(lambda e: e.tensor_copy(out=thr_all[:, i:i + 1], in_=b[:, 1:2]), rd=[r_b], wr=[r_thr])
                ms, r_ms = mst.next()
                for kb8 in range(8):
                    pb, r_pb = psb.next()
                    for j in range(8):
                        kb = kb8 * 8 + j
                        k.pe(lambda e: e.transpose(out=pb[:, j * 128:(j + 1) * 128], in_=sel[:, kb * 128:(kb + 1) * 128],
                                                   identity=ident[:]), rd=[r_sel, r_const], wr=[r_pb])
                    k.act(lambda e: e.activation(out=ms[:, kb8 * 8:(kb8 + 1) * 8, :],
                                                 in_=pb[:].rearrange("p (j q) -> p j q", q=128), func=AF.Copy),
                          rd=[r_pb], wr=[r_ms])
                k.dma("sync", MT[i], ms[:], rd=[r_ms], wr=[R["MT"]])
            k.dma("sync", THR[:, :], thr_all[:], rd=[r_thr], wr=[R["THR"]])
            k.barrier()

        if upto == "p4":
            return nc
        with ExitStack() as ph:
            qT = sb(ph, "qT", [128, 8, NT], BF16)
            r_q = Res()
            k.dma("sync", qT[:], QT.rearrange("(h d) t -> d h t", d=128), rd=[R["QT"]], wr=[r_q])
            kTr = Ring(ph, nc, "kT", [128, S], BF16, 2)
            vSr = Ring(ph, nc, "vS", [128, 64, 128], BF16, 2)
            mT = sb(ph, "mT", [128, 4, 64, 128], BF16)
            r_mT = Res()
            er = Ring(ph, nc, "er", [128, 512], BF16, 3)
            pr = Ring(ph, nc, "pr", [128, 512], BF16, 3)
            rdn = Ring(ph, nc, "rdn", [128, 512], F32, 2)
            oaT = sb(ph, "oaT", [128, 8, NT], BF16)
            r_oaT = Res()
            ps_s = [(psf.t[j], psf.r[j]) for j in (0, 1)]
            ps_o = [(psf.t[j], psf.r[j]) for j in (2, 3)]
            ps_d = [(psf.t[j], psf.r[j]) for j in (4, 5)]
            kvx = KVI_all.rearrange("(r x) t -> x r t", x=2112)
            it = 0
            for qc in range(2):
                k.dma("sync", mT[:], MT[qc * 4:(qc + 1) * 4].rearrange("i k b q -> k i b q"), rd=[R["MT"]], wr=[r_mT])
                for h in range(8):
                    kT, r_kT = kTr.next()
                    vS, r_vS = vSr.next()
                    k.dma("sync", kT[:].rearrange("d (r t) -> d r t", t=NT), kvx[h * 128:(h + 1) * 128],
                          rd=[R["KVI_all"]], wr=[r_kT])
                    for r in range(NCORES):
                        k.dma("sync", vS[:, r * 8:(r + 1) * 8, :],
                              KVI_all[r * 2112 + 1024:r * 2112 + 2048, h * 128:(h + 1) * 128].rearrange(
                                  "(t p) d -> p t d", p=128), rd=[R["KVI_all"]], wr=[r_vS])
                    po, r_po = ps_o[h % 2]
                    pd, r_pd = ps_d[h % 2]
                    for kb in range(64):
                        pS, r_pS = ps_s[it % 2]
                        it += 1
                        k.pe(lambda e: e.matmul(pS[:], lhsT=kT[:, kb * 128:(kb + 1) * 128],
                                                rhs=qT[:, h, qc * 512:(qc + 1) * 512], start=True, stop=True),
                             rd=[r_kT, r_q], wr=[r_pS])
                        ee, r_ee = er.next()
                        k.act(lambda e: e.activation(out=ee[:], in_=pS[:], func=AF.Exp), rd=[r_pS], wr=[r_ee])
                        pp, r_pp = pr.next()
                        k.dve(lambda e: e.tensor_tensor(out=pp[:].rearrange("p (i q) -> p i q", q=128),
                                                        in0=ee[:].rearrange("p (i q) -> p i q", q=128),
                                                        in1=mT[:, :, kb, :], op=ALU.mult),
                              rd=[r_ee, r_mT], wr=[r_pp])
                        k.pe(lambda e: e.matmul(po[:], lhsT=vS[:, kb, :], rhs=pp[:], start=(kb == 0), stop=(kb == 63)),
                             rd=[r_vS, r_pp], wr=[r_po])
                        k.pe(lambda e: e.matmul(pd[:], lhsT=ones[:], rhs=pp[:], start=(kb == 0), stop=(kb == 63)),
                             rd=[r_cs, r_pp], wr=[r_pd])
                    rd_, r_rd = rdn.next()
                    k.dve(lambda e: e.reciprocal(out=rd_[:], in_=pd[:]), rd=[r_pd], wr=[r_rd])
                    k.dve(lambda e: e.tensor_tensor(out=oaT[:, h, qc * 512:(qc + 1) * 512], in0=po[:], in1=rd_[:],
                                                    op=ALU.mult), rd=[r_po, r_rd], wr=[r_oaT])
            k.dma("sync", OAT.rearrange("(h d) t -> d h t", d=128), oaT[:], rd=[r_oaT], wr=[R["OAT"]])
            k.barrier()

        if upto == "p5":
            return nc
        with ExitStack() as ph:
            dmt = sb(ph, "dmt", [128, 8, 128], F32)
            qdec = sb(ph, "qdec", [128, 8, 128], F32)
            kdec = sb(ph, "kdec", [128, 8], F32)
            dch = sb(ph, "dch", [128, 8], F32)
            coef = sb(ph, "coef", [128, 64], F32)
            Lst = sb(ph, "Lst", [128, 8, 256], F32)
            Sst = sb(ph, "Sst", [128, 8, 256], F32)
            r_ld = Res()
            r_L = [Res() for _ in range(8)]
            r_S = [Res() for _ in range(8)]
            k.dma("sync", dmt[:].rearrange("p h i -> p (h i)"), dmt_d[:, :], wr=[r_ld])
            k.dma("sync", qdec[:].rearrange("p h i -> p (h i)"), qdec_d[:, :], wr=[r_ld])
            k.dma("sync", kdec[:], kdec_d[:, :], wr=[r_ld])
            k.dma("sync", dch[:], dch_d[:, :], wr=[r_ld])
            k.dma("sync", coef[:], coef_d[:, :], wr=[r_ld])

            def kv_update(st, r_st, kk, r_kk, vv, r_vv, h, n, first):
                ps, r_ps = psf.next()
                k.pe(lambda e: e.matmul(ps[:, 0:256], lhsT=kk[:, n, h * 128:(h + 1) * 128],
                                        rhs=vv[:, n, h * 256:(h + 1) * 256], start=True, stop=True),
                     rd=[r_kk, r_vv], wr=[r_ps])
                if first:
                    k.dve(lambda e: e.tensor_copy(out=st[:, h, :], in_=ps[:, 0:256]), rd=[r_ps], wr=[r_st[h]])
                else:
                    k.dve(lambda e: e.scalar_tensor_tensor(out=st[:, h, :], in0=st[:, h, :], scalar=dch[:, h:h + 1],
                                                           in1=ps[:, 0:256], op0=ALU.mult, op1=ALU.add),
                          rd=[r_ps, r_ld, r_st[h]], wr=[r_st[h]])

            with ExitStack() as pa:
                rkr = Ring(pa, nc, "rkA", [128, TT, 1024], BF16, 2)
                rvr = Ring(pa, nc, "rvA", [128, TT, 2048], BF16, 2)
                for c2 in range(NCORES):
                    rka, r_rka = rkr.next()
                    rva, r_rva = rvr.next()
                    k.dma("sync", rka[:], RK_all[c2 * NT:(c2 + 1) * NT, :].rearrange("(t p) n -> p t n", p=128),
                          rd=[R["RK_all"]], wr=[r_rka])
                    k.dma("sync", rva[:], RV_all[c2 * NT:(c2 + 1) * NT, :].rearrange("(t p) n -> p t n", p=128),
                          rd=[R["RV_all"]], wr=[r_rva])
                    for h in range(8):
                        k.dve(lambda e: e.tensor_scalar(out=rka[:, :, h * 128:(h + 1) * 128],
                                                        in0=rka[:, :, h * 128:(h + 1) * 128],
                                                        scalar1=kdec[:, h:h + 1], scalar2=None, op0=ALU.mult),
                              rd=[r_ld, r_rka], wr=[r_rka])
                    for h in range(8):
                        for n in range(TT):
                            kv_update(Lst, r_L, rka, r_rka, rva, r_rva, h, n, n == 0)
                        cf = coef[:, c2 * 8 + h:c2 * 8 + h + 1]
                        if c2 == 0:
                            k.dve(lambda e: e.tensor_scalar(out=Sst[:, h, :], in0=Lst[:, h, :], scalar1=cf, scalar2=None,
                                                            op0=ALU.mult), rd=[r_L[h], r_ld], wr=[r_S[h]])
                        else:
                            k.dve(lambda e: e.scalar_tensor_tensor(out=Sst[:, h, :], in0=Lst[:, h, :], scalar=cf,
                                                                   in1=Sst[:, h, :], op0=ALU.mult, op1=ALU.add),
                                  rd=[r_L[h], r_ld, r_S[h]], wr=[r_S[h]])
                k.barrier()

            rqT = sb(ph, "rqT", [128, 8, NT], BF16)
            rkT = sb(ph, "rkT", [128, 8, NT], BF16)
            rqTd = sb(ph, "rqTd", [128, 8, NT], BF16)
            rks = sb(ph, "rks", [128, TT, 1024], BF16)
            rv = sb(ph, "rv", [128, TT, 2048], BF16)
            Sbf = Ring(ph, nc, "Sbf", [128, 256], BF16, 2)
            rgr = Ring(ph, nc, "rg", [128, 2048], BF16, 2)
            sTr = Ring(ph, nc, "scT", [128, 128], BF16, 3)
            ynr = Ring(ph, nc, "yn", [128, 256], F32, 3)
            sgr = Ring(ph, nc, "sg", [128, 256], F32, 3)
            bnr = Ring(ph, nc, "bn", [128, 16], F32, 3)
            obr = Ring(ph, nc, "ob", [128, 2048], BF16, 2)
            obT = sb(ph, "obT", [128, 16, NT], BF16)
            r_rks = Res()
            r_rv = Res()
            r_rqTd = Res()
            r_obT = Res()
            k.dma("sync", rqT[:], RQT.rearrange("(h d) t -> d h t", d=128), rd=[R["RQT"]], wr=[r_ld])
            k.dma("sync", rkT[:], RKT.rearrange("(h d) t -> d h t", d=128), rd=[R["RKT"]], wr=[r_ld])
            k.dma("sync", rks[:], RK.rearrange("(t p) n -> p t n", p=128), rd=[R["RK"]], wr=[r_rks])
            k.dma("sync", rv[:], RV.rearrange("(t p) n -> p t n", p=128), rd=[R["RV"]], wr=[r_rv])
            for h in range(8):
                k.dve(lambda e: e.tensor_scalar(out=rks[:, :, h * 128:(h + 1) * 128], in0=rks[:, :, h * 128:(h + 1) * 128],
                                                scalar1=kdec[:, h:h + 1], scalar2=None, op0=ALU.mult),
                      rd=[r_ld, r_rks], wr=[r_rks])
                k.dve(lambda e: e.tensor_tensor(out=rqTd[:, h, :].rearrange("p (n i) -> p n i", i=128),
                                                in0=rqT[:, h, :].rearrange("p (n i) -> p n i", i=128),
                                                in1=qdec[:, h, :].unsqueeze(1).to_broadcast([128, TT, 128]),
                                                op=ALU.mult), rd=[r_ld], wr=[r_rqTd])
            for n in range(TT):
                rg, r_rg = rgr.next()
                k.dma("sync", rg[:], RG[n * 128:(n + 1) * 128, :], rd=[R["RG"]], wr=[r_rg])
                ob, r_ob = obr.next()
                for h in range(8):
                    sbf, r_sbf = Sbf.next()
                    k.act(lambda e: e.activation(out=sbf[:], in_=Sst[:, h, :], func=AF.Copy), rd=[r_S[h]], wr=[r_sbf])
                    pS, r_pS = psf.next()
                    k.pe(lambda e: e.matmul(pS[:, 0:128], lhsT=rkT[:, h, n * 128:(n + 1) * 128],
                                            rhs=rqT[:, h, n * 128:(n + 1) * 128], start=True, stop=True),
                         rd=[r_ld], wr=[r_pS])
                    sT_, r_sT_ = sTr.next()
                    k.dve(lambda e: e.tensor_tensor(out=sT_[:], in0=pS[:, 0:128], in1=dmt[:, h, :], op=ALU.mult),
                          rd=[r_pS, r_ld], wr=[r_sT_])
                    pY, r_pY = psf.next()
                    k.pe(lambda e: e.matmul(pY[:, 0:256], lhsT=sT_[:], rhs=rv[:, n, h * 256:(h + 1) * 256],
                                            start=True, stop=False), rd=[r_sT_, r_rv], wr=[r_pY])
                    k.pe(lambda e: e.matmul(pY[:, 0:256], lhsT=rqTd[:, h, n * 128:(n + 1) * 128], rhs=sbf[:],
                                            start=False, stop=True), rd=[r_rqTd, r_sbf], wr=[r_pY])
                    bn, r_bn = bnr.next()
                    k.dve(lambda e: e.bn_stats(out=bn[:, 0:6], in_=pY[:, 0:256]), rd=[r_pY], wr=[r_bn])
                    k.dve(lambda e: e.bn_aggr(out=bn[:, 8:10], in_=bn[:, 0:6]), rd=[r_bn], wr=[r_bn])
                    k.act(lambda e: e.activation(out=bn[:, 10:11], in_=bn[:, 9:10], func=AF.Sqrt, bias=EPS),
                          rd=[r_bn], wr=[r_bn])
                    k.dve(lambda e: e.reciprocal(out=bn[:, 11:12], in_=bn[:, 10:11]), rd=[r_bn], wr=[r_bn])
                    yn, r_yn = ynr.next()
                    k.dve(lambda e: e.tensor_scalar(out=yn[:], in0=pY[:, 0:256], scalar1=bn[:, 8:9], scalar2=bn[:, 11:12],
                                                    op0=ALU.subtract, op1=ALU.mult), rd=[r_pY, r_bn], wr=[r_yn])
                    sg, r_sg = sgr.next()
                    k.act(lambda e: e.activation(out=sg[:], in_=rg[:, h * 256:(h + 1) * 256], func=AF.Silu),
                          rd=[r_rg], wr=[r_sg])
                    k.dve(lambda e: e.tensor_tensor(out=ob[:, h * 256:(h + 1) * 256], in0=yn[:], in1=sg[:], op=ALU.mult),
                          rd=[r_yn, r_sg], wr=[r_ob])
                    if n < TT - 1:
                        kv_update(Sst, r_S, rks, r_rks, rv, r_rv, h, n, False)
                for half in range(2):
                    pb, r_pb = psb.next()
                    for j in range(8):
                        c = half * 8 + j
                        k.pe(lambda e: e.transpose(out=pb[:, j * 128:(j + 1) * 128], in_=ob[:, c * 128:(c + 1) * 128],
                                                   identity=ident[:]), rd=[r_ob, r_const], wr=[r_pb])
                    k.act(lambda e: e.activation(out=obT[:, half * 8:half * 8 + 8, n * 128:(n + 1) * 128],
                                                 in_=pb[:].rearrange("p (j q) -> p j q", q=128), func=AF.Copy),
                          rd=[r_pb], wr=[r_obT])
            k.dma("sync", OBT.rearrange("(c f) t -> f c t", f=128), obT[:], rd=[r_obT], wr=[R["OBT"]])
            k.barrier()

        if upto == "p6":
            return nc
        with ExitStack() as ph:
            memT = sb(ph, "memT", [128, KC, 256], BF16)
            r_memT = Res()
            with ExitStack() as p1:
                rings = [Ring(p1, nc, "mxt", [128, D], F32, 2), Ring(p1, nc, "mxb", [128, D], BF16, 2),
                         Ring(p1, nc, "mst_", [128, 4], F32, 2), Ring(p1, nc, "mjk", [128, D], BF16, 1)]
                for t in range(2):
                    norm_T([r.next() for r in rings], mem_d[t * 128:(t + 1) * 128, :], [], 32, memT, r_memT, t * 128)
                k.barrier()
            wmr = Ring(ph, nc, "wm", [128, KC, 512], BF16, 2)
            mkT = sb(ph, "mkT", [128, 8, 256], BF16)
            mv = sb(ph, "mv", [128, 2, 1024], BF16)
            mqT = sb(ph, "mqT", [128, 8, NT], BF16)
            ocT = sb(ph, "ocT", [128, 8, NT], BF16)
            pmr = Ring(ph, nc, "pm", [128, 512], BF16, 4)
            rdn = Ring(ph, nc, "rdm", [128, 512], F32, 2)
            r_mk = Res()
            r_mv = Res()
            r_mq = Res()
            r_oc = Res()
            k.dma("sync", mqT[:], MQT.rearrange("(c f) t -> f c t", f=128), rd=[R["MQT"]], wr=[r_mq])
            for g in range(2):
                wt, r_w = load_w(wmr, wmk_d, KC, g * 512, 512)
                for j in range(4):
                    ps, r_ps = psf.next()
                    for c in range(KC):
                        k.pe(lambda e: e.matmul(ps[:, 0:256], lhsT=wt[:, c, j * 128:(j + 1) * 128], rhs=memT[:, c, :],
                                                start=(c == 0), stop=(c == KC - 1)), rd=[r_w, r_memT], wr=[r_ps])
                    k.act(lambda e: e.activation(out=mkT[:, g * 4 + j, :], in_=ps[:, 0:256], func=AF.Copy),
                          rd=[r_ps], wr=[r_mk])
            for g in range(2):
                wt, r_w = load_w(wmr, wmk_d, KC, 1024 + g * 512, 512)
                for mt in range(2):
                    ps, r_ps = psf.next()
                    for c in range(KC):
                        k.pe(lambda e: e.matmul(ps[:], lhsT=memT[:, c, mt * 128:(mt + 1) * 128], rhs=wt[:, c, :],
                                                start=(c == 0), stop=(c == KC - 1)), rd=[r_w, r_memT], wr=[r_ps])
                    k.act(lambda e: e.activation(out=mv[:, mt, g * 512:(g + 1) * 512], in_=ps[:], func=AF.Copy),
                          rd=[r_ps], wr=[r_mv])
            for hm in range(4):
                for qc in range(2):
                    pms = []
                    for mt in range(2):
                        ps, r_ps = psf.next()
                        for dc in range(2):
                            k.pe(lambda e: e.matmul(ps[:], lhsT=mkT[:, hm * 2 + dc, mt * 128:(mt + 1) * 128],
                                                    rhs=mqT[:, hm * 2 + dc, qc * 512:(qc + 1) * 512],
                                                    start=(dc == 0), stop=(dc == 1)), rd=[r_mk, r_mq], wr=[r_ps])
                        pm, r_pm = pmr.next()
                        k.act(lambda e: e.activation(out=pm[:], in_=ps[:], func=AF.Exp), rd=[r_ps], wr=[r_pm])
                        pms.append((pm, r_pm))
                    pd, r_pd = psf.next()
                    for mt in range(2):
                        k.pe(lambda e: e.matmul(pd[:], lhsT=ones[:], rhs=pms[mt][0][:], start=(mt == 0), stop=(mt == 1)),
                             rd=[r_cs, pms[mt][1]], wr=[r_pd])
                    rd_, r_rd = rdn.next()
                    k.dve(lambda e: e.reciprocal(out=rd_[:], in_=pd[:]), rd=[r_pd], wr=[r_rd])
                    for ec in range(2):
                        po, r_po = psf.next()
                        for mt in range(2):
                            k.pe(lambda e: e.matmul(po[:], lhsT=mv[:, mt, hm * 256 + ec * 128:hm * 256 + (ec + 1) * 128],
                                                    rhs=pms[mt][0][:], start=(mt == 0), stop=(mt == 1)),
                                 rd=[r_mv, pms[mt][1]], wr=[r_po])
                        k.dve(lambda e: e.tensor_tensor(out=ocT[:, hm * 2 + ec, qc * 512:(qc + 1) * 512], in0=po[:],
                                                        in1=rd_[:], op=ALU.mult), rd=[r_po, r_rd], wr=[r_oc])
            k.dma("sync", OCT.rearrange("(c f) t -> f c t", f=128), ocT[:], rd=[r_oc], wr=[R["OCT"]])
            k.barrier()

        if upto == "p7":
            return nc
        with ExitStack() as ph:
            oT = sb(ph, "oT", [128, 32, NT], BF16)
            r_oT = Res()
            k.dma("sync", oT[:, 0:8, :], OAT.rearrange("(c f) t -> f c t", f=128), rd=[R["OAT"]], wr=[r_oT])
            k.dma("sync", oT[:, 8:24, :], OBT.rearrange("(c f) t -> f c t", f=128), rd=[R["OBT"]], wr=[r_oT])
            k.dma("sync", oT[:, 24:32, :], OCT.rearrange("(c f) t -> f c t", f=128), rd=[R["OCT"]], wr=[r_oT])
            wbr = Ring(ph, nc, "wbr", [128, 32, 512], BF16, 2)
            gtr = Ring(ph, nc, "gt", [128, TT, 3, 512], BF16, 1)
            tr = Ring(ph, nc, "tm3", [128, 512], F32, 4)
            mxr = Ring(ph, nc, "mx", [128, TT, 512], BF16, 2)
            branches = ((0, 8, wa_d), (8, 16, wb_d), (24, 8, wc_d))
            for cg in range(4):
                wt, r_w = wbr.next()
                for (c0, nk, wsrc) in branches:
                    for a in range(0, nk, 8):
                        k.dma("gpsimd", wt[:, c0 + a:c0 + a + 8, :],
                              wsrc[a * 128:(a + 8) * 128, cg * 512:(cg + 1) * 512].rearrange("(c p) n -> p c n", p=128),
                              wr=[r_w])
                gt, r_gt = gtr.next()
                for b in range(3):
                    k.dma("sync", gt[:, :, b, :],
                          GATES[:, b * D + cg * 512:b * D + (cg + 1) * 512].rearrange("(t p) n -> p t n", p=128),
                          rd=[R["GATES"]], wr=[r_gt])
                mx, r_mx = mxr.next()
                for t in range(TT):
                    tmps = []
                    for b, (c0, nk, wsrc) in enumerate(branches):
                        ps, r_ps = psf.next()
                        for c in range(nk):
                            k.pe(lambda e: e.matmul(ps[:], lhsT=oT[:, c0 + c, t * 128:(t + 1) * 128], rhs=wt[:, c0 + c, :],
                                                    start=(c == 0), stop=(c == nk - 1)), rd=[r_oT, r_w], wr=[r_ps])
                        tm_, r_tm = tr.next()
                        k.dve(lambda e: e.tensor_tensor(out=tm_[:], in0=ps[:], in1=gt[:, t, b, :], op=ALU.mult),
                              rd=[r_ps, r_gt], wr=[r_tm])
                        tmps.append((tm_, r_tm))
                    k.dve(lambda e: e.tensor_tensor(out=tmps[0][0][:], in0=tmps[0][0][:], in1=tmps[1][0][:], op=ALU.add),
                          rd=[tmps[0][1], tmps[1][1]], wr=[tmps[0][1]])
                    k.dve(lambda e: e.tensor_tensor(out=mx[:, t, :], in0=tmps[0][0][:], in1=tmps[2][0][:], op=ALU.add),
                          rd=[tmps[0][1], tmps[2][1]], wr=[r_mx])
                k.dma("sync", MIX[:, cg * 512:(cg + 1) * 512].rearrange("(t p) n -> p t n", p=128), mx[:],
                      rd=[r_mx], wr=[R["MIX"]])
            k.barrier()

        if upto == "p8a":
            return nc
        with ExitStack() as ph0:
            h2T = sb(ph0, "h2T", [128, KC, NT], BF16)
            r_h2T = Res()
            gpm = sb(ph0, "gpm", [128, D], F32)
            r_g = Res()
            k.dma("sync", gpm[:], gpm_d[:, :], wr=[r_g])

            def post_norm_residual(z, r_z, gp, resid, r_resid, outt, r_out, stt, r_stt, jk, r_jk):
                k.act(lambda e: e.activation(out=jk[:], in_=z[:], func=AF.Square, accum_out=stt[:, 0:1]),
                      rd=[r_z], wr=[r_jk, r_stt])
                k.act(lambda e: e.activation(out=stt[:, 1:2], in_=stt[:, 0:1], func=AF.Sqrt, scale=1.0 / D, bias=EPS),
                      rd=[r_stt], wr=[r_stt])
                k.dve(lambda e: e.reciprocal(out=stt[:, 2:3], in_=stt[:, 1:2]), rd=[r_stt], wr=[r_stt])
                k.dve(lambda e: e.scalar_tensor_tensor(out=z[:], in0=z[:], scalar=stt[:, 2:3], in1=gp[:],
                                                       op0=ALU.mult, op1=ALU.mult), rd=[r_z, r_stt, r_g], wr=[r_z])
                k.dve(lambda e: e.tensor_tensor(out=outt[:], in0=z[:], in1=resid[:], op=ALU.add),
                      rd=[r_z, r_resid], wr=[r_out])

            with ExitStack() as ph:
                wo = sb(ph, "wo", [128, KC, D], BF16)
                r_wo = Res()
                for a in range(0, KC, 2):
                    k.dma("gpsimd", wo[:, a:a + 2, :], wo_d[a * 128:(a + 2) * 128, :].rearrange("(c p) n -> p c n", p=128),
                          wr=[r_wo])
                mixT = sb(ph, "mixT", [128, KC, NT], BF16)
                r_mixT = Res()
                mtr = Ring(ph, nc, "mtile", [128, D], BF16, 1)
                for t in range(TT):
                    mt_, r_mt = mtr.next()
                    k.dma("sync", mt_[:], MIX[t * 128:(t + 1) * 128, :], rd=[R["MIX"]], wr=[r_mt])
                    for half in range(2):
                        pb, r_pb = psb.next()
                        for j in range(8):
                            c = half * 8 + j
                            k.pe(lambda e: e.transpose(out=pb[:, j * 128:(j + 1) * 128], in_=mt_[:, c * 128:(c + 1) * 128],
                                                       identity=ident[:]), rd=[r_mt, r_const], wr=[r_pb])
                        k.act(lambda e: e.activation(out=mixT[:, half * 8:half * 8 + 8, t * 128:(t + 1) * 128],
                                                     in_=pb[:].rearrange("p (j q) -> p j q", q=128), func=AF.Copy),
                              rd=[r_pb], wr=[r_mixT])
                zr = Ring(ph, nc, "z", [128, D], F32, 2)
                xr_ = Ring(ph, nc, "xres", [128, D], F32, 1)
                x1r = Ring(ph, nc, "x1", [128, D], F32, 2)
                str_ = Ring(ph, nc, "pst", [128, 4], F32, 2)
                jkr = Ring(ph, nc, "pjk", [128, D], BF16, 1)
                hbr = Ring(ph, nc, "hb", [128, D], BF16, 1)
                for t in range(TT):
                    z, r_z = zr.next()
                    for cg in range(4):
                        ps, r_ps = psf.next()
                        for c in range(KC):
                            k.pe(lambda e: e.matmul(ps[:], lhsT=mixT[:, c, t * 128:(t + 1) * 128],
                                                    rhs=wo[:, c, cg * 512:(cg + 1) * 512],
                                                    start=(c == 0), stop=(c == KC - 1)), rd=[r_mixT, r_wo], wr=[r_ps])
                        k.act(lambda e: e.activation(out=z[:, cg * 512:(cg + 1) * 512], in_=ps[:], func=AF.Copy),
                              rd=[r_ps], wr=[r_z])
                    xres, r_xres = xr_.next()
                    k.dma("sync", xres[:], x_d[t * 128:(t + 1) * 128, :], wr=[r_xres])
                    x1, r_x1 = x1r.next()
                    stt, r_stt = str_.next()
                    jk, r_jk = jkr.next()
                    post_norm_residual(z, r_z, gpm, xres, r_xres, x1, r_x1, stt, r_stt, jk, r_jk)
                    k.dma("sync", X1[t * 128:(t + 1) * 128, :], x1[:], rd=[r_x1], wr=[R["X1"]])
                    stt2, r_stt2 = str_.next()
                    hb, r_hb = hbr.next()
                    k.act(lambda e: e.activation(out=jk[:], in_=x1[:], func=AF.Square, accum_out=stt2[:, 0:1]),
                          rd=[r_x1], wr=[r_jk, r_stt2])
                    k.act(lambda e: e.activation(out=stt2[:, 1:2], in_=stt2[:, 0:1], func=AF.Sqrt, scale=1.0 / D, bias=EPS),
                          rd=[r_stt2], wr=[r_stt2])
                    k.dve(lambda e: e.reciprocal(out=stt2[:, 2:3], in_=stt2[:, 1:2]), rd=[r_stt2], wr=[r_stt2])
                    k.dve(lambda e: e.tensor_scalar(out=hb[:], in0=x1[:], scalar1=stt2[:, 2:3], scalar2=None, op0=ALU.mult),
                          rd=[r_x1, r_stt2], wr=[r_hb])
                    for half in range(2):
                        pb, r_pb = psb.next()
                        for j in range(8):
                            c = half * 8 + j
                            k.pe(lambda e: e.transpose(out=pb[:, j * 128:(j + 1) * 128], in_=hb[:, c * 128:(c + 1) * 128],
                                                       identity=ident[:]), rd=[r_hb, r_const], wr=[r_pb])
                        gb = gfm[:, 16 + half * 8:16 + half * 8 + 8].unsqueeze(2).to_broadcast([128, 8, 128])
                        k.dve(lambda e: e.tensor_tensor(out=h2T[:, half * 8:half * 8 + 8, t * 128:(t + 1) * 128],
                                                        in0=pb[:].rearrange("p (j q) -> p j q", q=128), in1=gb,
                                                        op=ALU.mult), rd=[r_pb, r_const], wr=[r_h2T])
                k.barrier()

            if upto == "p8b":
                return nc
            with ExitStack() as ph:
                actT = sb(ph, "actT", [128, 44, NT], BF16)
                r_act = Res()
                with ExitStack() as p9:
                    wgr = Ring(p9, nc, "wg", [128, KC, 256], BF16, 2)
                    wur = Ring(p9, nc, "wu", [128, KC, 256], BF16, 2)
                    sgr = Ring(p9, nc, "fsg", [128, 512], F32, 3)
                    pend = []
                    nxt = 0

                    def pre9():
                        nonlocal nxt
                        while nxt < 22 and len(pend) < 1:
                            a = load_w(wgr, wfi_d, KC, nxt * 256, 256, nsplit=2)
                            b = load_w(wur, wfi_d, KC, DFF + nxt * 256, 256, nsplit=2)
                            pend.append((a, b))
                            nxt += 1
                    pre9()
                    for jj in range(22):
                        (wg, r_wg), (wu, r_wu) = pend.pop(0)
                        pre9()
                        for a in range(2):
                            j = jj * 2 + a
                            for b in range(2):
                                pg, r_pg = psf.next()
                                pu, r_pu = psf.next()
                                for c in range(KC):
                                    k.pe(lambda e: e.matmul(pg[:], lhsT=wg[:, c, a * 128:(a + 1) * 128],
                                                            rhs=h2T[:, c, b * 512:(b + 1) * 512],
                                                            start=(c == 0), stop=(c == KC - 1)), rd=[r_wg, r_h2T], wr=[r_pg])
                                for c in range(KC):
                                    k.pe(lambda e: e.matmul(pu[:], lhsT=wu[:, c, a * 128:(a + 1) * 128],
                                                            rhs=h2T[:, c, b * 512:(b + 1) * 512],
                                                            start=(c == 0), stop=(c == KC - 1)), rd=[r_wu, r_h2T], wr=[r_pu])
                                sg, r_sg = sgr.next()
                                k.act(lambda e: e.activation(out=sg[:], in_=pg[:], func=AF.Silu), rd=[r_pg], wr=[r_sg])
                                k.dve(lambda e: e.tensor_tensor(out=actT[:, j, b * 512:(b + 1) * 512], in0=sg[:], in1=pu[:],
                                                                op=ALU.mult), rd=[r_sg, r_pu], wr=[r_act])
                    k.barrier()
                with ExitStack() as p10:
                    wfr = Ring(p10, nc, "wf", [128, 44, 256], BF16, 2)
                    yst = Ring(p10, nc, "yst", [128, TT, 256], F32, 2)
                    pend = []
                    nxt = 0

                    def pre10():
                        nonlocal nxt
                        while nxt < 8 and len(pend) < 1:
                            pend.append(load_w(wfr, wfo_d, 44, nxt * 256, 256, nsplit=4))
                            nxt += 1
                    pre10()
                    for cg in range(8):
                        wf, r_wf = pend.pop(0)
                        pre10()
                        ys, r_ys = yst.next()
                        for t in range(TT):
                            ps, r_ps = psf.next()
                            for j in range(44):
                                k.pe(lambda e: e.matmul(ps[:, 0:256], lhsT=actT[:, j, t * 128:(t + 1) * 128], rhs=wf[:, j, :],
                                                        start=(j == 0), stop=(j == 43)), rd=[r_act, r_wf], wr=[r_ps])
                            k.act(lambda e: e.activation(out=ys[:, t, :], in_=ps[:, 0:256], func=AF.Copy),
                                  rd=[r_ps], wr=[r_ys])
                        k.dma("sync", Y2[:, cg * 256:(cg + 1) * 256].rearrange("(t p) n -> p t n", p=128), ys[:],
                              rd=[r_ys], wr=[R["Y2"]])
                    k.barrier()

            with ExitStack() as ph:
                gpf = sb(ph, "gpf", [128, D], F32)
                k.dma("sync", gpf[:], gpf_d[:, :], wr=[r_g])
                zr = Ring(ph, nc, "fz", [128, D], F32, 2)
                xr_ = Ring(ph, nc, "fx1", [128, D], F32, 2)
                orr = Ring(ph, nc, "fo", [128, D], F32, 2)
                str_ = Ring(ph, nc, "fst", [128, 4], F32, 2)
                jkr = Ring(ph, nc, "fjk", [128, D], BF16, 1)
                for t in range(TT):
                    z, r_z = zr.next()
                    x1, r_x1 = xr_.next()
                    o, r_o = orr.next()
                    stt, r_stt = str_.next()
                    jk, r_jk = jkr.next()
                    k.dma("sync", z[:], Y2[t * 128:(t + 1) * 128, :], rd=[R["Y2"]], wr=[r_z])
                    k.dma("sync", x1[:], X1[t * 128:(t + 1) * 128, :], rd=[R["X1"]], wr=[r_x1])
                    post_norm_residual(z, r_z, gpf, x1, r_x1, o, r_o, stt, r_stt, jk, r_jk)
                    k.dma("sync", out_d[t * 128:(t + 1) * 128, :], o[:], rd=[r_o], wr=[R["out"]])
                k.barrier()
    return nc


_NC_CACHE = {}


def _consts():
    ident = np.eye(128, dtype=np.float32).astype(ml_dtypes.bfloat16)
    fr = (10000.0 ** (-np.arange(0, 128, 2, dtype=np.float32) / np.float32(128))).astype(np.float32)
    freqs = np.broadcast_to(fr[None, :], (128, 64)).copy()
    q = np.arange(128)[:, None]
    sidx = np.arange(512)[None, :]
    d4 = (sidx - q).astype(np.float32)
    H = 8
    log_gamma = np.log1p(-np.exp2(-5.0 - np.arange(H, dtype=np.float64)))
    i = np.arange(128, dtype=np.float64)
    diff = i[None, :] - i[:, None]
    dmt = np.where(diff[:, None, :] >= 0, np.exp(np.maximum(diff, 0)[:, None, :] * log_gamma[None, :, None]), 0.0) * 128 ** -0.5
    qdec = np.exp((i + 1)[None, None, :] * log_gamma[None, :, None]) * np.ones((128, 1, 1))
    kdec = np.exp((127 - i)[:, None] * log_gamma[None, :]) * 128 ** -0.5
    dch = np.exp(128 * log_gamma)[None, :] * np.ones((128, 1))
    return dict(ident=ident, freqs=freqs, d4=d4, dmt=dmt.reshape(128, 1024).astype(np.float32),
                qdec=qdec.reshape(128, 1024).astype(np.float32), kdec=kdec.astype(np.float32),
                dch=dch.astype(np.float32)), log_gamma


def make_in_maps(inputs):
    c, log_gamma = _consts()
    x = np.asarray(inputs["x"], dtype=np.float32)[0]
    mem = np.ascontiguousarray(np.asarray(inputs["mem"], dtype=np.float32)[0])
    pos = np.asarray(inputs["positions"]).astype(np.int32)[0]

    def fm(g):
        return np.asarray(g, dtype=np.float32)[0].reshape(16, 128).T
    gfm = np.ascontiguousarray(np.concatenate([fm(inputs["g_pre_mix"]), fm(inputs["g_pre_ffn"]), fm(inputs["g_mem"])], axis=1))
    gpm = np.ascontiguousarray(np.broadcast_to(np.asarray(inputs["g_post_mix"], dtype=np.float32)[0][None, :], (128, D)))
    gpf = np.ascontiguousarray(np.broadcast_to(np.asarray(inputs["g_post_ffn"], dtype=np.float32)[0][None, :], (128, D)))
    shared = dict(mem=mem, gfm=gfm, gpm=gpm, gpf=gpf, x_all=np.ascontiguousarray(x),
                  pos_all=np.ascontiguousarray(pos.reshape(NCORES * TT, 128).T),
                  w_in=np.ascontiguousarray(np.asarray(inputs["w_in"], dtype=np.float32)[0]),
                  w_mem_kv=np.ascontiguousarray(np.asarray(inputs["w_mem_kv"], dtype=np.float32)[0]),
                  w_a=np.ascontiguousarray(np.asarray(inputs["w_branch_a"], dtype=np.float32)[0]),
                  w_b=np.ascontiguousarray(np.asarray(inputs["w_branch_b"], dtype=np.float32)[0]),
                  w_c=np.ascontiguousarray(np.asarray(inputs["w_branch_c"], dtype=np.float32)[0]),
                  w_out=np.ascontiguousarray(np.asarray(inputs["w_out"], dtype=np.float32)[0]),
                  w_fi=np.ascontiguousarray(np.asarray(inputs["w_ffn_in"], dtype=np.float32)[0]),
                  w_fo=np.ascontiguousarray(np.asarray(inputs["w_ffn_out"], dtype=np.float32)[0]),
                  **c)
    maps = []
    for core in range(NCORES):
        m = dict(shared)
        m["x"] = np.ascontiguousarray(x[core * NT:(core + 1) * NT])
        m["pos"] = np.ascontiguousarray(pos[core * NT:(core + 1) * NT].reshape(TT, 128).T)
        cbg = np.zeros((TT, 16), np.float32)
        for i in range(TT):
            for g in range(16):
                cbg[i, g] = 128.0 * ((core * TT + i) - 4 * g)
        m["cbg"] = np.ascontiguousarray(np.broadcast_to(cbg.reshape(1, 128), (128, 128)))
        coef = np.zeros((NCORES, 8), np.float64)
        for c2 in range(NCORES):
            if c2 < core:
                coef[c2] = np.exp(128.0 * TT * (core - 1 - c2) * log_gamma)
        m["coef"] = np.ascontiguousarray(np.broadcast_to(coef.reshape(1, 64).astype(np.float32), (128, 64)))
        maps.append(m)
    return maps


def kernel(**inputs):
    if "nc" not in _NC_CACHE:
        _NC_CACHE["nc"] = build(False)
    nc = _NC_CACHE["nc"]
    maps = make_in_maps(inputs)
    res = run_bass_kernel_spmd(nc, maps, core_ids=list(range(NCORES)))
    out = np.concatenate([np.asarray(r["out"], dtype=np.float32) for r in res.results], axis=0)
    return out.reshape(1, S, D)
```

```python
import math
from contextlib import ExitStack

import numpy as np
import ml_dtypes
import concourse.bass as bass
import concourse.mybir as mybir
from concourse.bass_utils import run_bass_kernel_spmd

F32 = mybir.dt.float32
BF16 = mybir.dt.bfloat16
I32 = mybir.dt.int32
ALU = mybir.AluOpType
AF = mybir.ActivationFunctionType
AX = mybir.AxisListType

NCORES = 8
S = 8192
D = 2048
NT = S // NCORES
TT = NT // 128
KC = D // 128
IN_COLS = 17488
DFF = 5632
EPS = 1e-6
NEG = -1000.0
NBIS = 16
TOPK = 256

C_AQ, C_AK, C_AV, C_IQ, C_IK, C_IW, C_RQ, C_RK, C_RV, C_RG, C_MQ, C_GT = (
    0, 1024, 2048, 3072, 4096, 4160, 4176, 5200, 6224, 8272, 10320, 11344)


class Res:
    __slots__ = ("w", "rd")

    def __init__(self):
        self.w = {}
        self.rd = {}


class Issuer:
    def __init__(self, eng):
        self.eng = eng
        self.waited = {}


class KB:
    def __init__(self, nc, es):
        self.nc = nc
        self.iss = {n: Issuer(getattr(nc, n)) for n in ("sync", "scalar", "vector", "gpsimd", "tensor")}
        self.csem = {n: es.enter_context(nc.semaphore("c_" + n)) for n in ("scalar", "vector", "gpsimd", "tensor")}
        self.ccount = {n: 0 for n in self.csem}
        self.dsems = {}
        self.dcount = {}
        self.dnext = {}
        for q, n in (("sync", 16), ("gpsimd", 8), ("scalar", 4)):
            self.dsems[q] = [es.enter_context(nc.semaphore("d_%s%d" % (q, i))) for i in range(n)]
            self.dnext[q] = 0
            for s in self.dsems[q]:
                self.dcount[s] = 0

    def _wait(self, iss, need):
        for s, v in need.items():
            if iss.waited.get(s, 0) >= v:
                continue
            iss.eng.wait_ge(s, v)
            iss.waited[s] = v

    def _deps(self, rd, wr, own, raw_same, acc=False):
        need = {}

        def add(h, same_ok):
            s, v = h
            if s is own and not same_ok:
                return
            if need.get(s, 0) < v:
                need[s] = v
        for r in rd:
            for h in r.w.items():
                add(h, raw_same)
        for r in wr:
            if not acc:
                for h in r.w.items():
                    add(h, False)
            for h in r.rd.values():
                add(h, False)
        return need

    def op(self, en, fn, rd=(), wr=()):
        iss = self.iss[en]
        sem = self.csem[en]
        need = self._deps(rd, wr, sem, en != "tensor")
        self._wait(iss, need)
        ins = fn(iss.eng)
        self.ccount[en] += 1
        ins.then_inc(sem, 1)
        h = (sem, self.ccount[en])
        for r in rd:
            r.rd[sem] = h
        for r in wr:
            r.w = {h[0]: h[1]}
            r.rd = {}
        return ins

    def act(self, fn, rd=(), wr=()):
        return self.op("scalar", fn, rd, wr)

    def dve(self, fn, rd=(), wr=()):
        return self.op("vector", fn, rd, wr)

    def pe(self, fn, rd=(), wr=()):
        return self.op("tensor", fn, rd, wr)

    def dma(self, q, out, in_, rd=(), wr=(), acc=False, **kw):
        iss = self.iss[q]
        sems = self.dsems[q]
        s = sems[self.dnext[q] % len(sems)]
        self.dnext[q] += 1
        need = self._deps(rd, wr, None, True, acc)
        if self.dcount[s] > 0 and need.get(s, 0) < self.dcount[s]:
            need[s] = self.dcount[s]
        self._wait(iss, need)
        ins = iss.eng.dma_start(out=out, in_=in_, **kw)
        ins.then_inc(s, 16)
        self.dcount[s] += 16
        h = (s, self.dcount[s])
        for r in rd:
            r.rd[s] = h
        for r in wr:
            if acc:
                r.w[s] = h[1]
            else:
                r.w = {s: h[1]}
                r.rd = {}
        return ins

    def collective(self, kind, ins_, outs_, rd=(), wr=()):
        q = "gpsimd"
        iss = self.iss[q]
        sems = self.dsems[q]
        s = sems[self.dnext[q] % len(sems)]
        self.dnext[q] += 1
        need = self._deps(rd, wr, None, True)
        if self.dcount[s] > 0 and need.get(s, 0) < self.dcount[s]:
            need[s] = self.dcount[s]
        self._wait(iss, need)
        ins = iss.eng.collective_compute(kind, ALU.bypass, replica_groups=[list(range(NCORES))],
                                         ins=ins_, outs=outs_)
        ins.then_inc(s, 16)
        self.dcount[s] += 16
        h = (s, self.dcount[s])
        for r in rd:
            r.rd[s] = h
        for r in wr:
            r.w = {s: h[1]}
            r.rd = {}
        return ins

    def barrier(self):
        need = {}
        for n, sem in self.csem.items():
            if self.ccount[n] > 0:
                need[sem] = self.ccount[n]
        for s, v in self.dcount.items():
            if v > 0:
                need[s] = v
        for n, iss in self.iss.items():
            self._wait(iss, dict(need))


_UID = [0]


class Ring:
    def __init__(self, es, nc, name, shape, dt, n, psum=False):
        self.t = []
        self.r = []
        _UID[0] += 1
        name = "r%d_%s" % (_UID[0], name)
        for i in range(n):
            if psum:
                t = es.enter_context(nc.psum_tensor("%s%d" % (name, i), shape, dt))
            else:
                t = es.enter_context(nc.sbuf_tensor("%s%d" % (name, i), shape, dt))
            self.t.append(t)
            self.r.append(Res())
        self.i = 0

    def next(self):
        j = self.i % len(self.t)
        self.i += 1
        return self.t[j], self.r[j]


def build(debug=False, upto=None):
    nc = bass.Bass("TRN2", target_bir_lowering=False)
    okind = "ExternalOutput" if debug else "Internal"

    def din(name, shape, dt):
        return nc.dram_tensor(name, shape, dt, kind="ExternalInput").ap()

    def dscr(name, shape, dt, dbg=True):
        return nc.dram_tensor(name, shape, dt, kind=(okind if dbg else "Internal")).ap()

    x_d = din("x", [NT, D], F32)
    xall_d = din("x_all", [S, D], F32)
    pos_d = din("pos", [128, TT], I32)
    posall_d = din("pos_all", [128, NCORES * TT], I32)
    mem_d = din("mem", [256, D], F32)
    gfm_d = din("gfm", [128, 48], F32)
    gpm_d = din("gpm", [128, D], F32)
    gpf_d = din("gpf", [128, D], F32)
    win_d = din("w_in", [D, IN_COLS], F32)
    wmk_d = din("w_mem_kv", [D, 2048], F32)
    wa_d = din("w_a", [1024, D], F32)
    wb_d = din("w_b", [2048, D], F32)
    wc_d = din("w_c", [1024, D], F32)
    wo_d = din("w_out", [D, D], F32)
    wfi_d = din("w_fi", [D, 2 * DFF], F32)
    wfo_d = din("w_fo", [DFF, D], F32)
    ident_d = din("ident", [128, 128], BF16)
    freqs_d = din("freqs", [128, 64], F32)
    d4_d = din("d4", [128, 512], F32)
    cbg_d = din("cbg", [128, 128], F32)
    dmt_d = din("dmt", [128, 1024], F32)
    qdec_d = din("qdec", [128, 1024], F32)
    kdec_d = din("kdec", [128, 8], F32)
    dch_d = din("dch", [128, 8], F32)
    coef_d = din("coef", [128, 64], F32)
    out_d = nc.dram_tensor("out", [NT, D], F32, kind="ExternalOutput").ap()

    QT = dscr("QT", [1024, NT], BF16)
    KVI_loc = dscr("KVI_loc", [2112, NT], BF16, dbg=False)
    KVI_dbg = dscr("KVI_dbg", [2112, NT], BF16) if debug else None
    KVI_all = dscr("KVI_all", [NCORES * 2112, NT], BF16)
    RK_all = dscr("RK_all", [S, 1024], BF16, dbg=False)
    RV_all = dscr("RV_all", [S, 2048], BF16, dbg=False)
    IQT = dscr("IQT", [1024, NT], BF16)
    IW = dscr("IW", [NT, 16], F32)
    RQT = dscr("RQT", [1024, NT], BF16)
    RKT = dscr("RKT", [1024, NT], BF16)
    RK = dscr("RK", [NT, 1024], BF16)
    RV = dscr("RV", [NT, 2048], BF16)
    RG = dscr("RG", [NT, 2048], BF16)
    MQT = dscr("MQT", [1024, NT], BF16)
    GATES = dscr("GATES", [NT, 3 * D], BF16)
    MT = dscr("MT", [TT, 128, 64, 128], BF16, dbg=False)
    SCORE = dscr("SCORE", [NT, S], F32) if debug else None
    THR = dscr("THR", [128, TT], F32)
    LST_loc = dscr("LST_loc", [1024, 256], F32, dbg=False)
    LST_all = dscr("LST_all", [NCORES * 1024, 256], F32, dbg=False)
    OAT = dscr("OAT", [1024, NT], BF16)
    OBT = dscr("OBT", [2048, NT], BF16)
    OCT = dscr("OCT", [1024, NT], BF16)
    MIX = dscr("MIX", [NT, D], BF16)
    X1 = dscr("X1", [NT, D], F32)
    WSH = dscr("WSH", [D, 5632], BF16, dbg=False)
    Y2 = dscr("Y2", [NT, D], F32)

    R = {}
    for nme in ("QT", "KVI_loc", "KVI_all", "IQT", "IW", "RQT", "RKT", "RK", "RV", "RG", "MQT", "GATES", "MT",
                "SCORE", "THR", "LST_loc", "LST_all", "RK_all", "RV_all", "WSH", "OAT", "OBT", "OCT", "MIX", "X1", "Y2", "out"):
        R[nme] = Res()

    es = ExitStack()
    with es:
        k = KB(nc, es)

        def sb(st, name, shape, dt):
            _UID[0] += 1
            return st.enter_context(nc.sbuf_tensor("s%d_%s" % (_UID[0], name), shape, dt))

        ident = sb(es, "ident", [128, 128], BF16)
        freqs = sb(es, "freqs", [128, 64], F32)
        gfm = sb(es, "gfm", [128, 48], F32)
        posi = sb(es, "posi", [128, TT], I32)
        posf = sb(es, "posf", [128, TT], F32)
        ones = sb(es, "ones", [128, 128], BF16)
        r_const = Res()
        r_cs = Res()
        k.dma("sync", ident[:], ident_d[:, :], wr=[r_const])
        k.dma("sync", freqs[:], freqs_d[:, :], wr=[r_const])
        k.dma("sync", gfm[:], gfm_d[:, :], wr=[r_const])
        k.dma("sync", posi[:], pos_d[:, :], wr=[r_const])
        k.dve(lambda e: e.tensor_copy(out=posf[:], in_=posi[:]), rd=[r_const], wr=[r_cs])
        k.dve(lambda e: e.memset(ones[:], 1.0), wr=[r_cs])

        psf = Ring(es, nc, "psf", [128, 512], F32, 6, psum=True)
        psb = Ring(es, nc, "psb", [128, 1024], BF16, 2, psum=True)

        ang = sb(es, "ang", [128, 64], F32)
        u = sb(es, "u", [128, 64], F32)
        ki = sb(es, "ki", [128, 64], I32)
        kf = sb(es, "kf", [128, 64], F32)
        mm = sb(es, "mm", [128, 64], F32)
        rt = Res()
        posi_all = sb(es, "posi_all", [128, NCORES * TT], I32)
        posf_all = sb(es, "posf_all", [128, NCORES * TT], F32)
        k.dma("sync", posi_all[:], posall_d[:, :], wr=[r_const])
        k.dve(lambda e: e.tensor_copy(out=posf_all[:], in_=posi_all[:]), rd=[r_const], wr=[r_cs])

        def compute_cs(cs, r_cst, pf, col0):
            for t in range(TT):
                k.dve(lambda e: e.tensor_scalar(out=ang[:], in0=freqs[:], scalar1=pf[:, col0 + t:col0 + t + 1], scalar2=None,
                                                op0=ALU.mult), rd=[r_const, r_cs, rt], wr=[rt])
                for which, off in ((1, 0.5), (0, 0.75)):
                    k.dve(lambda e: e.tensor_scalar(out=u[:], in0=ang[:], scalar1=1.0 / (2 * math.pi), scalar2=off,
                                                    op0=ALU.mult, op1=ALU.add), rd=[rt], wr=[rt])
                    k.dve(lambda e: e.tensor_copy(out=ki[:], in_=u[:]), rd=[rt], wr=[rt])
                    k.dve(lambda e: e.tensor_copy(out=kf[:], in_=ki[:]), rd=[rt], wr=[rt])
                    k.dve(lambda e: e.tensor_tensor(out=u[:], in0=u[:], in1=kf[:], op=ALU.subtract), rd=[rt], wr=[rt])
                    k.dve(lambda e: e.tensor_scalar(out=mm[:], in0=u[:], scalar1=0.5, scalar2=None, op0=ALU.is_gt),
                          rd=[rt], wr=[rt])
                    k.dve(lambda e: e.tensor_tensor(out=u[:], in0=u[:], in1=mm[:], op=ALU.subtract), rd=[rt], wr=[rt])
                    k.act(lambda e: e.activation(out=cs[:, t, which, :], in_=u[:], func=AF.Sin,
                                                 scale=-2 * math.pi * (1 - 1e-6)), rd=[rt], wr=[r_cst])

        csr = Ring(es, nc, "cs", [128, TT, 2, 64], F32, 2)
        csh = list(csr.next())
        compute_cs(csh[0], csh[1], posf, 0)

        def norm_T(st_ring, src_ap, r_src_rd, g_col0, dstT, r_dst, col0):
            (xt, r_xt), (xb, r_xb), (st, r_st), (jk, r_jk) = st_ring
            k.dma("sync", xt[:], src_ap, rd=r_src_rd, wr=[r_xt])
            k.act(lambda e: e.activation(out=jk[:], in_=xt[:], func=AF.Square, accum_out=st[:, 0:1]),
                  rd=[r_xt], wr=[r_jk, r_st])
            k.act(lambda e: e.activation(out=st[:, 1:2], in_=st[:, 0:1], func=AF.Sqrt, scale=1.0 / D, bias=EPS),
                  rd=[r_st], wr=[r_st])
            k.dve(lambda e: e.reciprocal(out=st[:, 2:3], in_=st[:, 1:2]), rd=[r_st], wr=[r_st])
            k.dve(lambda e: e.tensor_scalar(out=xb[:], in0=xt[:], scalar1=st[:, 2:3], scalar2=None, op0=ALU.mult),
                  rd=[r_xt, r_st], wr=[r_xb])
            for half in range(2):
                pb, r_pb = psb.next()
                for j in range(8):
                    c = half * 8 + j
                    k.pe(lambda e: e.transpose(out=pb[:, j * 128:(j + 1) * 128], in_=xb[:, c * 128:(c + 1) * 128],
                                               identity=ident[:]), rd=[r_xb, r_const], wr=[r_pb])
                gb = gfm[:, g_col0 + half * 8:g_col0 + half * 8 + 8].unsqueeze(2).to_broadcast([128, 8, 128])
                k.dve(lambda e: e.tensor_tensor(out=dstT[:, half * 8:half * 8 + 8, col0:col0 + 128],
                                                in0=pb[:].rearrange("p (j q) -> p j q", q=128), in1=gb, op=ALU.mult),
                      rd=[r_pb, r_const], wr=[r_dst])

        def load_w(ring, src, rows_kc, c0, n, q="gpsimd", nsplit=4):
            wt, r_w = ring.next()
            step = max(1, rows_kc // nsplit)
            for a in range(0, rows_kc, step):
                b = min(rows_kc, a + step)
                k.dma(q, wt[:, a:b, 0:n],
                      src[a * 128:b * 128, c0:c0 + n].rearrange("(c p) n -> p c n", p=128), wr=[r_w], acc=(a > 0))
            return wt, r_w

        with ExitStack() as ph:
            hTr = Ring(ph, nc, "hT", [128, KC, NT], BF16, 2)
            hT, r_hT = hTr.next()
            nrings = [Ring(ph, nc, "xt", [128, D], F32, 2), Ring(ph, nc, "xb", [128, D], BF16, 2),
                      Ring(ph, nc, "st", [128, 4], F32, 2), Ring(ph, nc, "jk", [128, D], BF16, 1)]
            for t in range(TT):
                norm_T([r.next() for r in nrings], x_d[t * 128:(t + 1) * 128, :], [], 0, hT, r_hT, t * 128)

            wring = Ring(ph, nc, "win", [128, KC, 512], BF16, 2)
            xsr = Ring(ph, nc, "xs", [128, 512], F32, 2)
            xrr = Ring(ph, nc, "xr", [128, 512], BF16, 2)
            tmr = Ring(ph, nc, "tmp", [128, 256], F32, 4)
            stT = Ring(ph, nc, "stT", [128, 4, NT], BF16, 2)
            stM = Ring(ph, nc, "stM", [128, TT, 512], BF16, 2)
            iwst = sb(ph, "iwst", [128, TT, 16], F32)
            r_iwst = Res()

            def rope(xs, r_xs, xr, r_xr, n, hd, t):
                nh = n // hd
                half = hd // 2
                xv = xs[:, 0:n].rearrange("p (h two f) -> p h two f", two=2, f=half)
                ov = xr[:, 0:n].rearrange("p (h two f) -> p h two f", two=2, f=half)
                cs, r_csx = csh
                if hd == 128:
                    cos = cs[:, t, 0, :]
                    sin = cs[:, t, 1, :]
                else:
                    cos = cs[:, t, 0, 0::2]
                    sin = cs[:, t, 1, 0::2]
                cosb = cos.unsqueeze(1).to_broadcast([128, nh, half])
                sinb = sin.unsqueeze(1).to_broadcast([128, nh, half])
                hw = nh * half
                (t1, r1), (t2, r2) = tmr.next(), tmr.next()
                t1v = t1[:, 0:hw].rearrange("p (h f) -> p h f", f=half)
                t2v = t2[:, 0:hw].rearrange("p (h f) -> p h f", f=half)
                k.dve(lambda e: e.tensor_tensor(out=t1v, in0=xv[:, :, 0, :], in1=cosb, op=ALU.mult),
                      rd=[r_xs, r_csx], wr=[r1])
                k.dve(lambda e: e.tensor_tensor(out=t2v, in0=xv[:, :, 1, :], in1=sinb, op=ALU.mult),
                      rd=[r_xs, r_csx], wr=[r2])
                k.dve(lambda e: e.tensor_tensor(out=ov[:, :, 0, :], in0=t1v, in1=t2v, op=ALU.subtract),
                      rd=[r1, r2], wr=[r_xr])
                (t3, r3), (t4, r4) = tmr.next(), tmr.next()
                t3v = t3[:, 0:hw].rearrange("p (h f) -> p h f", f=half)
                t4v = t4[:, 0:hw].rearrange("p (h f) -> p h f", f=half)
                k.dve(lambda e: e.tensor_tensor(out=t3v, in0=xv[:, :, 1, :], in1=cosb, op=ALU.mult),
                      rd=[r_xs, r_csx], wr=[r3])
                k.dve(lambda e: e.tensor_tensor(out=t4v, in0=xv[:, :, 0, :], in1=sinb, op=ALU.mult),
                      rd=[r_xs, r_csx], wr=[r4])
                k.dve(lambda e: e.tensor_tensor(out=ov[:, :, 1, :], in0=t3v, in1=t4v, op=ALU.add),
                      rd=[r3, r4], wr=[r_xr])

            def transposes(xr, r_xr, n, fw, sT, r_sT, t):
                nj = n // 128
                pb, r_pb = psb.next()
                for j in range(nj):
                    k.pe(lambda e: e.transpose(out=pb[:, j * 128:(j + 1) * 128], in_=xr[:, j * 128:(j + 1) * 128],
                                               identity=ident[:]), rd=[r_xr, r_const], wr=[r_pb])
                k.act(lambda e: e.activation(out=sT[:, 0:nj, t * 128:(t + 1) * 128],
                                             in_=pb[:, 0:nj * 128].rearrange("p (j q) -> p j q", q=128), func=AF.Copy),
                      rd=[r_pb], wr=[r_sT])

            st64 = Ring(ph, nc, "st64", [64, 8, NT], BF16, 1)

            def transposes64(xr, r_xr, n, sT, r_sT, t):
                nj = n // 64
                pb, r_pb = psb.next()
                for j in range(nj):
                    k.pe(lambda e: e.transpose(out=pb[0:64, j * 128:(j + 1) * 128], in_=xr[:, j * 64:(j + 1) * 64],
                                               identity=ident[:]), rd=[r_xr, r_const], wr=[r_pb])
                k.act(lambda e: e.activation(out=sT[0:64, 0:nj, t * 128:(t + 1) * 128],
                                             in_=pb[0:64, 0:nj * 128].rearrange("p (j q) -> p j q", q=128),
                                             func=AF.Copy), rd=[r_pb], wr=[r_sT])

            groups = []
            for i in range(2):
                groups.append((C_AQ + 512 * i, 512, "ropeT", dict(hd=128, scale=128 ** -0.5, dst=QT, rn="QT", row0=512 * i)))
            for i in range(2):
                groups.append((C_IQ + 512 * i, 512, "rope64", dict(scale=64 ** -0.5, dst=IQT, rn="IQT", row0=512 * i)))
            groups.append((C_IK, 80, "ikw", dict(iw=True, row0=0, dst=KVI_loc, rn="KVI_loc")))
            for i in range(2):
                groups.append((C_RQ + 512 * i, 512, "ropeT", dict(hd=128, scale=1.0, dst=RQT, rn="RQT", row0=512 * i)))
            for i in range(2):
                groups.append((C_RK + 512 * i, 512, "ropeT", dict(hd=128, scale=1.0, dst=RKT, rn="RKT", row0=512 * i,
                                                                 tm_dst=RK, tm_rn="RK", tm_col0=512 * i)))
            for i in range(4):
                groups.append((C_RV + 512 * i, 512, "tm", dict(func=AF.Copy, scale=1.0, dst=RV, rn="RV", row0=0, col0=512 * i)))
            for i in range(4):
                groups.append((C_RG + 512 * i, 512, "tm", dict(func=AF.Copy, scale=1.0, dst=RG, rn="RG", row0=0, col0=512 * i)))
            for i in range(2):
                groups.append((C_MQ + 512 * i, 512, "T", dict(scale=256 ** -0.5, dst=MQT, rn="MQT", row0=512 * i)))
            for i in range(12):
                groups.append((C_GT + 512 * i, 512, "tm", dict(func=AF.Sigmoid, scale=1.0, dst=GATES, rn="GATES", row0=0, col0=512 * i)))

            def run_groups(groups, hT, r_hT, cached=False):
                pending = []
                gi_next = [0]

                def prefetch():
                    while gi_next[0] < len(groups) and len(pending) < 1:
                        c0, n, kind, prm = groups[gi_next[0]]
                        if cached and c0 in wsh_off:
                            wt_, r_w_ = wring.next()
                            o = wsh_off[c0]
                            for a in range(0, KC, 4):
                                k.dma("sync", wt_[:, a:a + 4, 0:n],
                                      WSH[a * 128:(a + 4) * 128, o:o + n].rearrange("(c p) n -> p c n", p=128),
                                      rd=[R["WSH"]], wr=[r_w_], acc=(a > 0))
                            pending.append((wt_, r_w_))
                        else:
                            wt_, r_w_ = load_w(wring, win_d, KC, c0, n)
                            if (not cached) and c0 in wsh_off:
                                o = wsh_off[c0]
                                k.dma("sync", WSH[:, o:o + n].rearrange("(c p) n -> p c n", p=128), wt_[:, :, 0:n],
                                      rd=[r_w_], wr=[R["WSH"]], acc=True)
                            pending.append((wt_, r_w_))
                        gi_next[0] += 1

                prefetch()
                for gi, (c0, n, kind, prm) in enumerate(groups):
                    wt, r_w = pending.pop(0)
                    prefetch()
                    sT = r_sT = sM = r_sM = None
                    if kind in ("ropeT", "T"):
                        sT, r_sT = stT.next()
                    if kind in ("rope64", "ikw"):
                        sT, r_sT = st64.next()
                    if kind in ("tm", "rope_tm") or (kind == "ropeT" and "tm_dst" in prm):
                        sM, r_sM = stM.next()
                    for t in range(TT):
                        ps, r_ps = psf.next()
                        for c in range(KC):
                            k.pe(lambda e: e.matmul(ps[:, 0:n], lhsT=hT[:, c, t * 128:(t + 1) * 128], rhs=wt[:, c, 0:n],
                                                    start=(c == 0), stop=(c == KC - 1)), rd=[r_hT, r_w], wr=[r_ps])
                        if kind == "tm":
                            k.act(lambda e: e.activation(out=sM[:, t, 0:n], in_=ps[:, 0:n], func=prm["func"],
                                                         scale=prm["scale"]), rd=[r_ps], wr=[r_sM])
                        elif kind == "T":
                            xr, r_xr = xrr.next()
                            k.act(lambda e: e.activation(out=xr[:, 0:n], in_=ps[:, 0:n], func=AF.Copy, scale=prm["scale"]),
                                  rd=[r_ps], wr=[r_xr])
                            transposes(xr, r_xr, n, 128, sT, r_sT, t)
                        elif kind == "ropeT":
                            xs, r_xs = xsr.next()
                            xr, r_xr = xrr.next()
                            k.act(lambda e: e.activation(out=xs[:, 0:n], in_=ps[:, 0:n], func=AF.Copy, scale=prm["scale"]),
                                  rd=[r_ps], wr=[r_xs])
                            rope(xs, r_xs, xr, r_xr, n, prm["hd"], t)
                            transposes(xr, r_xr, n, 128, sT, r_sT, t)
                            if sM is not None:
                                k.dve(lambda e: e.tensor_copy(out=sM[:, t, 0:n], in_=xr[:, 0:n]), rd=[r_xr], wr=[r_sM])
                        elif kind == "rope_tm":
                            xs, r_xs = xsr.next()
                            k.act(lambda e: e.activation(out=xs[:, 0:n], in_=ps[:, 0:n], func=AF.Copy, scale=prm["scale"]),
                                  rd=[r_ps], wr=[r_xs])
                            rope(xs, r_xs, sM[:, t, :], r_sM, n, prm["hd"], t)
                        elif kind == "rope64":
                            xs, r_xs = xsr.next()
                            xr, r_xr = xrr.next()
                            k.act(lambda e: e.activation(out=xs[:, 0:n], in_=ps[:, 0:n], func=AF.Copy, scale=prm["scale"]),
                                  rd=[r_ps], wr=[r_xs])
                            rope(xs, r_xs, xr, r_xr, n, 64, t)
                            transposes64(xr, r_xr, n, sT, r_sT, t)
                        elif kind == "ikw":
                            xs, r_xs = xsr.next()
                            xr, r_xr = xrr.next()
                            k.act(lambda e: e.activation(out=xs[:, 0:64], in_=ps[:, 0:64], func=AF.Copy), rd=[r_ps], wr=[r_xs])
                            if prm["iw"]:
                                k.act(lambda e: e.activation(out=iwst[:, t, :], in_=ps[:, 64:80], func=AF.Copy, scale=0.25),
                                      rd=[r_ps], wr=[r_iwst])
                            rope(xs, r_xs, xr, r_xr, 64, 64, t)
                            transposes64(xr, r_xr, 64, sT, r_sT, t)
                    if kind in ("ropeT", "T"):
                        dst = prm["dst"][prm["row0"]:prm["row0"] + n, :].rearrange("(j f) t -> f j t", f=128)
                        k.dma("sync", dst, sT[:, 0:n // 128, :], rd=[r_sT], wr=[R[prm["rn"]]], acc=True)
                        if sM is not None:
                            dst = prm["tm_dst"][:, prm["tm_col0"]:prm["tm_col0"] + n].rearrange("(t p) n -> p t n", p=128)
                            k.dma("sync", dst, sM[:, :, 0:n], rd=[r_sM], wr=[R[prm["tm_rn"]]], acc=True)
                    elif kind == "rope64":
                        dst = prm["dst"][prm["row0"]:prm["row0"] + n, :].rearrange("(j f) t -> f j t", f=64)
                        k.dma("sync", dst, sT[0:64, 0:n // 64, :], rd=[r_sT], wr=[R[prm["rn"]]], acc=True)
                    elif kind == "ikw":
                        if prm["iw"]:
                            k.dma("sync", IW.rearrange("(t p) n -> p t n", p=128), iwst[:], rd=[r_iwst], wr=[R["IW"]], acc=True)
                        else:
                            k.dma("sync", prm["dst"][prm["row0"]:prm["row0"] + 64, :], sT[0:64, 0, :], rd=[r_sT],
                                  wr=[R[prm["rn"]]], acc=True)
                    elif kind == "tm":
                        dst = prm["dst"][prm["row0"]:prm["row0"] + NT, prm["col0"]:prm["col0"] + n].rearrange(
                            "(t p) n -> p t n", p=128)
                        k.dma("sync", dst, sM[:, :, 0:n], rd=[r_sM], wr=[R[prm["rn"]]], acc=True)
                    elif kind == "rope_tm":
                        dst = prm["tm_dst"][prm["tm_row0"]:prm["tm_row0"] + NT, prm["tm_col0"]:prm["tm_col0"] + n].rearrange(
                            "(t p) n -> p t n", p=128)
                        k.dma("sync", dst, sM[:, :, 0:n], rd=[r_sM], wr=[R[prm["tm_rn"]]], acc=True)

            wsh_off = {}
            _o = 0
            for c0_ in ([C_AK, C_AK + 512, C_AV, C_AV + 512, C_IK, C_RK, C_RK + 512] + [C_RV + 512 * i for i in range(4)]):
                wsh_off[c0_] = _o
                _o += 512
            for c0_ in (C_AK, C_AK + 512, C_AV, C_AV + 512):
                wt_, r_w_ = load_w(wring, win_d, KC, c0_, 512)
                k.dma("sync", WSH[:, wsh_off[c0_]:wsh_off[c0_] + 512].rearrange("(c p) n -> p c n", p=128), wt_[:, :, :],
                      rd=[r_w_], wr=[R["WSH"]], acc=True)
            run_groups(groups, hT, r_hT)
            k.barrier()
            if upto == "p2":
                k.barrier()
                return nc
            for r in range(NCORES):
                hT, r_hT = hTr.next()
                csh[0], csh[1] = csr.next()
                compute_cs(csh[0], csh[1], posf_all, r * TT)
                for t in range(TT):
                    norm_T([q.next() for q in nrings], xall_d[r * NT + t * 128:r * NT + (t + 1) * 128, :], [], 0, hT, r_hT,
                           t * 128)
                rgl = []
                for i in range(2):
                    rgl.append((C_AK + 512 * i, 512, "ropeT", dict(hd=128, scale=1.0, dst=KVI_all, rn="KVI_all",
                                                                   row0=r * 2112 + 512 * i)))
                for i in range(2):
                    rgl.append((C_AV + 512 * i, 512, "tm", dict(func=AF.Copy, scale=1.0, dst=KVI_all, rn="KVI_all",
                                                                row0=r * 2112 + 1024, col0=512 * i)))
                rgl.append((C_IK, 64, "ikw", dict(iw=False, dst=KVI_all, rn="KVI_all", row0=r * 2112 + 2048)))
                for i in range(2):
                    rgl.append((C_RK + 512 * i, 512, "rope_tm", dict(hd=128, scale=1.0, tm_dst=RK_all, tm_rn="RK_all",
                                                                     tm_row0=r * NT, tm_col0=512 * i)))
                for i in range(4):
                    rgl.append((C_RV + 512 * i, 512, "tm", dict(func=AF.Copy, scale=1.0, dst=RV_all, rn="RV_all",
                                                                row0=r * NT, col0=512 * i)))
                run_groups(rgl, hT, r_hT, cached=True)
            k.barrier()

        if upto == "p2r":
            return nc
        with ExitStack() as ph:
            iqT = sb(ph, "iqT", [64, 16, NT], BF16)
            ikT = sb(ph, "ikT", [64, S], BF16)
            iw = sb(ph, "iw", [128, TT, 16], F32)
            d4 = sb(ph, "d4", [128, 512], F32)
            cbg = sb(ph, "cbg", [128, 128], F32)
            acc = sb(ph, "acc", [128, S], F32)
            sel = sb(ph, "sel", [128, S], BF16)
            mst = Ring(ph, nc, "mst", [128, 64, 128], BF16, 1)
            rr = Ring(ph, nc, "rr", [128, 512], F32, 3)
            bs = Ring(ph, nc, "bs", [128, 8], F32, 2)
            r_in = Res()
            r_acc = [Res() for _ in range(16)]
            r_sel = Res()
            k.dma("sync", iqT[:], IQT.rearrange("(h d) t -> d h t", d=64), rd=[R["IQT"]], wr=[r_in])
            k.dma("sync", ikT[:].rearrange("d (r t) -> d r t", t=NT),
                  KVI_all.rearrange("(r x) t -> x r t", x=2112)[2048:2112], rd=[R["KVI_all"]], wr=[r_in])
            k.dma("sync", iw[:], IW.rearrange("(t p) n -> p t n", p=128), rd=[R["IW"]], wr=[r_in])
            k.dma("sync", d4[:], d4_d[:, :], wr=[r_in])
            k.dma("sync", cbg[:], cbg_d[:, :], wr=[r_in])
            thr_all = sb(ph, "thr_all", [128, TT], F32)
            r_thr = Res()
            for i in range(TT):
                for g in range(16):
                    a_g = acc[:, g * 512:(g + 1) * 512]
                    k.dve(lambda e: e.tensor_scalar(out=a_g, in0=d4[:], scalar1=cbg[:, i * 16 + g:i * 16 + g + 1],
                                                    scalar2=NEG, op0=ALU.is_gt, op1=ALU.mult),
                          rd=[r_in], wr=[r_acc[g]])
                    for h in range(16):
                        ps, r_ps = psf.next()
                        k.pe(lambda e: e.matmul(ps[:], lhsT=iqT[:, h, i * 128:(i + 1) * 128],
                                                rhs=ikT[:, g * 512:(g + 1) * 512], start=True, stop=True),
                             rd=[r_in], wr=[r_ps])
                        rl, r_rl = rr.next()
                        k.act(lambda e: e.activation(out=rl[:], in_=ps[:], func=AF.Relu), rd=[r_ps], wr=[r_rl])
                        k.dve(lambda e: e.scalar_tensor_tensor(out=a_g, in0=rl[:], scalar=iw[:, i, h:h + 1], in1=a_g,
                                                               op0=ALU.mult, op1=ALU.add),
                              rd=[r_rl, r_in, r_acc[g]], wr=[r_acc[g]])
                if SCORE is not None:
                    k.dma("sync", SCORE[i * 128:(i + 1) * 128, :], acc[:], rd=r_acc, wr=[R["SCORE"]], acc=True)
                b, r_b = bs.next()
                k.dve(lambda e: e.reduce_max(out=b[:, 0:1], in_=acc[:], axis=AX.X), rd=r_acc, wr=[r_b])
                k.dve(lambda e: e.tensor_scalar(out=b[:, 0:1], in0=b[:, 0:1], scalar1=50.0 + 1e-3, scalar2=None,
                                                op0=ALU.add), rd=[r_b], wr=[r_b])
                k.dve(lambda e: e.memset(b[:, 1:2], -50.0), rd=[r_b], wr=[r_b])
                for it in range(NBIS):
                    k.dve(lambda e: e.tensor_scalar(out=b[:, 2:3], in0=b[:, 0:1], scalar1=2.0 ** -(it + 1), scalar2=None,
                                                    op0=ALU.mult), rd=[r_b], wr=[r_b])
                    k.dve(lambda e: e.tensor_tensor(out=b[:, 3:4], in0=b[:, 2:3], in1=b[:, 1:2], op=ALU.add),
                          rd=[r_b], wr=[r_b])
                    k.dve(lambda e: e.tensor_scalar(out=sel[:], in0=acc[:], scalar1=b[:, 3:4], scalar2=None,
                                                    op0=ALU.is_ge, op1=ALU.add, accum_out=b[:, 4:5]),
                          rd=[r_b] + r_acc, wr=[r_sel, r_b])
                    k.dve(lambda e: e.tensor_scalar(out=b[:, 5:6], in0=b[:, 4:5], scalar1=TOPK - 0.5, scalar2=None,
                                                    op0=ALU.is_ge), rd=[r_b], wr=[r_b])
                    k.dve(lambda e: e.scalar_tensor_tensor(out=b[:, 1:2], in0=b[:, 2:3], scalar=b[:, 5:6],
                                                           in1=b[:, 1:2], op0=ALU.mult, op1=ALU.add),
                          rd=[r_b], wr=[r_b])
                k.dve(lambda e: e.tensor_scalar(out=sel[:], in0=acc[:], scalar1=b[:, 1:2], scalar2=None, op0=ALU.is_ge),
                      rd=[r_b] + r_acc, wr=[r_sel])
                k.dve(lambda e: e.tensor_copy(out=thr_all[:, i:i + 1], in_=b[:, 1:2]), rd=[r_b], wr=[r_thr])
                ms, r_ms = mst.next()
                for kb8 in range(8):
                    pb, r_pb = psb.next()
                    for j in range(8):
                        kb = kb8 * 8 + j
                        k.pe(lambda e: e.transpose(out=pb[:, j * 128:(j + 1) * 128], in_=sel[:, kb * 128:(kb + 1) * 128],
                                                   identity=ident[:]), rd=[r_sel, r_const], wr=[r_pb])
                    k.act(lambda e: e.activation(out=ms[:, kb8 * 8:(kb8 + 1) * 8, :],
                                                 in_=pb[:].rearrange("p (j q) -> p j q", q=128), func=AF.Copy),
                          rd=[r_pb], wr=[r_ms])
                k.dma("sync", MT[i], ms[:], rd=[r_ms], wr=[R["MT"]], acc=True)
            k.dma("sync", THR[:, :], thr_all[:], rd=[r_thr], wr=[R["THR"]], acc=True)
            k.barrier()

        if upto == "p4":
            return nc
        with ExitStack() as ph:
            qT = sb(ph, "qT", [128, 8, NT], BF16)
            r_q = Res()
            k.dma("sync", qT[:], QT.rearrange("(h d) t -> d h t", d=128), rd=[R["QT"]], wr=[r_q])
            kTr = Ring(ph, nc, "kT", [128, S], BF16, 2)
            vSr = Ring(ph, nc, "vS", [128, 64, 128], BF16, 2)
            mT = sb(ph, "mT", [128, 4, 64, 128], BF16)
            r_mT = Res()
            er = Ring(ph, nc, "er", [128, 512], BF16, 3)
            pr = Ring(ph, nc, "pr", [128, 512], BF16, 3)
            rdn = Ring(ph, nc, "rdn", [128, 512], F32, 2)
            oaT = sb(ph, "oaT", [128, 8, NT], BF16)
            r_oaT = Res()
            ps_s = [(psf.t[j], psf.r[j]) for j in (0, 1)]
            ps_o = [(psf.t[j], psf.r[j]) for j in (2, 3)]
            ps_d = [(psf.t[j], psf.r[j]) for j in (4, 5)]
            kvx = KVI_all.rearrange("(r x) t -> x r t", x=2112)
            it = 0
            for qc in range(2):
                k.dma("sync", mT[:], MT[qc * 4:(qc + 1) * 4].rearrange("i k b q -> k i b q"), rd=[R["MT"]], wr=[r_mT])
                for h in range(8):
                    kT, r_kT = kTr.next()
                    vS, r_vS = vSr.next()
                    k.dma("sync", kT[:].rearrange("d (r t) -> d r t", t=NT), kvx[h * 128:(h + 1) * 128],
                          rd=[R["KVI_all"]], wr=[r_kT])
                    for r in range(NCORES):
                        k.dma("sync", vS[:, r * 8:(r + 1) * 8, :],
                              KVI_all[r * 2112 + 1024:r * 2112 + 2048, h * 128:(h + 1) * 128].rearrange(
                                  "(t p) d -> p t d", p=128), rd=[R["KVI_all"]], wr=[r_vS], acc=(r > 0))
                    po, r_po = ps_o[h % 2]
                    pd, r_pd = ps_d[h % 2]
                    for kb in range(64):
                        pS, r_pS = ps_s[it % 2]
                        it += 1
                        k.pe(lambda e: e.matmul(pS[:], lhsT=kT[:, kb * 128:(kb + 1) * 128],
                                                rhs=qT[:, h, qc * 512:(qc + 1) * 512], start=True, stop=True),
                             rd=[r_kT, r_q], wr=[r_pS])
                        ee, r_ee = er.next()
                        k.act(lambda e: e.activation(out=ee[:], in_=pS[:], func=AF.Exp), rd=[r_pS], wr=[r_ee])
                        pp, r_pp = pr.next()
                        k.dve(lambda e: e.tensor_tensor(out=pp[:].rearrange("p (i q) -> p i q", q=128),
                                                        in0=ee[:].rearrange("p (i q) -> p i q", q=128),
                                                        in1=mT[:, :, kb, :], op=ALU.mult),
                              rd=[r_ee, r_mT], wr=[r_pp])
                        k.pe(lambda e: e.matmul(po[:], lhsT=vS[:, kb, :], rhs=pp[:], start=(kb == 0), stop=(kb == 63)),
                             rd=[r_vS, r_pp], wr=[r_po])
                        k.pe(lambda e: e.matmul(pd[:], lhsT=ones[:], rhs=pp[:], start=(kb == 0), stop=(kb == 63)),
                             rd=[r_cs, r_pp], wr=[r_pd])
                    rd_, r_rd = rdn.next()
                    k.dve(lambda e: e.reciprocal(out=rd_[:], in_=pd[:]), rd=[r_pd], wr=[r_rd])
                    k.dve(lambda e: e.tensor_tensor(out=oaT[:, h, qc * 512:(qc + 1) * 512], in0=po[:], in1=rd_[:],
                                                    op=ALU.mult), rd=[r_po, r_rd], wr=[r_oaT])
            k.dma("sync", OAT.rearrange("(h d) t -> d h t", d=128), oaT[:], rd=[r_oaT], wr=[R["OAT"]], acc=True)
            k.barrier()

        if upto == "p5":
            return nc
        with ExitStack() as ph:
            dmt = sb(ph, "dmt", [128, 8, 128], F32)
            qdec = sb(ph, "qdec", [128, 8, 128], F32)
            kdec = sb(ph, "kdec", [128, 8], F32)
            dch = sb(ph, "dch", [128, 8], F32)
            coef = sb(ph, "coef", [128, 64], F32)
            Lst = sb(ph, "Lst", [128, 8, 256], F32)
            Sst = sb(ph, "Sst", [128, 8, 256], F32)
            r_ld = Res()
            r_L = [Res() for _ in range(8)]
            r_S = [Res() for _ in range(8)]
            k.dma("sync", dmt[:].rearrange("p h i -> p (h i)"), dmt_d[:, :], wr=[r_ld])
            k.dma("sync", qdec[:].rearrange("p h i -> p (h i)"), qdec_d[:, :], wr=[r_ld])
            k.dma("sync", kdec[:], kdec_d[:, :], wr=[r_ld])
            k.dma("sync", dch[:], dch_d[:, :], wr=[r_ld])
            k.dma("sync", coef[:], coef_d[:, :], wr=[r_ld])

            def kv_update(st, r_st, kk, r_kk, vv, r_vv, h, n, first):
                ps, r_ps = psf.next()
                k.pe(lambda e: e.matmul(ps[:, 0:256], lhsT=kk[:, n, h * 128:(h + 1) * 128],
                                        rhs=vv[:, n, h * 256:(h + 1) * 256], start=True, stop=True),
                     rd=[r_kk, r_vv], wr=[r_ps])
                if first:
                    k.dve(lambda e: e.tensor_copy(out=st[:, h, :], in_=ps[:, 0:256]), rd=[r_ps], wr=[r_st[h]])
                else:
                    k.dve(lambda e: e.scalar_tensor_tensor(out=st[:, h, :], in0=st[:, h, :], scalar=dch[:, h:h + 1],
                                                           in1=ps[:, 0:256], op0=ALU.mult, op1=ALU.add),
                          rd=[r_ps, r_ld, r_st[h]], wr=[r_st[h]])

            with ExitStack() as pa:
                rkr = Ring(pa, nc, "rkA", [128, TT, 1024], BF16, 2)
                rvr = Ring(pa, nc, "rvA", [128, TT, 2048], BF16, 2)
                for c2 in range(NCORES):
                    rka, r_rka = rkr.next()
                    rva, r_rva = rvr.next()
                    k.dma("sync", rka[:], RK_all[c2 * NT:(c2 + 1) * NT, :].rearrange("(t p) n -> p t n", p=128),
                          rd=[R["RK_all"]], wr=[r_rka])
                    k.dma("sync", rva[:], RV_all[c2 * NT:(c2 + 1) * NT, :].rearrange("(t p) n -> p t n", p=128),
                          rd=[R["RV_all"]], wr=[r_rva])
                    for h in range(8):
                        k.dve(lambda e: e.tensor_scalar(out=rka[:, :, h * 128:(h + 1) * 128],
                                                        in0=rka[:, :, h * 128:(h + 1) * 128],
                                                        scalar1=kdec[:, h:h + 1], scalar2=None, op0=ALU.mult),
                              rd=[r_ld, r_rka], wr=[r_rka])
                    for h in range(8):
                        for n in range(TT):
                            kv_update(Lst, r_L, rka, r_rka, rva, r_rva, h, n, n == 0)
                        cf = coef[:, c2 * 8 + h:c2 * 8 + h + 1]
                        if c2 == 0:
                            k.dve(lambda e: e.tensor_scalar(out=Sst[:, h, :], in0=Lst[:, h, :], scalar1=cf, scalar2=None,
                                                            op0=ALU.mult), rd=[r_L[h], r_ld], wr=[r_S[h]])
                        else:
                            k.dve(lambda e: e.scalar_tensor_tensor(out=Sst[:, h, :], in0=Lst[:, h, :], scalar=cf,
                                                                   in1=Sst[:, h, :], op0=ALU.mult, op1=ALU.add),
                                  rd=[r_L[h], r_ld, r_S[h]], wr=[r_S[h]])
                k.barrier()

            rqT = sb(ph, "rqT", [128, 8, NT], BF16)
            rkT = sb(ph, "rkT", [128, 8, NT], BF16)
            rqTd = sb(ph, "rqTd", [128, 8, NT], BF16)
            rks = sb(ph, "rks", [128, TT, 1024], BF16)
            rv = sb(ph, "rv", [128, TT, 2048], BF16)
            Sbf = Ring(ph, nc, "Sbf", [128, 256], BF16, 2)
            rgr = Ring(ph, nc, "rg", [128, 2048], BF16, 2)
            sTr = Ring(ph, nc, "scT", [128, 128], BF16, 3)
            ynr = Ring(ph, nc, "yn", [128, 256], F32, 3)
            sgr = Ring(ph, nc, "sg", [128, 256], F32, 3)
            bnr = Ring(ph, nc, "bn", [128, 16], F32, 3)
            obr = Ring(ph, nc, "ob", [128, 2048], BF16, 2)
            obT = sb(ph, "obT", [128, 16, NT], BF16)
            r_rks = Res()
            r_rv = Res()
            r_rqTd = Res()
            r_obT = Res()
            k.dma("sync", rqT[:], RQT.rearrange("(h d) t -> d h t", d=128), rd=[R["RQT"]], wr=[r_ld])
            k.dma("sync", rkT[:], RKT.rearrange("(h d) t -> d h t", d=128), rd=[R["RKT"]], wr=[r_ld])
            k.dma("sync", rks[:], RK.rearrange("(t p) n -> p t n", p=128), rd=[R["RK"]], wr=[r_rks])
            k.dma("sync", rv[:], RV.rearrange("(t p) n -> p t n", p=128), rd=[R["RV"]], wr=[r_rv])
            for h in range(8):
                k.dve(lambda e: e.tensor_scalar(out=rks[:, :, h * 128:(h + 1) * 128], in0=rks[:, :, h * 128:(h + 1) * 128],
                                                scalar1=kdec[:, h:h + 1], scalar2=None, op0=ALU.mult),
                      rd=[r_ld, r_rks], wr=[r_rks])
                k.dve(lambda e: e.tensor_tensor(out=rqTd[:, h, :].rearrange("p (n i) -> p n i", i=128),
                                                in0=rqT[:, h, :].rearrange("p (n i) -> p n i", i=128),
                                                in1=qdec[:, h, :].unsqueeze(1).to_broadcast([128, TT, 128]),
                                                op=ALU.mult), rd=[r_ld], wr=[r_rqTd])
            for n in range(TT):
                rg, r_rg = rgr.next()
                k.dma("sync", rg[:], RG[n * 128:(n + 1) * 128, :], rd=[R["RG"]], wr=[r_rg])
                ob, r_ob = obr.next()
                for h in range(8):
                    sbf, r_sbf = Sbf.next()
                    k.act(lambda e: e.activation(out=sbf[:], in_=Sst[:, h, :], func=AF.Copy), rd=[r_S[h]], wr=[r_sbf])
                    pS, r_pS = psf.next()
                    k.pe(lambda e: e.matmul(pS[:, 0:128], lhsT=rkT[:, h, n * 128:(n + 1) * 128],
                                            rhs=rqT[:, h, n * 128:(n + 1) * 128], start=True, stop=True),
                         rd=[r_ld], wr=[r_pS])
                    sT_, r_sT_ = sTr.next()
                    k.dve(lambda e: e.tensor_tensor(out=sT_[:], in0=pS[:, 0:128], in1=dmt[:, h, :], op=ALU.mult),
                          rd=[r_pS, r_ld], wr=[r_sT_])
                    pY, r_pY = psf.next()
                    k.pe(lambda e: e.matmul(pY[:, 0:256], lhsT=sT_[:], rhs=rv[:, n, h * 256:(h + 1) * 256],
                                            start=True, stop=False), rd=[r_sT_, r_rv], wr=[r_pY])
                    k.pe(lambda e: e.matmul(pY[:, 0:256], lhsT=rqTd[:, h, n * 128:(n + 1) * 128], rhs=sbf[:],
                                            start=False, stop=True), rd=[r_rqTd, r_sbf], wr=[r_pY])
                    bn, r_bn = bnr.next()
                    k.dve(lambda e: e.bn_stats(out=bn[:, 0:6], in_=pY[:, 0:256]), rd=[r_pY], wr=[r_bn])
                    k.dve(lambda e: e.bn_aggr(out=bn[:, 8:10], in_=bn[:, 0:6]), rd=[r_bn], wr=[r_bn])
                    k.act(lambda e: e.activation(out=bn[:, 10:11], in_=bn[:, 9:10], func=AF.Sqrt, bias=EPS),
                          rd=[r_bn], wr=[r_bn])
                    k.dve(lambda e: e.reciprocal(out=bn[:, 11:12], in_=bn[:, 10:11]), rd=[r_bn], wr=[r_bn])
                    yn, r_yn = ynr.next()
                    k.dve(lambda e: e.tensor_scalar(out=yn[:], in0=pY[:, 0:256], scalar1=bn[:, 8:9], scalar2=bn[:, 11:12],
                                                    op0=ALU.subtract, op1=ALU.mult), rd=[r_pY, r_bn], wr=[r_yn])
                    sg, r_sg = sgr.next()
                    k.act(lambda e: e.activation(out=sg[:], in_=rg[:, h * 256:(h + 1) * 256], func=AF.Silu),
                          rd=[r_rg], wr=[r_sg])
                    k.dve(lambda e: e.tensor_tensor(out=ob[:, h * 256:(h + 1) * 256], in0=yn[:], in1=sg[:], op=ALU.mult),
                          rd=[r_yn, r_sg], wr=[r_ob])
                    if n < TT - 1:
                        kv_update(Sst, r_S, rks, r_rks, rv, r_rv, h, n, False)
                for half in range(2):
                    pb, r_pb = psb.next()
                    for j in range(8):
                        c = half * 8 + j
                        k.pe(lambda e: e.transpose(out=pb[:, j * 128:(j + 1) * 128], in_=ob[:, c * 128:(c + 1) * 128],
                                                   identity=ident[:]), rd=[r_ob, r_const], wr=[r_pb])
                    k.act(lambda e: e.activation(out=obT[:, half * 8:half * 8 + 8, n * 128:(n + 1) * 128],
                                                 in_=pb[:].rearrange("p (j q) -> p j q", q=128), func=AF.Copy),
                          rd=[r_pb], wr=[r_obT])
            k.dma("sync", OBT.rearrange("(c f) t -> f c t", f=128), obT[:], rd=[r_obT], wr=[R["OBT"]], acc=True)
            k.barrier()

        if upto == "p6":
            return nc
        with ExitStack() as ph:
            memT = sb(ph, "memT", [128, KC, 256], BF16)
            r_memT = Res()
            with ExitStack() as p1:
                rings = [Ring(p1, nc, "mxt", [128, D], F32, 2), Ring(p1, nc, "mxb", [128, D], BF16, 2),
                         Ring(p1, nc, "mst_", [128, 4], F32, 2), Ring(p1, nc, "mjk", [128, D], BF16, 1)]
                for t in range(2):
                    norm_T([r.next() for r in rings], mem_d[t * 128:(t + 1) * 128, :], [], 32, memT, r_memT, t * 128)
                k.barrier()
            wmr = Ring(ph, nc, "wm", [128, KC, 512], BF16, 2)
            mkT = sb(ph, "mkT", [128, 8, 256], BF16)
            mv = sb(ph, "mv", [128, 2, 1024], BF16)
            mqT = sb(ph, "mqT", [128, 8, NT], BF16)
            ocT = sb(ph, "ocT", [128, 8, NT], BF16)
            pmr = Ring(ph, nc, "pm", [128, 512], BF16, 4)
            rdn = Ring(ph, nc, "rdm", [128, 512], F32, 2)
            r_mk = Res()
            r_mv = Res()
            r_mq = Res()
            r_oc = Res()
            k.dma("sync", mqT[:], MQT.rearrange("(c f) t -> f c t", f=128), rd=[R["MQT"]], wr=[r_mq])
            for g in range(2):
                wt, r_w = load_w(wmr, wmk_d, KC, g * 512, 512)
                for j in range(4):
                    ps, r_ps = psf.next()
                    for c in range(KC):
                        k.pe(lambda e: e.matmul(ps[:, 0:256], lhsT=wt[:, c, j * 128:(j + 1) * 128], rhs=memT[:, c, :],
                                                start=(c == 0), stop=(c == KC - 1)), rd=[r_w, r_memT], wr=[r_ps])
                    k.act(lambda e: e.activation(out=mkT[:, g * 4 + j, :], in_=ps[:, 0:256], func=AF.Copy),
                          rd=[r_ps], wr=[r_mk])
            for g in range(2):
                wt, r_w = load_w(wmr, wmk_d, KC, 1024 + g * 512, 512)
                for mt in range(2):
                    ps, r_ps = psf.next()
                    for c in range(KC):
                        k.pe(lambda e: e.matmul(ps[:], lhsT=memT[:, c, mt * 128:(mt + 1) * 128], rhs=wt[:, c, :],
                                                start=(c == 0), stop=(c == KC - 1)), rd=[r_w, r_memT], wr=[r_ps])
                    k.act(lambda e: e.activation(out=mv[:, mt, g * 512:(g + 1) * 512], in_=ps[:], func=AF.Copy),
                          rd=[r_ps], wr=[r_mv])
            for hm in range(4):
                for qc in range(2):
                    pms = []
                    for mt in range(2):
                        ps, r_ps = psf.next()
                        for dc in range(2):
                            k.pe(lambda e: e.matmul(ps[:], lhsT=mkT[:, hm * 2 + dc, mt * 128:(mt + 1) * 128],
                                                    rhs=mqT[:, hm * 2 + dc, qc * 512:(qc + 1) * 512],
                                                    start=(dc == 0), stop=(dc == 1)), rd=[r_mk, r_mq], wr=[r_ps])
                        pm, r_pm = pmr.next()
                        k.act(lambda e: e.activation(out=pm[:], in_=ps[:], func=AF.Exp), rd=[r_ps], wr=[r_pm])
                        pms.append((pm, r_pm))
                    pd, r_pd = psf.next()
                    for mt in range(2):
                        k.pe(lambda e: e.matmul(pd[:], lhsT=ones[:], rhs=pms[mt][0][:], start=(mt == 0), stop=(mt == 1)),
                             rd=[r_cs, pms[mt][1]], wr=[r_pd])
                    rd_, r_rd = rdn.next()
                    k.dve(lambda e: e.reciprocal(out=rd_[:], in_=pd[:]), rd=[r_pd], wr=[r_rd])
                    for ec in range(2):
                        po, r_po = psf.next()
                        for mt in range(2):
                            k.pe(lambda e: e.matmul(po[:], lhsT=mv[:, mt, hm * 256 + ec * 128:hm * 256 + (ec + 1) * 128],
                                                    rhs=pms[mt][0][:], start=(mt == 0), stop=(mt == 1)),
                                 rd=[r_mv, pms[mt][1]], wr=[r_po])
                        k.dve(lambda e: e.tensor_tensor(out=ocT[:, hm * 2 + ec, qc * 512:(qc + 1) * 512], in0=po[:],
                                                        in1=rd_[:], op=ALU.mult), rd=[r_po, r_rd], wr=[r_oc])
            k.dma("sync", OCT.rearrange("(c f) t -> f c t", f=128), ocT[:], rd=[r_oc], wr=[R["OCT"]], acc=True)
            k.barrier()

        if upto == "p7":
            return nc
        with ExitStack() as ph:
            oT = sb(ph, "oT", [128, 32, NT], BF16)
            r_oT = Res()
            k.dma("sync", oT[:, 0:8, :], OAT.rearrange("(c f) t -> f c t", f=128), rd=[R["OAT"]], wr=[r_oT])
            k.dma("sync", oT[:, 8:24, :], OBT.rearrange("(c f) t -> f c t", f=128), rd=[R["OBT"]], wr=[r_oT])
            k.dma("sync", oT[:, 24:32, :], OCT.rearrange("(c f) t -> f c t", f=128), rd=[R["OCT"]], wr=[r_oT])
            wbr = Ring(ph, nc, "wbr", [128, 32, 512], BF16, 2)
            gtr = Ring(ph, nc, "gt", [128, TT, 3, 512], BF16, 1)
            tr = Ring(ph, nc, "tm3", [128, 512], F32, 4)
            mxr = Ring(ph, nc, "mx", [128, TT, 512], BF16, 2)
            branches = ((0, 8, wa_d), (8, 16, wb_d), (24, 8, wc_d))
            for cg in range(4):
                wt, r_w = wbr.next()
                for (c0, nk, wsrc) in branches:
                    for a in range(0, nk, 8):
                        k.dma("gpsimd", wt[:, c0 + a:c0 + a + 8, :],
                              wsrc[a * 128:(a + 8) * 128, cg * 512:(cg + 1) * 512].rearrange("(c p) n -> p c n", p=128),
                              wr=[r_w], acc=not (c0 == 0 and a == 0))
                gt, r_gt = gtr.next()
                for b in range(3):
                    k.dma("sync", gt[:, :, b, :],
                          GATES[:, b * D + cg * 512:b * D + (cg + 1) * 512].rearrange("(t p) n -> p t n", p=128),
                          rd=[R["GATES"]], wr=[r_gt], acc=(b > 0))
                mx, r_mx = mxr.next()
                for t in range(TT):
                    tmps = []
                    for b, (c0, nk, wsrc) in enumerate(branches):
                        ps, r_ps = psf.next()
                        for c in range(nk):
                            k.pe(lambda e: e.matmul(ps[:], lhsT=oT[:, c0 + c, t * 128:(t + 1) * 128], rhs=wt[:, c0 + c, :],
                                                    start=(c == 0), stop=(c == nk - 1)), rd=[r_oT, r_w], wr=[r_ps])
                        tm_, r_tm = tr.next()
                        k.dve(lambda e: e.tensor_tensor(out=tm_[:], in0=ps[:], in1=gt[:, t, b, :], op=ALU.mult),
                              rd=[r_ps, r_gt], wr=[r_tm])
                        tmps.append((tm_, r_tm))
                    k.dve(lambda e: e.tensor_tensor(out=tmps[0][0][:], in0=tmps[0][0][:], in1=tmps[1][0][:], op=ALU.add),
                          rd=[tmps[0][1], tmps[1][1]], wr=[tmps[0][1]])
                    k.dve(lambda e: e.tensor_tensor(out=mx[:, t, :], in0=tmps[0][0][:], in1=tmps[2][0][:], op=ALU.add),
                          rd=[tmps[0][1], tmps[2][1]], wr=[r_mx])
                k.dma("sync", MIX[:, cg * 512:(cg + 1) * 512].rearrange("(t p) n -> p t n", p=128), mx[:],
                      rd=[r_mx], wr=[R["MIX"]], acc=True)
            k.barrier()

        if upto == "p8a":
            return nc
        with ExitStack() as ph0:
            h2T = sb(ph0, "h2T", [128, KC, NT], BF16)
            r_h2T = Res()
            gpm = sb(ph0, "gpm", [128, D], F32)
            r_g = Res()
            k.dma("sync", gpm[:], gpm_d[:, :], wr=[r_g])

            def post_norm_residual(z, r_z, gp, resid, r_resid, outt, r_out, stt, r_stt, jk, r_jk):
                k.act(lambda e: e.activation(out=jk[:], in_=z[:], func=AF.Square, accum_out=stt[:, 0:1]),
                      rd=[r_z], wr=[r_jk, r_stt])
                k.act(lambda e: e.activation(out=stt[:, 1:2], in_=stt[:, 0:1], func=AF.Sqrt, scale=1.0 / D, bias=EPS),
                      rd=[r_stt], wr=[r_stt])
                k.dve(lambda e: e.reciprocal(out=stt[:, 2:3], in_=stt[:, 1:2]), rd=[r_stt], wr=[r_stt])
                k.dve(lambda e: e.scalar_tensor_tensor(out=z[:], in0=z[:], scalar=stt[:, 2:3], in1=gp[:],
                                                       op0=ALU.mult, op1=ALU.mult), rd=[r_z, r_stt, r_g], wr=[r_z])
                k.dve(lambda e: e.tensor_tensor(out=outt[:], in0=z[:], in1=resid[:], op=ALU.add),
                      rd=[r_z, r_resid], wr=[r_out])

            with ExitStack() as ph:
                wo = sb(ph, "wo", [128, KC, D], BF16)
                r_wo = Res()
                for a in range(0, KC, 2):
                    k.dma("gpsimd", wo[:, a:a + 2, :], wo_d[a * 128:(a + 2) * 128, :].rearrange("(c p) n -> p c n", p=128),
                          wr=[r_wo], acc=(a > 0))
                mixT = sb(ph, "mixT", [128, KC, NT], BF16)
                r_mixT = Res()
                mtr = Ring(ph, nc, "mtile", [128, D], BF16, 1)
                for t in range(TT):
                    mt_, r_mt = mtr.next()
                    k.dma("sync", mt_[:], MIX[t * 128:(t + 1) * 128, :], rd=[R["MIX"]], wr=[r_mt])
                    for half in range(2):
                        pb, r_pb = psb.next()
                        for j in range(8):
                            c = half * 8 + j
                            k.pe(lambda e: e.transpose(out=pb[:, j * 128:(j + 1) * 128], in_=mt_[:, c * 128:(c + 1) * 128],
                                                       identity=ident[:]), rd=[r_mt, r_const], wr=[r_pb])
                        k.act(lambda e: e.activation(out=mixT[:, half * 8:half * 8 + 8, t * 128:(t + 1) * 128],
                                                     in_=pb[:].rearrange("p (j q) -> p j q", q=128), func=AF.Copy),
                              rd=[r_pb], wr=[r_mixT])
                zr = Ring(ph, nc, "z", [128, D], F32, 2)
                xr_ = Ring(ph, nc, "xres", [128, D], F32, 1)
                x1r = Ring(ph, nc, "x1", [128, D], F32, 2)
                str_ = Ring(ph, nc, "pst", [128, 4], F32, 2)
                jkr = Ring(ph, nc, "pjk", [128, D], BF16, 1)
                hbr = Ring(ph, nc, "hb", [128, D], BF16, 1)
                for t in range(TT):
                    z, r_z = zr.next()
                    for cg in range(4):
                        ps, r_ps = psf.next()
                        for c in range(KC):
                            k.pe(lambda e: e.matmul(ps[:], lhsT=mixT[:, c, t * 128:(t + 1) * 128],
                                                    rhs=wo[:, c, cg * 512:(cg + 1) * 512],
                                                    start=(c == 0), stop=(c == KC - 1)), rd=[r_mixT, r_wo], wr=[r_ps])
                        k.act(lambda e: e.activation(out=z[:, cg * 512:(cg + 1) * 512], in_=ps[:], func=AF.Copy),
                              rd=[r_ps], wr=[r_z])
                    xres, r_xres = xr_.next()
                    k.dma("sync", xres[:], x_d[t * 128:(t + 1) * 128, :], wr=[r_xres])
                    x1, r_x1 = x1r.next()
                    stt, r_stt = str_.next()
                    jk, r_jk = jkr.next()
                    post_norm_residual(z, r_z, gpm, xres, r_xres, x1, r_x1, stt, r_stt, jk, r_jk)
                    k.dma("sync", X1[t * 128:(t + 1) * 128, :], x1[:], rd=[r_x1], wr=[R["X1"]], acc=True)
                    stt2, r_stt2 = str_.next()
                    hb, r_hb = hbr.next()
                    k.act(lambda e: e.activation(out=jk[:], in_=x1[:], func=AF.Square, accum_out=stt2[:, 0:1]),
                          rd=[r_x1], wr=[r_jk, r_stt2])
                    k.act(lambda e: e.activation(out=stt2[:, 1:2], in_=stt2[:, 0:1], func=AF.Sqrt, scale=1.0 / D, bias=EPS),
                          rd=[r_stt2], wr=[r_stt2])
                    k.dve(lambda e: e.reciprocal(out=stt2[:, 2:3], in_=stt2[:, 1:2]), rd=[r_stt2], wr=[r_stt2])
                    k.dve(lambda e: e.tensor_scalar(out=hb[:], in0=x1[:], scalar1=stt2[:, 2:3], scalar2=None, op0=ALU.mult),
                          rd=[r_x1, r_stt2], wr=[r_hb])
                    for half in range(2):
                        pb, r_pb = psb.next()
                        for j in range(8):
                            c = half * 8 + j
                            k.pe(lambda e: e.transpose(out=pb[:, j * 128:(j + 1) * 128], in_=hb[:, c * 128:(c + 1) * 128],
                                                       identity=ident[:]), rd=[r_hb, r_const], wr=[r_pb])
                        gb = gfm[:, 16 + half * 8:16 + half * 8 + 8].unsqueeze(2).to_broadcast([128, 8, 128])
                        k.dve(lambda e: e.tensor_tensor(out=h2T[:, half * 8:half * 8 + 8, t * 128:(t + 1) * 128],
                                                        in0=pb[:].rearrange("p (j q) -> p j q", q=128), in1=gb,
                                                        op=ALU.mult), rd=[r_pb, r_const], wr=[r_h2T])
                k.barrier()

            if upto == "p8b":
                return nc
            with ExitStack() as ph:
                actT = sb(ph, "actT", [128, 44, NT], BF16)
                r_act = Res()
                with ExitStack() as p9:
                    wgr = Ring(p9, nc, "wg", [128, KC, 256], BF16, 2)
                    wur = Ring(p9, nc, "wu", [128, KC, 256], BF16, 2)
                    sgr = Ring(p9, nc, "fsg", [128, 512], F32, 3)
                    pend = []
                    nxt = 0

                    def pre9():
                        nonlocal nxt
                        while nxt < 22 and len(pend) < 1:
                            a = load_w(wgr, wfi_d, KC, nxt * 256, 256, nsplit=2)
                            b = load_w(wur, wfi_d, KC, DFF + nxt * 256, 256, nsplit=2)
                            pend.append((a, b))
                            nxt += 1
                    pre9()
                    for jj in range(22):
                        (wg, r_wg), (wu, r_wu) = pend.pop(0)
                        pre9()
                        for a in range(2):
                            j = jj * 2 + a
                            for b in range(2):
                                pg, r_pg = psf.next()
                                pu, r_pu = psf.next()
                                for c in range(KC):
                                    k.pe(lambda e: e.matmul(pg[:], lhsT=wg[:, c, a * 128:(a + 1) * 128],
                                                            rhs=h2T[:, c, b * 512:(b + 1) * 512],
                                                            start=(c == 0), stop=(c == KC - 1)), rd=[r_wg, r_h2T], wr=[r_pg])
                                for c in range(KC):
                                    k.pe(lambda e: e.matmul(pu[:], lhsT=wu[:, c, a * 128:(a + 1) * 128],
                                                            rhs=h2T[:, c, b * 512:(b + 1) * 512],
                                                            start=(c == 0), stop=(c == KC - 1)), rd=[r_wu, r_h2T], wr=[r_pu])
                                sg, r_sg = sgr.next()
                                k.act(lambda e: e.activation(out=sg[:], in_=pg[:], func=AF.Silu), rd=[r_pg], wr=[r_sg])
                                k.dve(lambda e: e.tensor_tensor(out=actT[:, j, b * 512:(b + 1) * 512], in0=sg[:], in1=pu[:],
                                                                op=ALU.mult), rd=[r_sg, r_pu], wr=[r_act])
                    k.barrier()
                with ExitStack() as p10:
                    wfr = Ring(p10, nc, "wf", [128, 44, 256], BF16, 2)
                    yst = Ring(p10, nc, "yst", [128, TT, 256], F32, 2)
                    pend = []
                    nxt = 0

                    def pre10():
                        nonlocal nxt
                        while nxt < 8 and len(pend) < 1:
                            pend.append(load_w(wfr, wfo_d, 44, nxt * 256, 256, nsplit=4))
                            nxt += 1
                    pre10()
                    for cg in range(8):
                        wf, r_wf = pend.pop(0)
                        pre10()
                        ys, r_ys = yst.next()
                        for t in range(TT):
                            ps, r_ps = psf.next()
                            for j in range(44):
                                k.pe(lambda e: e.matmul(ps[:, 0:256], lhsT=actT[:, j, t * 128:(t + 1) * 128], rhs=wf[:, j, :],
                                                        start=(j == 0), stop=(j == 43)), rd=[r_act, r_wf], wr=[r_ps])
                            k.act(lambda e: e.activation(out=ys[:, t, :], in_=ps[:, 0:256], func=AF.Copy),
                                  rd=[r_ps], wr=[r_ys])
                        k.dma("sync", Y2[:, cg * 256:(cg + 1) * 256].rearrange("(t p) n -> p t n", p=128), ys[:],
                              rd=[r_ys], wr=[R["Y2"]], acc=True)
                    k.barrier()

            with ExitStack() as ph:
                gpf = sb(ph, "gpf", [128, D], F32)
                k.dma("sync", gpf[:], gpf_d[:, :], wr=[r_g])
                zr = Ring(ph, nc, "fz", [128, D], F32, 2)
                xr_ = Ring(ph, nc, "fx1", [128, D], F32, 2)
                orr = Ring(ph, nc, "fo", [128, D], F32, 2)
                str_ = Ring(ph, nc, "fst", [128, 4], F32, 2)
                jkr = Ring(ph, nc, "fjk", [128, D], BF16, 1)
                for t in range(TT):
                    z, r_z = zr.next()
                    x1, r_x1 = xr_.next()
                    o, r_o = orr.next()
                    stt, r_stt = str_.next()
                    jk, r_jk = jkr.next()
                    k.dma("sync", z[:], Y2[t * 128:(t + 1) * 128, :], rd=[R["Y2"]], wr=[r_z])
                    k.dma("sync", x1[:], X1[t * 128:(t + 1) * 128, :], rd=[R["X1"]], wr=[r_x1])
                    post_norm_residual(z, r_z, gpf, x1, r_x1, o, r_o, stt, r_stt, jk, r_jk)
                    k.dma("sync", out_d[t * 128:(t + 1) * 128, :], o[:], rd=[r_o], wr=[R["out"]], acc=True)
                k.barrier()
    return nc


_NC_CACHE = {}


def _consts():
    ident = np.eye(128, dtype=np.float32).astype(ml_dtypes.bfloat16)
    fr = (10000.0 ** (-np.arange(0, 128, 2, dtype=np.float32) / np.float32(128))).astype(np.float32)
    freqs = np.broadcast_to(fr[None, :], (128, 64)).copy()
    q = np.arange(128)[:, None]
    sidx = np.arange(512)[None, :]
    d4 = (sidx - q).astype(np.float32)
    H = 8
    log_gamma = np.log1p(-np.exp2(-5.0 - np.arange(H, dtype=np.float64)))
    i = np.arange(128, dtype=np.float64)
    diff = i[None, :] - i[:, None]
    dmt = np.where(diff[:, None, :] >= 0, np.exp(np.maximum(diff, 0)[:, None, :] * log_gamma[None, :, None]), 0.0) * 128 ** -0.5
    qdec = np.exp((i + 1)[None, None, :] * log_gamma[None, :, None]) * np.ones((128, 1, 1))
    kdec = np.exp((127 - i)[:, None] * log_gamma[None, :]) * 128 ** -0.5
    dch = np.exp(128 * log_gamma)[None, :] * np.ones((128, 1))
    return dict(ident=ident, freqs=freqs, d4=d4, dmt=dmt.reshape(128, 1024).astype(np.float32),
                qdec=qdec.reshape(128, 1024).astype(np.float32), kdec=kdec.astype(np.float32),
                dch=dch.astype(np.float32)), log_gamma


def make_in_maps(inputs):
    c, log_gamma = _consts()
    x = np.asarray(inputs["x"], dtype=np.float32)[0]
    mem = np.ascontiguousarray(np.asarray(inputs["mem"], dtype=np.float32)[0])
    pos = np.asarray(inputs["positions"]).astype(np.int32)[0]

    def fm(g):
        return np.asarray(g, dtype=np.float32)[0].reshape(16, 128).T
    gfm = np.ascontiguousarray(np.concatenate([fm(inputs["g_pre_mix"]), fm(inputs["g_pre_ffn"]), fm(inputs["g_mem"])], axis=1))
    gpm = np.ascontiguousarray(np.broadcast_to(np.asarray(inputs["g_post_mix"], dtype=np.float32)[0][None, :], (128, D)))
    gpf = np.ascontiguousarray(np.broadcast_to(np.asarray(inputs["g_post_ffn"], dtype=np.float32)[0][None, :], (128, D)))
    shared = dict(mem=mem, gfm=gfm, gpm=gpm, gpf=gpf, x_all=np.ascontiguousarray(x),
                  pos_all=np.ascontiguousarray(pos.reshape(NCORES * TT, 128).T),
                  w_in=np.ascontiguousarray(np.asarray(inputs["w_in"], dtype=np.float32)[0]),
                  w_mem_kv=np.ascontiguousarray(np.asarray(inputs["w_mem_kv"], dtype=np.float32)[0]),
                  w_a=np.ascontiguousarray(np.asarray(inputs["w_branch_a"], dtype=np.float32)[0]),
                  w_b=np.ascontiguousarray(np.asarray(inputs["w_branch_b"], dtype=np.float32)[0]),
                  w_c=np.ascontiguousarray(np.asarray(inputs["w_branch_c"], dtype=np.float32)[0]),
                  w_out=np.ascontiguousarray(np.asarray(inputs["w_out"], dtype=np.float32)[0]),
                  w_fi=np.ascontiguousarray(np.asarray(inputs["w_ffn_in"], dtype=np.float32)[0]),
                  w_fo=np.ascontiguousarray(np.asarray(inputs["w_ffn_out"], dtype=np.float32)[0]),
                  **c)
    maps = []
    for core in range(NCORES):
        m = dict(shared)
        m["x"] = np.ascontiguousarray(x[core * NT:(core + 1) * NT])
        m["pos"] = np.ascontiguousarray(pos[core * NT:(core + 1) * NT].reshape(TT, 128).T)
        cbg = np.zeros((TT, 16), np.float32)
        for i in range(TT):
            for g in range(16):
                cbg[i, g] = 128.0 * ((core * TT + i) - 4 * g)
        m["cbg"] = np.ascontiguousarray(np.broadcast_to(cbg.reshape(1, 128), (128, 128)))
        coef = np.zeros((NCORES, 8), np.float64)
        for c2 in range(NCORES):
            if c2 < core:
                coef[c2] = np.exp(128.0 * TT * (core - 1 - c2) * log_gamma)
        m["coef"] = np.ascontiguousarray(np.broadcast_to(coef.reshape(1, 64).astype(np.float32), (128, 64)))
        maps.append(m)
    return maps


def kernel(**inputs):
    if "nc" not in _NC_CACHE:
        _NC_CACHE["nc"] = build(False)
    nc = _NC_CACHE["nc"]
    maps = make_in_maps(inputs)
    res = run_bass_kernel_spmd(nc, maps, core_ids=list(range(NCORES)))
    out = np.concatenate([np.asarray(r["out"], dtype=np.float32) for r in res.results], axis=0)
    return out.reshape(1, S, D)
```

```python
import math
from contextlib import ExitStack

import numpy as np
import ml_dtypes
import concourse.bass as bass
import concourse.mybir as mybir
from concourse.bass_utils import run_bass_kernel_spmd

F32 = mybir.dt.float32
BF16 = mybir.dt.bfloat16
I32 = mybir.dt.int32
ALU = mybir.AluOpType
AF = mybir.ActivationFunctionType
AX = mybir.AxisListType

NCORES = 8
S = 8192
D = 2048
NT = S // NCORES
TT = NT // 128
KC = D // 128
IN_COLS = 17488
DFF = 5632
EPS = 1e-6
NEG = -30000.0
NBIS = 13
BW0 = 12.0
TOPK = 256

C_AQ, C_AK, C_AV, C_IQ, C_IK, C_IW, C_RQ, C_RK, C_RV, C_RG, C_MQ, C_GT = (
    0, 1024, 2048, 3072, 4096, 4160, 4176, 5200, 6224, 8272, 10320, 11344)


class Res:
    __slots__ = ("w", "rd")

    def __init__(self):
        self.w = {}
        self.rd = {}


class Issuer:
    def __init__(self, eng):
        self.eng = eng
        self.waited = {}


class KB:
    def __init__(self, nc, es):
        self.nc = nc
        self.iss = {n: Issuer(getattr(nc, n)) for n in ("sync", "scalar", "vector", "gpsimd", "tensor")}
        self.csem = {n: es.enter_context(nc.semaphore("c_" + n)) for n in ("scalar", "vector", "gpsimd", "tensor")}
        self.ccount = {n: 0 for n in self.csem}
        self.dsems = {}
        self.dcount = {}
        self.dnext = {}
        for q, n in (("sync", 16), ("gpsimd", 8), ("scalar", 4)):
            self.dsems[q] = [es.enter_context(nc.semaphore("d_%s%d" % (q, i))) for i in range(n)]
            self.dnext[q] = 0
            for s in self.dsems[q]:
                self.dcount[s] = 0

    def _wait(self, iss, need):
        for s, v in need.items():
            if iss.waited.get(s, 0) >= v:
                continue
            iss.eng.wait_ge(s, v)
            iss.waited[s] = v

    def _deps(self, rd, wr, own, raw_same, acc=False):
        need = {}

        def add(h, same_ok):
            s, v = h
            if s is own and not same_ok:
                return
            if need.get(s, 0) < v:
                need[s] = v
        for r in rd:
            for h in r.w.items():
                add(h, raw_same)
        for r in wr:
            if not acc:
                for h in r.w.items():
                    add(h, False)
            for h in r.rd.values():
                add(h, False)
        return need

    def op(self, en, fn, rd=(), wr=()):
        iss = self.iss[en]
        sem = self.csem[en]
        need = self._deps(rd, wr, sem, en != "tensor")
        self._wait(iss, need)
        ins = fn(iss.eng)
        self.ccount[en] += 1
        ins.then_inc(sem, 1)
        h = (sem, self.ccount[en])
        for r in rd:
            r.rd[sem] = h
        for r in wr:
            r.w = {h[0]: h[1]}
            r.rd = {}
        return ins

    def act(self, fn, rd=(), wr=()):
        return self.op("scalar", fn, rd, wr)

    def dve(self, fn, rd=(), wr=()):
        return self.op("vector", fn, rd, wr)

    def pe(self, fn, rd=(), wr=()):
        return self.op("tensor", fn, rd, wr)

    def dma(self, q, out, in_, rd=(), wr=(), acc=False, **kw):
        iss = self.iss[q]
        sems = self.dsems[q]
        s = sems[self.dnext[q] % len(sems)]
        self.dnext[q] += 1
        need = self._deps(rd, wr, None, True, acc)
        if self.dcount[s] > 0 and need.get(s, 0) < self.dcount[s]:
            need[s] = self.dcount[s]
        self._wait(iss, need)
        ins = iss.eng.dma_start(out=out, in_=in_, **kw)
        ins.then_inc(s, 16)
        self.dcount[s] += 16
        h = (s, self.dcount[s])
        for r in rd:
            r.rd[s] = h
        for r in wr:
            if acc:
                r.w[s] = h[1]
            else:
                r.w = {s: h[1]}
                r.rd = {}
        return ins

    def collective(self, kind, ins_, outs_, rd=(), wr=()):
        q = "gpsimd"
        iss = self.iss[q]
        sems = self.dsems[q]
        s = sems[self.dnext[q] % len(sems)]
        self.dnext[q] += 1
        need = self._deps(rd, wr, None, True)
        if self.dcount[s] > 0 and need.get(s, 0) < self.dcount[s]:
            need[s] = self.dcount[s]
        self._wait(iss, need)
        ins = iss.eng.collective_compute(kind, ALU.bypass, replica_groups=[list(range(NCORES))],
                                         ins=ins_, outs=outs_)
        ins.then_inc(s, 16)
        self.dcount[s] += 16
        h = (s, self.dcount[s])
        for r in rd:
            r.rd[s] = h
        for r in wr:
            r.w = {s: h[1]}
            r.rd = {}
        return ins

    def barrier(self):
        need = {}
        for n, sem in self.csem.items():
            if self.ccount[n] > 0:
                need[sem] = self.ccount[n]
        for s, v in self.dcount.items():
            if v > 0:
                need[s] = v
        for n, iss in self.iss.items():
            self._wait(iss, dict(need))


_UID = [0]


class Ring:
    def __init__(self, es, nc, name, shape, dt, n, psum=False):
        self.t = []
        self.r = []
        _UID[0] += 1
        name = "r%d_%s" % (_UID[0], name)
        for i in range(n):
            if psum:
                t = es.enter_context(nc.psum_tensor("%s%d" % (name, i), shape, dt))
            else:
                t = es.enter_context(nc.sbuf_tensor("%s%d" % (name, i), shape, dt))
            self.t.append(t)
            self.r.append(Res())
        self.i = 0

    def next(self):
        j = self.i % len(self.t)
        self.i += 1
        return self.t[j], self.r[j]


def build(debug=False, upto=None):
    nc = bass.Bass("TRN2", target_bir_lowering=False)
    okind = "ExternalOutput" if debug else "Internal"

    def din(name, shape, dt):
        return nc.dram_tensor(name, shape, dt, kind="ExternalInput").ap()

    def dscr(name, shape, dt, dbg=True):
        return nc.dram_tensor(name, shape, dt, kind=(okind if dbg else "Internal")).ap()

    x_d = din("x", [NT, D], F32)
    xall_d = din("x_all", [S, D], F32)
    pos_d = din("pos", [128, TT], I32)
    posall_d = din("pos_all", [128, NCORES * TT], I32)
    mem_d = din("mem", [256, D], F32)
    gfm_d = din("gfm", [128, 48], F32)
    gpm_d = din("gpm", [128, D], F32)
    gpf_d = din("gpf", [128, D], F32)
    win_d = din("w_in", [D, IN_COLS], F32)
    wmk_d = din("w_mem_kv", [D, 2048], F32)
    wa_d = din("w_a", [1024, D], F32)
    wb_d = din("w_b", [2048, D], F32)
    wc_d = din("w_c", [1024, D], F32)
    wo_d = din("w_out", [D, D], F32)
    wfi_d = din("w_fi", [D, 2 * DFF], F32)
    wfo_d = din("w_fo", [DFF, D], F32)
    ident_d = din("ident", [128, 128], BF16)
    freqs_d = din("freqs", [128, 64], F32)
    d4_d = din("d4", [128, 512], F32)
    cbg_d = din("cbg", [128, 128], F32)
    dmt_d = din("dmt", [128, 1024], F32)
    qdec_d = din("qdec", [128, 1024], F32)
    kdec_d = din("kdec", [128, 8], F32)
    dch_d = din("dch", [128, 8], F32)
    coef_d = din("coef", [128, 64], F32)
    out_d = nc.dram_tensor("out", [NT, D], F32, kind="ExternalOutput").ap()

    QT = dscr("QT", [1024, NT], BF16)
    KVI_loc = dscr("KVI_loc", [2112, NT], BF16, dbg=False)
    KVI_dbg = dscr("KVI_dbg", [2112, NT], BF16) if debug else None
    KVI_all = dscr("KVI_all", [NCORES * 2112, NT], BF16)
    RK_all = dscr("RK_all", [S, 1024], BF16, dbg=False)
    RV_all = dscr("RV_all", [S, 2048], BF16, dbg=False)
    IQT = dscr("IQT", [1024, NT], BF16)
    IW = dscr("IW", [NT, 16], F32)
    RQT = dscr("RQT", [1024, NT], BF16)
    RKT = dscr("RKT", [1024, NT], BF16)
    RK = dscr("RK", [NT, 1024], BF16)
    RV = dscr("RV", [NT, 2048], BF16)
    RG = dscr("RG", [NT, 2048], BF16)
    MQT = dscr("MQT", [1024, NT], BF16)
    GATES = dscr("GATES", [NT, 3 * D], BF16)
    MT = dscr("MT", [TT, 128, 64, 128], BF16, dbg=False)
    SCORE = dscr("SCORE", [NT, S], F32) if debug else None
    THR = dscr("THR", [128, TT], F32)
    LST_loc = dscr("LST_loc", [1024, 256], F32, dbg=False)
    LST_all = dscr("LST_all", [NCORES * 1024, 256], F32, dbg=False)
    OAT = dscr("OAT", [1024, NT], BF16)
    OBT = dscr("OBT", [2048, NT], BF16)
    OCT = dscr("OCT", [1024, NT], BF16)
    MIX = dscr("MIX", [NT, D], BF16)
    X1 = dscr("X1", [NT, D], F32)
    WSH = dscr("WSH", [D, 5632], BF16, dbg=False)
    Y2 = dscr("Y2", [NT, D], F32)

    R = {}
    for nme in ("QT", "KVI_loc", "KVI_all", "IQT", "IW", "RQT", "RKT", "RK", "RV", "RG", "MQT", "GATES", "MT",
                "SCORE", "THR", "LST_loc", "LST_all", "RK_all", "RV_all", "WSH", "OAT", "OBT", "OCT", "MIX", "X1", "Y2", "out"):
        R[nme] = Res()

    es = ExitStack()
    with es:
        k = KB(nc, es)

        def sb(st, name, shape, dt):
            _UID[0] += 1
            return st.enter_context(nc.sbuf_tensor("s%d_%s" % (_UID[0], name), shape, dt))

        ident = sb(es, "ident", [128, 128], BF16)
        freqs = sb(es, "freqs", [128, 64], F32)
        gfm = sb(es, "gfm", [128, 48], F32)
        posi = sb(es, "posi", [128, TT], I32)
        posf = sb(es, "posf", [128, TT], F32)
        ones = sb(es, "ones", [128, 128], BF16)
        r_const = Res()
        r_cs = Res()
        k.dma("sync", ident[:], ident_d[:, :], wr=[r_const])
        k.dma("sync", freqs[:], freqs_d[:, :], wr=[r_const])
        k.dma("sync", gfm[:], gfm_d[:, :], wr=[r_const])
        k.dma("sync", posi[:], pos_d[:, :], wr=[r_const])
        k.dve(lambda e: e.tensor_copy(out=posf[:], in_=posi[:]), rd=[r_const], wr=[r_cs])
        k.dve(lambda e: e.memset(ones[:], 1.0), wr=[r_cs])

        psf = Ring(es, nc, "psf", [128, 512], F32, 6, psum=True)
        psb = Ring(es, nc, "psb", [128, 1024], BF16, 2, psum=True)

        ang = sb(es, "ang", [128, 64], F32)
        u = sb(es, "u", [128, 64], F32)
        ki = sb(es, "ki", [128, 64], I32)
        kf = sb(es, "kf", [128, 64], F32)
        mm = sb(es, "mm", [128, 64], F32)
        rt = Res()
        posi_all = sb(es, "posi_all", [128, NCORES * TT], I32)
        posf_all = sb(es, "posf_all", [128, NCORES * TT], F32)
        k.dma("sync", posi_all[:], posall_d[:, :], wr=[r_const])
        k.dve(lambda e: e.tensor_copy(out=posf_all[:], in_=posi_all[:]), rd=[r_const], wr=[r_cs])

        def compute_cs(cs, r_cst, pf, col0):
            for t in range(TT):
                k.dve(lambda e: e.tensor_scalar(out=ang[:], in0=freqs[:], scalar1=pf[:, col0 + t:col0 + t + 1], scalar2=None,
                                                op0=ALU.mult), rd=[r_const, r_cs, rt], wr=[rt])
                for which, off in ((1, 0.5), (0, 0.75)):
                    k.dve(lambda e: e.tensor_scalar(out=u[:], in0=ang[:], scalar1=1.0 / (2 * math.pi), scalar2=off,
                                                    op0=ALU.mult, op1=ALU.add), rd=[rt], wr=[rt])
                    k.dve(lambda e: e.tensor_copy(out=ki[:], in_=u[:]), rd=[rt], wr=[rt])
                    k.dve(lambda e: e.tensor_copy(out=kf[:], in_=ki[:]), rd=[rt], wr=[rt])
                    k.dve(lambda e: e.tensor_tensor(out=u[:], in0=u[:], in1=kf[:], op=ALU.subtract), rd=[rt], wr=[rt])
                    k.dve(lambda e: e.tensor_scalar(out=mm[:], in0=u[:], scalar1=0.5, scalar2=None, op0=ALU.is_gt),
                          rd=[rt], wr=[rt])
                    k.dve(lambda e: e.tensor_tensor(out=u[:], in0=u[:], in1=mm[:], op=ALU.subtract), rd=[rt], wr=[rt])
                    k.act(lambda e: e.activation(out=cs[:, t, which, :], in_=u[:], func=AF.Sin,
                                                 scale=-2 * math.pi * (1 - 1e-6)), rd=[rt], wr=[r_cst])

        csr = Ring(es, nc, "cs", [128, TT, 2, 64], F32, 2)
        csh = list(csr.next())
        compute_cs(csh[0], csh[1], posf, 0)

        def norm_T(st_ring, src_ap, r_src_rd, g_col0, dstT, r_dst, col0):
            (xt, r_xt), (xb, r_xb), (st, r_st), (jk, r_jk) = st_ring
            k.dma("sync", xt[:], src_ap, rd=r_src_rd, wr=[r_xt])
            k.act(lambda e: e.activation(out=jk[:], in_=xt[:], func=AF.Square, accum_out=st[:, 0:1]),
                  rd=[r_xt], wr=[r_jk, r_st])
            k.act(lambda e: e.activation(out=st[:, 1:2], in_=st[:, 0:1], func=AF.Sqrt, scale=1.0 / D, bias=EPS),
                  rd=[r_st], wr=[r_st])
            k.dve(lambda e: e.reciprocal(out=st[:, 2:3], in_=st[:, 1:2]), rd=[r_st], wr=[r_st])
            k.dve(lambda e: e.tensor_scalar(out=xb[:], in0=xt[:], scalar1=st[:, 2:3], scalar2=None, op0=ALU.mult),
                  rd=[r_xt, r_st], wr=[r_xb])
            for half in range(2):
                pb, r_pb = psb.next()
                for j in range(8):
                    c = half * 8 + j
                    k.pe(lambda e: e.transpose(out=pb[:, j * 128:(j + 1) * 128], in_=xb[:, c * 128:(c + 1) * 128],
                                               identity=ident[:]), rd=[r_xb, r_const], wr=[r_pb])
                gb = gfm[:, g_col0 + half * 8:g_col0 + half * 8 + 8].unsqueeze(2).to_broadcast([128, 8, 128])
                k.dve(lambda e: e.tensor_tensor(out=dstT[:, half * 8:half * 8 + 8, col0:col0 + 128],
                                                in0=pb[:].rearrange("p (j q) -> p j q", q=128), in1=gb, op=ALU.mult),
                      rd=[r_pb, r_const], wr=[r_dst])

        def load_w(ring, src, rows_kc, c0, n, q="gpsimd", nsplit=4):
            wt, r_w = ring.next()
            step = max(1, rows_kc // nsplit)
            for a in range(0, rows_kc, step):
                b = min(rows_kc, a + step)
                k.dma(q, wt[:, a:b, 0:n],
                      src[a * 128:b * 128, c0:c0 + n].rearrange("(c p) n -> p c n", p=128), wr=[r_w], acc=(a > 0))
            return wt, r_w

        with ExitStack() as ph:
            hTr = Ring(ph, nc, "hT", [128, KC, NT], BF16, 2)
            hT, r_hT = hTr.next()
            nrings = [Ring(ph, nc, "xt", [128, D], F32, 2), Ring(ph, nc, "xb", [128, D], BF16, 2),
                      Ring(ph, nc, "st", [128, 4], F32, 2), Ring(ph, nc, "jk", [128, D], BF16, 1)]
            for t in range(TT):
                norm_T([r.next() for r in nrings], x_d[t * 128:(t + 1) * 128, :], [], 0, hT, r_hT, t * 128)

            wring = Ring(ph, nc, "win", [128, KC, 512], BF16, 2)
            xsr = Ring(ph, nc, "xs", [128, 512], F32, 2)
            xrr = Ring(ph, nc, "xr", [128, 512], BF16, 3)
            tmr = Ring(ph, nc, "tmp", [128, 256], F32, 4)
            stT = Ring(ph, nc, "stT", [128, 4, NT], BF16, 2)
            stM = Ring(ph, nc, "stM", [128, TT, 512], BF16, 2)
            iwst = sb(ph, "iwst", [128, TT, 16], F32)
            r_iwst = Res()

            def rope(xs, r_xs, xr, r_xr, n, hd, t):
                nh = n // hd
                half = hd // 2
                xv = xs[:, 0:n].rearrange("p (h two f) -> p h two f", two=2, f=half)
                ov = xr[:, 0:n].rearrange("p (h two f) -> p h two f", two=2, f=half)
                cs, r_csx = csh
                if hd == 128:
                    cos = cs[:, t, 0, :]
                    sin = cs[:, t, 1, :]
                else:
                    cos = cs[:, t, 0, 0::2]
                    sin = cs[:, t, 1, 0::2]
                cosb = cos.unsqueeze(1).to_broadcast([128, nh, half])
                sinb = sin.unsqueeze(1).to_broadcast([128, nh, half])
                hw = nh * half
                (t1, r1), (t2, r2) = tmr.next(), tmr.next()
                t1v = t1[:, 0:hw].rearrange("p (h f) -> p h f", f=half)
                t2v = t2[:, 0:hw].rearrange("p (h f) -> p h f", f=half)
                k.dve(lambda e: e.tensor_tensor(out=t1v, in0=xv[:, :, 0, :], in1=cosb, op=ALU.mult),
                      rd=[r_xs, r_csx], wr=[r1])
                k.dve(lambda e: e.tensor_tensor(out=t2v, in0=xv[:, :, 1, :], in1=sinb, op=ALU.mult),
                      rd=[r_xs, r_csx], wr=[r2])
                k.dve(lambda e: e.tensor_tensor(out=ov[:, :, 0, :], in0=t1v, in1=t2v, op=ALU.subtract),
                      rd=[r1, r2], wr=[r_xr])
                (t3, r3), (t4, r4) = tmr.next(), tmr.next()
                t3v = t3[:, 0:hw].rearrange("p (h f) -> p h f", f=half)
                t4v = t4[:, 0:hw].rearrange("p (h f) -> p h f", f=half)
                k.dve(lambda e: e.tensor_tensor(out=t3v, in0=xv[:, :, 1, :], in1=cosb, op=ALU.mult),
                      rd=[r_xs, r_csx], wr=[r3])
                k.dve(lambda e: e.tensor_tensor(out=t4v, in0=xv[:, :, 0, :], in1=sinb, op=ALU.mult),
                      rd=[r_xs, r_csx], wr=[r4])
                k.dve(lambda e: e.tensor_tensor(out=ov[:, :, 1, :], in0=t3v, in1=t4v, op=ALU.add),
                      rd=[r3, r4], wr=[r_xr])

            def transposes(xr, r_xr, n, fw, sT, r_sT, t):
                nj = n // 128
                pb, r_pb = psb.next()
                for j in range(nj):
                    k.pe(lambda e: e.transpose(out=pb[:, j * 128:(j + 1) * 128], in_=xr[:, j * 128:(j + 1) * 128],
                                               identity=ident[:]), rd=[r_xr, r_const], wr=[r_pb])
                k.act(lambda e: e.activation(out=sT[:, 0:nj, t * 128:(t + 1) * 128],
                                             in_=pb[:, 0:nj * 128].rearrange("p (j q) -> p j q", q=128), func=AF.Copy),
                      rd=[r_pb], wr=[r_sT])

            st64 = Ring(ph, nc, "st64", [64, 8, NT], BF16, 1)

            def transposes64(xr, r_xr, n, sT, r_sT, t):
                nj = n // 64
                pb, r_pb = psb.next()
                for j in range(nj):
                    k.pe(lambda e: e.transpose(out=pb[0:64, j * 128:(j + 1) * 128], in_=xr[:, j * 64:(j + 1) * 64],
                                               identity=ident[:]), rd=[r_xr, r_const], wr=[r_pb])
                k.act(lambda e: e.activation(out=sT[0:64, 0:nj, t * 128:(t + 1) * 128],
                                             in_=pb[0:64, 0:nj * 128].rearrange("p (j q) -> p j q", q=128),
                                             func=AF.Copy), rd=[r_pb], wr=[r_sT])

            groups = []
            for i in range(2):
                groups.append((C_AQ + 512 * i, 512, "ropeT", dict(hd=128, scale=128 ** -0.5, dst=QT, rn="QT", row0=512 * i)))
            for i in range(2):
                groups.append((C_IQ + 512 * i, 512, "rope64", dict(scale=64 ** -0.5, dst=IQT, rn="IQT", row0=512 * i)))
            groups.append((C_IK, 80, "ikw", dict(iw=True, row0=0, dst=KVI_loc, rn="KVI_loc")))
            for i in range(2):
                groups.append((C_RQ + 512 * i, 512, "ropeT", dict(hd=128, scale=1.0, dst=RQT, rn="RQT", row0=512 * i)))
            for i in range(2):
                groups.append((C_RK + 512 * i, 512, "ropeT", dict(hd=128, scale=1.0, dst=RKT, rn="RKT", row0=512 * i,
                                                                 tm_dst=RK, tm_rn="RK", tm_col0=512 * i)))
            for i in range(4):
                groups.append((C_RV + 512 * i, 512, "tm", dict(func=AF.Copy, scale=1.0, dst=RV, rn="RV", row0=0, col0=512 * i)))
            for i in range(4):
                groups.append((C_RG + 512 * i, 512, "tm", dict(func=AF.Copy, scale=1.0, dst=RG, rn="RG", row0=0, col0=512 * i)))
            for i in range(2):
                groups.append((C_MQ + 512 * i, 512, "T", dict(scale=256 ** -0.5, dst=MQT, rn="MQT", row0=512 * i)))
            for i in range(12):
                groups.append((C_GT + 512 * i, 512, "tm", dict(func=AF.Sigmoid, scale=1.0, dst=GATES, rn="GATES", row0=0, col0=512 * i)))

            def run_groups(groups, hT, r_hT, cached=False):
                pending = []
                gi_next = [0]

                def prefetch():
                    while gi_next[0] < len(groups) and len(pending) < 1:
                        c0, n, kind, prm = groups[gi_next[0]]
                        if cached and c0 in wsh_off:
                            wt_, r_w_ = wring.next()
                            o = wsh_off[c0]
                            for a in range(0, KC, 4):
                                k.dma("sync", wt_[:, a:a + 4, 0:n],
                                      WSH[a * 128:(a + 4) * 128, o:o + n].rearrange("(c p) n -> p c n", p=128),
                                      rd=[R["WSH"]], wr=[r_w_], acc=(a > 0))
                            pending.append((wt_, r_w_))
                        else:
                            wt_, r_w_ = load_w(wring, win_d, KC, c0, n)
                            if (not cached) and c0 in wsh_off:
                                o = wsh_off[c0]
                                k.dma("sync", WSH[:, o:o + n].rearrange("(c p) n -> p c n", p=128), wt_[:, :, 0:n],
                                      rd=[r_w_], wr=[R["WSH"]], acc=True)
                            pending.append((wt_, r_w_))
                        gi_next[0] += 1

                prefetch()
                for gi, (c0, n, kind, prm) in enumerate(groups):
                    wt, r_w = pending.pop(0)
                    prefetch()
                    sT = r_sT = sM = r_sM = None
                    if kind in ("ropeT", "T"):
                        sT, r_sT = stT.next()
                    if kind in ("rope64", "ikw"):
                        sT, r_sT = st64.next()
                    if kind in ("tm", "rope_tm") or (kind == "ropeT" and "tm_dst" in prm):
                        sM, r_sM = stM.next()
                    deferred = []
                    for t in range(TT):
                        ps, r_ps = psf.next()
                        for c in range(KC):
                            k.pe(lambda e: e.matmul(ps[:, 0:n], lhsT=hT[:, c, t * 128:(t + 1) * 128], rhs=wt[:, c, 0:n],
                                                    start=(c == 0), stop=(c == KC - 1)), rd=[r_hT, r_w], wr=[r_ps])
                        while deferred:
                            deferred.pop(0)()
                        if kind == "tm":
                            k.act(lambda e: e.activation(out=sM[:, t, 0:n], in_=ps[:, 0:n], func=prm["func"],
                                                         scale=prm["scale"]), rd=[r_ps], wr=[r_sM])
                        elif kind == "T":
                            xr, r_xr = xrr.next()
                            k.act(lambda e: e.activation(out=xr[:, 0:n], in_=ps[:, 0:n], func=AF.Copy, scale=prm["scale"]),
                                  rd=[r_ps], wr=[r_xr])
                            deferred.append(lambda xr=xr, r_xr=r_xr, t=t: transposes(xr, r_xr, n, 128, sT, r_sT, t))
                        elif kind == "ropeT":
                            xs, r_xs = xsr.next()
                            xr, r_xr = xrr.next()
                            k.act(lambda e: e.activation(out=xs[:, 0:n], in_=ps[:, 0:n], func=AF.Copy, scale=prm["scale"]),
                                  rd=[r_ps], wr=[r_xs])
                            rope(xs, r_xs, xr, r_xr, n, prm["hd"], t)
                            deferred.append(lambda xr=xr, r_xr=r_xr, t=t: transposes(xr, r_xr, n, 128, sT, r_sT, t))
                            if sM is not None:
                                k.dve(lambda e: e.tensor_copy(out=sM[:, t, 0:n], in_=xr[:, 0:n]), rd=[r_xr], wr=[r_sM])
                        elif kind == "rope_tm":
                            xs, r_xs = xsr.next()
                            k.act(lambda e: e.activation(out=xs[:, 0:n], in_=ps[:, 0:n], func=AF.Copy, scale=prm["scale"]),
                                  rd=[r_ps], wr=[r_xs])
                            rope(xs, r_xs, sM[:, t, :], r_sM, n, prm["hd"], t)
                        elif kind == "rope64":
                            xs, r_xs = xsr.next()
                            xr, r_xr = xrr.next()
                            k.act(lambda e: e.activation(out=xs[:, 0:n], in_=ps[:, 0:n], func=AF.Copy, scale=prm["scale"]),
                                  rd=[r_ps], wr=[r_xs])
                            rope(xs, r_xs, xr, r_xr, n, 64, t)
                            deferred.append(lambda xr=xr, r_xr=r_xr, t=t: transposes64(xr, r_xr, n, sT, r_sT, t))
                        elif kind == "ikw":
                            xs, r_xs = xsr.next()
                            xr, r_xr = xrr.next()
                            k.act(lambda e: e.activation(out=xs[:, 0:64], in_=ps[:, 0:64], func=AF.Copy), rd=[r_ps], wr=[r_xs])
                            if prm["iw"]:
                                k.act(lambda e: e.activation(out=iwst[:, t, :], in_=ps[:, 64:80], func=AF.Copy, scale=0.25),
                                      rd=[r_ps], wr=[r_iwst])
                            rope(xs, r_xs, xr, r_xr, 64, 64, t)
                            deferred.append(lambda xr=xr, r_xr=r_xr, t=t: transposes64(xr, r_xr, 64, sT, r_sT, t))
                    while deferred:
                        deferred.pop(0)()
                    if kind in ("ropeT", "T"):
                        dst = prm["dst"][prm["row0"]:prm["row0"] + n, :].rearrange("(j f) t -> f j t", f=128)
                        k.dma("sync", dst, sT[:, 0:n // 128, :], rd=[r_sT], wr=[R[prm["rn"]]], acc=True)
                        if sM is not None:
                            dst = prm["tm_dst"][:, prm["tm_col0"]:prm["tm_col0"] + n].rearrange("(t p) n -> p t n", p=128)
                            k.dma("sync", dst, sM[:, :, 0:n], rd=[r_sM], wr=[R[prm["tm_rn"]]], acc=True)
                    elif kind == "rope64":
                        dst = prm["dst"][prm["row0"]:prm["row0"] + n, :].rearrange("(j f) t -> f j t", f=64)
                        k.dma("sync", dst, sT[0:64, 0:n // 64, :], rd=[r_sT], wr=[R[prm["rn"]]], acc=True)
                    elif kind == "ikw":
                        if prm["iw"]:
                            k.dma("sync", IW.rearrange("(t p) n -> p t n", p=128), iwst[:], rd=[r_iwst], wr=[R["IW"]], acc=True)
                        else:
                            k.dma("sync", prm["dst"][prm["row0"]:prm["row0"] + 64, :], sT[0:64, 0, :], rd=[r_sT],
                                  wr=[R[prm["rn"]]], acc=True)
                    elif kind == "tm":
                        dst = prm["dst"][prm["row0"]:prm["row0"] + NT, prm["col0"]:prm["col0"] + n].rearrange(
                            "(t p) n -> p t n", p=128)
                        k.dma("sync", dst, sM[:, :, 0:n], rd=[r_sM], wr=[R[prm["rn"]]], acc=True)
                    elif kind == "rope_tm":
                        dst = prm["tm_dst"][prm["tm_row0"]:prm["tm_row0"] + NT, prm["tm_col0"]:prm["tm_col0"] + n].rearrange(
                            "(t p) n -> p t n", p=128)
                        k.dma("sync", dst, sM[:, :, 0:n], rd=[r_sM], wr=[R[prm["tm_rn"]]], acc=True)

            wsh_off = {}
            _o = 0
            for c0_ in ([C_AK, C_AK + 512, C_AV, C_AV + 512, C_IK, C_RK, C_RK + 512] + [C_RV + 512 * i for i in range(4)]):
                wsh_off[c0_] = _o
                _o += 512
            run_groups(groups, hT, r_hT)
            for c0_ in (C_AK, C_AK + 512, C_AV, C_AV + 512):
                wt_, r_w_ = load_w(wring, win_d, KC, c0_, 512)
                k.dma("sync", WSH[:, wsh_off[c0_]:wsh_off[c0_] + 512].rearrange("(c p) n -> p c n", p=128), wt_[:, :, :],
                      rd=[r_w_], wr=[R["WSH"]], acc=True)
            k.barrier()
            if upto == "p2":
                k.barrier()
                return nc
            for r in range(NCORES):
                hT, r_hT = hTr.next()
                csh[0], csh[1] = csr.next()
                compute_cs(csh[0], csh[1], posf_all, r * TT)
                for t in range(TT):
                    norm_T([q.next() for q in nrings], xall_d[r * NT + t * 128:r * NT + (t + 1) * 128, :], [], 0, hT, r_hT,
                           t * 128)
                rgl = []
                for i in range(2):
                    rgl.append((C_AK + 512 * i, 512, "ropeT", dict(hd=128, scale=1.0, dst=KVI_all, rn="KVI_all",
                                                                   row0=r * 2112 + 512 * i)))
                for i in range(2):
                    rgl.append((C_AV + 512 * i, 512, "tm", dict(func=AF.Copy, scale=1.0, dst=KVI_all, rn="KVI_all",
                                                                row0=r * 2112 + 1024, col0=512 * i)))
                rgl.append((C_IK, 64, "ikw", dict(iw=False, dst=KVI_all, rn="KVI_all", row0=r * 2112 + 2048)))
                for i in range(2):
                    rgl.append((C_RK + 512 * i, 512, "rope_tm", dict(hd=128, scale=1.0, tm_dst=RK_all, tm_rn="RK_all",
                                                                     tm_row0=r * NT, tm_col0=512 * i)))
                for i in range(4):
                    rgl.append((C_RV + 512 * i, 512, "tm", dict(func=AF.Copy, scale=1.0, dst=RV_all, rn="RV_all",
                                                                row0=r * NT, col0=512 * i)))
                run_groups(rgl, hT, r_hT, cached=True)
            k.barrier()

        if upto == "p2r":
            return nc
        with ExitStack() as ph:
            iqT = sb(ph, "iqT", [64, 16, NT], BF16)
            ikT = sb(ph, "ikT", [64, S], BF16)
            iw = sb(ph, "iw", [128, TT, 16], F32)
            d4 = sb(ph, "d4", [128, 512], F32)
            cbg = sb(ph, "cbg", [128, 128], F32)
            accr = Ring(ph, nc, "acc", [128, S], F32, 2)
            accres = [[Res() for _ in range(16)] for _ in range(2)]
            sel = sb(ph, "sel", [128, S], BF16)
            mst = Ring(ph, nc, "mst", [128, 64, 128], BF16, 1)
            rr = Ring(ph, nc, "rr", [128, 512], BF16, 4)
            penr = Ring(ph, nc, "pen", [128, 512], BF16, 2)
            wdr = Ring(ph, nc, "wd", [128, 16, 128], BF16, 2)
            bs = Ring(ph, nc, "bs", [128, 8], F32, 2)
            r_in = Res()
            r_sel = Res()
            k.dma("sync", iqT[:], IQT.rearrange("(h d) t -> d h t", d=64), rd=[R["IQT"]], wr=[r_in])
            k.dma("sync", ikT[:].rearrange("d (r t) -> d r t", t=NT),
                  KVI_all.rearrange("(r x) t -> x r t", x=2112)[2048:2112], rd=[R["KVI_all"]], wr=[r_in], acc=True)
            k.dma("sync", iw[:], IW.rearrange("(t p) n -> p t n", p=128), rd=[R["IW"]], wr=[r_in], acc=True)
            k.dma("sync", d4[:], d4_d[:, :], wr=[r_in], acc=True)
            k.dma("sync", cbg[:], cbg_d[:, :], wr=[r_in], acc=True)
            thr_all = sb(ph, "thr_all", [128, TT], F32)
            r_thr = Res()
            psz = [(psf.t[j], psf.r[j]) for j in (0, 1, 2, 3)]
            psa = [(psf.t[j], psf.r[j]) for j in (4, 5)]
            LOOK = 3
            zc = [0]

            def indexer(i):
                acc = accr.t[i % 2]
                r_acc = accres[i % 2]
                wd, r_wd = wdr.next()
                for h in range(16):
                    k.dve(lambda e: e.tensor_scalar(out=wd[:, h, :], in0=ident[:], scalar1=iw[:, i, h:h + 1], scalar2=None,
                                                    op0=ALU.mult), rd=[r_const, r_in], wr=[r_wd])
                for g in range(16):
                    pen, r_pen = penr.next()
                    k.dve(lambda e: e.tensor_scalar(out=pen[:], in0=d4[:], scalar1=cbg[:, i * 16 + g:i * 16 + g + 1],
                                                    scalar2=NEG, op0=ALU.is_gt, op1=ALU.mult), rd=[r_in], wr=[r_pen])
                    pA, r_pA = psa[g % 2]
                    zs = {}

                    def emit_z(h):
                        pz, r_pz = psz[zc[0] % 4]
                        zc[0] += 1
                        k.pe(lambda e: e.matmul(pz[:], lhsT=iqT[:, h, i * 128:(i + 1) * 128],
                                                rhs=ikT[:, g * 512:(g + 1) * 512], start=True, stop=True),
                             rd=[r_in], wr=[r_pz])
                        zs[h] = (pz, r_pz)
                    for h in range(LOOK):
                        emit_z(h)
                    for h in range(16):
                        if h + LOOK < 16:
                            emit_z(h + LOOK)
                        pz, r_pz = zs.pop(h)
                        rl, r_rl = rr.next()
                        k.act(lambda e: e.activation(out=rl[:], in_=pz[:], func=AF.Relu), rd=[r_pz], wr=[r_rl])
                        k.pe(lambda e: e.matmul(pA[:], lhsT=wd[:, h, :], rhs=rl[:], start=(h == 0), stop=False),
                             rd=[r_wd, r_rl], wr=[r_pA])
                    k.pe(lambda e: e.matmul(pA[:], lhsT=ident[:], rhs=pen[:], start=False, stop=True),
                         rd=[r_const, r_pen], wr=[r_pA])
                    k.act(lambda e: e.activation(out=acc[:, g * 512:(g + 1) * 512], in_=pA[:], func=AF.Copy),
                          rd=[r_pA], wr=[r_acc[g]])

            def select(i):
                acc = accr.t[i % 2]
                r_acc = accres[i % 2]
                if SCORE is not None:
                    k.dma("sync", SCORE[i * 128:(i + 1) * 128, :], acc[:], rd=r_acc, wr=[R["SCORE"]], acc=True)
                b, r_b = bs.next()
                k.dve(lambda e: e.reduce_max(out=b[:, 0:1], in_=acc[:], axis=AX.X), rd=r_acc, wr=[r_b])
                k.dve(lambda e: e.tensor_scalar(out=b[:, 1:2], in0=b[:, 0:1], scalar1=-BW0, scalar2=None, op0=ALU.add),
                      rd=[r_b], wr=[r_b])
                k.dve(lambda e: e.tensor_scalar(out=b[:, 6:7], in0=b[:, 0:1], scalar1=-BW0, scalar2=None, op0=ALU.add),
                      rd=[r_b], wr=[r_b])
                for it in range(NBIS):
                    wk = (BW0 + 1e-3) * 2.0 ** -(it + 1)
                    k.dve(lambda e: e.tensor_scalar(out=b[:, 3:4], in0=b[:, 1:2], scalar1=wk, scalar2=None, op0=ALU.add),
                          rd=[r_b], wr=[r_b])
                    k.dve(lambda e: e.tensor_scalar(out=sel[:], in0=acc[:], scalar1=b[:, 3:4], scalar2=None,
                                                    op0=ALU.is_ge, op1=ALU.add, accum_out=b[:, 4:5]),
                          rd=[r_b] + r_acc, wr=[r_sel, r_b])
                    k.dve(lambda e: e.tensor_scalar(out=b[:, 5:6], in0=b[:, 4:5], scalar1=TOPK - 0.5, scalar2=None,
                                                    op0=ALU.is_ge), rd=[r_b], wr=[r_b])
                    k.dve(lambda e: e.scalar_tensor_tensor(out=b[:, 1:2], in0=b[:, 5:6], scalar=wk, in1=b[:, 1:2],
                                                           op0=ALU.mult, op1=ALU.add), rd=[r_b], wr=[r_b])
                k.dve(lambda e: e.tensor_tensor(out=b[:, 5:6], in0=b[:, 1:2], in1=b[:, 6:7], op=ALU.is_le),
                      rd=[r_b], wr=[r_b])
                k.dve(lambda e: e.scalar_tensor_tensor(out=b[:, 1:2], in0=b[:, 5:6], scalar=-1000.0, in1=b[:, 1:2],
                                                       op0=ALU.mult, op1=ALU.add), rd=[r_b], wr=[r_b])
                k.dve(lambda e: e.tensor_scalar(out=sel[:], in0=acc[:], scalar1=b[:, 1:2], scalar2=None, op0=ALU.is_ge),
                      rd=[r_b] + r_acc, wr=[r_sel])
                k.dve(lambda e: e.tensor_copy(out=thr_all[:, i:i + 1], in_=b[:, 1:2]), rd=[r_b], wr=[r_thr])
                ms, r_ms = mst.next()
                for kb8 in range(8):
                    pb, r_pb = psb.next()
                    for j in range(8):
                        kb = kb8 * 8 + j
                        k.pe(lambda e: e.transpose(out=pb[:, j * 128:(j + 1) * 128], in_=sel[:, kb * 128:(kb + 1) * 128],
                                                   identity=ident[:]), rd=[r_sel, r_const], wr=[r_pb])
                    k.act(lambda e: e.activation(out=ms[:, kb8 * 8:(kb8 + 1) * 8, :],
                                                 in_=pb[:].rearrange("p (j q) -> p j q", q=128), func=AF.Copy),
                          rd=[r_pb], wr=[r_ms])
                k.dma("sync", MT[i], ms[:], rd=[r_ms], wr=[R["MT"]], acc=True)

            indexer(0)
            for i in range(TT):
                if i + 1 < TT:
                    indexer(i + 1)
                select(i)
            k.dma("sync", THR[:, :], thr_all[:], rd=[r_thr], wr=[R["THR"]], acc=True)
            k.barrier()

        if upto == "p4":
            return nc
        with ExitStack() as ph:
            qT = sb(ph, "qT", [128, 8, NT], BF16)
            r_q = Res()
            k.dma("sync", qT[:], QT.rearrange("(h d) t -> d h t", d=128), rd=[R["QT"]], wr=[r_q])
            kTr = Ring(ph, nc, "kT", [128, S], BF16, 2)
            vSr = Ring(ph, nc, "vS", [128, 64, 128], BF16, 2)
            mT = sb(ph, "mT", [128, 4, 64, 128], BF16)
            r_mT = Res()
            er = Ring(ph, nc, "er", [128, 512], BF16, 4)
            pr = Ring(ph, nc, "pr", [128, 512], BF16, 4)
            rdn = Ring(ph, nc, "rdn", [128, 512], F32, 2)
            oaT = sb(ph, "oaT", [128, 8, NT], BF16)
            r_oaT = Res()
            ps_s = [(psf.t[j], psf.r[j]) for j in (0, 1, 2, 3)]
            ps_o = [(psf.t[4], psf.r[4])]
            ps_d = [(psf.t[5], psf.r[5])]
            PRE = 3
            kvx = KVI_all.rearrange("(r x) t -> x r t", x=2112)
            it = 0
            for qc in range(2):
                k.dma("sync", mT[:], MT[qc * 4:(qc + 1) * 4].rearrange("i k b q -> k i b q"), rd=[R["MT"]], wr=[r_mT])
                for h in range(8):
                    kT, r_kT = kTr.next()
                    vS, r_vS = vSr.next()
                    k.dma("sync", kT[:].rearrange("d (r t) -> d r t", t=NT), kvx[h * 128:(h + 1) * 128],
                          rd=[R["KVI_all"]], wr=[r_kT])
                    for r in range(NCORES):
                        k.dma("sync", vS[:, r * 8:(r + 1) * 8, :],
                              KVI_all[r * 2112 + 1024:r * 2112 + 2048, h * 128:(h + 1) * 128].rearrange(
                                  "(t p) d -> p t d", p=128), rd=[R["KVI_all"]], wr=[r_vS], acc=(r > 0))
                    po, r_po = ps_o[0]
                    pd, r_pd = ps_d[0]

                    def emit_s(kb):
                        pS, r_pS = ps_s[kb % 4]
                        k.pe(lambda e: e.matmul(pS[:], lhsT=kT[:, kb * 128:(kb + 1) * 128],
                                                rhs=qT[:, h, qc * 512:(qc + 1) * 512], start=True, stop=True),
                             rd=[r_kT, r_q], wr=[r_pS])
                    for kb in range(PRE):
                        emit_s(kb)
                    for kb in range(64):
                        if kb + PRE < 64:
                            emit_s(kb + PRE)
                        pS, r_pS = ps_s[kb % 4]
                        ee, r_ee = er.next()
                        k.act(lambda e: e.activation(out=ee[:], in_=pS[:], func=AF.Exp), rd=[r_pS], wr=[r_ee])
                        pp, r_pp = pr.next()
                        k.dve(lambda e: e.tensor_tensor(out=pp[:].rearrange("p (i q) -> p i q", q=128),
                                                        in0=ee[:].rearrange("p (i q) -> p i q", q=128),
                                                        in1=mT[:, :, kb, :], op=ALU.mult),
                              rd=[r_ee, r_mT], wr=[r_pp])
                        k.pe(lambda e: e.matmul(po[:], lhsT=vS[:, kb, :], rhs=pp[:], start=(kb == 0), stop=(kb == 63)),
                             rd=[r_vS, r_pp], wr=[r_po])
                        k.pe(lambda e: e.matmul(pd[:], lhsT=ones[:], rhs=pp[:], start=(kb == 0), stop=(kb == 63)),
                             rd=[r_cs, r_pp], wr=[r_pd])
                    rd_, r_rd = rdn.next()
                    k.dve(lambda e: e.reciprocal(out=rd_[:], in_=pd[:]), rd=[r_pd], wr=[r_rd])
                    k.dve(lambda e: e.tensor_tensor(out=oaT[:, h, qc * 512:(qc + 1) * 512], in0=po[:], in1=rd_[:],
                                                    op=ALU.mult), rd=[r_po, r_rd], wr=[r_oaT])
            k.dma("sync", OAT.rearrange("(h d) t -> d h t", d=128), oaT[:], rd=[r_oaT], wr=[R["OAT"]], acc=True)
            k.barrier()

        if upto == "p5":
            return nc
        with ExitStack() as ph:
            dmt = sb(ph, "dmt", [128, 8, 128], F32)
            qdec = sb(ph, "qdec", [128, 8, 128], F32)
            kdec = sb(ph, "kdec", [128, 8], F32)
            dch = sb(ph, "dch", [128, 8], F32)
            coef = sb(ph, "coef", [128, 64], F32)
            Lst = sb(ph, "Lst", [128, 8, 256], F32)
            Sst = sb(ph, "Sst", [128, 8, 256], F32)
            r_ld = Res()
            r_L = [Res() for _ in range(8)]
            r_S = [Res() for _ in range(8)]
            k.dma("sync", dmt[:].rearrange("p h i -> p (h i)"), dmt_d[:, :], wr=[r_ld])
            k.dma("sync", qdec[:].rearrange("p h i -> p (h i)"), qdec_d[:, :], wr=[r_ld])
            k.dma("sync", kdec[:], kdec_d[:, :], wr=[r_ld])
            k.dma("sync", dch[:], dch_d[:, :], wr=[r_ld])
            k.dma("sync", coef[:], coef_d[:, :], wr=[r_ld])

            def kv_update(st, r_st, kk, r_kk, vv, r_vv, h, n, first):
                ps, r_ps = psf.next()
                k.pe(lambda e: e.matmul(ps[:, 0:256], lhsT=kk[:, n, h * 128:(h + 1) * 128],
                                        rhs=vv[:, n, h * 256:(h + 1) * 256], start=True, stop=True),
                     rd=[r_kk, r_vv], wr=[r_ps])
                if first:
                    k.dve(lambda e: e.tensor_copy(out=st[:, h, :], in_=ps[:, 0:256]), rd=[r_ps], wr=[r_st[h]])
                else:
                    k.dve(lambda e: e.scalar_tensor_tensor(out=st[:, h, :], in0=st[:, h, :], scalar=dch[:, h:h + 1],
                                                           in1=ps[:, 0:256], op0=ALU.mult, op1=ALU.add),
                          rd=[r_ps, r_ld, r_st[h]], wr=[r_st[h]])

            with ExitStack() as pa:
                rkr = Ring(pa, nc, "rkA", [128, TT, 1024], BF16, 2)
                rvr = Ring(pa, nc, "rvA", [128, TT, 2048], BF16, 2)
                for c2 in range(NCORES):
                    rka, r_rka = rkr.next()
                    rva, r_rva = rvr.next()
                    k.dma("sync", rka[:], RK_all[c2 * NT:(c2 + 1) * NT, :].rearrange("(t p) n -> p t n", p=128),
                          rd=[R["RK_all"]], wr=[r_rka])
                    k.dma("sync", rva[:], RV_all[c2 * NT:(c2 + 1) * NT, :].rearrange("(t p) n -> p t n", p=128),
                          rd=[R["RV_all"]], wr=[r_rva])
                    for h in range(8):
                        k.dve(lambda e: e.tensor_scalar(out=rka[:, :, h * 128:(h + 1) * 128],
                                                        in0=rka[:, :, h * 128:(h + 1) * 128],
                                                        scalar1=kdec[:, h:h + 1], scalar2=None, op0=ALU.mult),
                              rd=[r_ld, r_rka], wr=[r_rka])
                    for h in range(8):
                        for n in range(TT):
                            kv_update(Lst, r_L, rka, r_rka, rva, r_rva, h, n, n == 0)
                        cf = coef[:, c2 * 8 + h:c2 * 8 + h + 1]
                        if c2 == 0:
                            k.dve(lambda e: e.tensor_scalar(out=Sst[:, h, :], in0=Lst[:, h, :], scalar1=cf, scalar2=None,
                                                            op0=ALU.mult), rd=[r_L[h], r_ld], wr=[r_S[h]])
                        else:
                            k.dve(lambda e: e.scalar_tensor_tensor(out=Sst[:, h, :], in0=Lst[:, h, :], scalar=cf,
                                                                   in1=Sst[:, h, :], op0=ALU.mult, op1=ALU.add),
                                  rd=[r_L[h], r_ld, r_S[h]], wr=[r_S[h]])
                k.barrier()

            rqT = sb(ph, "rqT", [128, 8, NT], BF16)
            rkT = sb(ph, "rkT", [128, 8, NT], BF16)
            rqTd = sb(ph, "rqTd", [128, 8, NT], BF16)
            rks = sb(ph, "rks", [128, TT, 1024], BF16)
            rv = sb(ph, "rv", [128, TT, 2048], BF16)
            Sbf = Ring(ph, nc, "Sbf", [128, 256], BF16, 2)
            rgr = Ring(ph, nc, "rg", [128, 2048], BF16, 2)
            sTr = Ring(ph, nc, "scT", [128, 128], BF16, 3)
            ynr = Ring(ph, nc, "yn", [128, 256], F32, 3)
            sgr = Ring(ph, nc, "sg", [128, 256], F32, 3)
            bnr = Ring(ph, nc, "bn", [128, 16], F32, 3)
            obr = Ring(ph, nc, "ob", [128, 2048], BF16, 2)
            obT = sb(ph, "obT", [128, 16, NT], BF16)
            r_rks = Res()
            r_rv = Res()
            r_rqTd = Res()
            r_obT = Res()
            k.dma("sync", rqT[:], RQT.rearrange("(h d) t -> d h t", d=128), rd=[R["RQT"]], wr=[r_ld])
            k.dma("sync", rkT[:], RKT.rearrange("(h d) t -> d h t", d=128), rd=[R["RKT"]], wr=[r_ld])
            k.dma("sync", rks[:], RK.rearrange("(t p) n -> p t n", p=128), rd=[R["RK"]], wr=[r_rks])
            k.dma("sync", rv[:], RV.rearrange("(t p) n -> p t n", p=128), rd=[R["RV"]], wr=[r_rv])
            for h in range(8):
                k.dve(lambda e: e.tensor_scalar(out=rks[:, :, h * 128:(h + 1) * 128], in0=rks[:, :, h * 128:(h + 1) * 128],
                                                scalar1=kdec[:, h:h + 1], scalar2=None, op0=ALU.mult),
                      rd=[r_ld, r_rks], wr=[r_rks])
                k.dve(lambda e: e.tensor_tensor(out=rqTd[:, h, :].rearrange("p (n i) -> p n i", i=128),
                                                in0=rqT[:, h, :].rearrange("p (n i) -> p n i", i=128),
                                                in1=qdec[:, h, :].unsqueeze(1).to_broadcast([128, TT, 128]),
                                                op=ALU.mult), rd=[r_ld], wr=[r_rqTd])
            for n in range(TT):
                rg, r_rg = rgr.next()
                k.dma("sync", rg[:], RG[n * 128:(n + 1) * 128, :], rd=[R["RG"]], wr=[r_rg])
                ob, r_ob = obr.next()
                for h in range(8):
                    sbf, r_sbf = Sbf.next()
                    k.act(lambda e: e.activation(out=sbf[:], in_=Sst[:, h, :], func=AF.Copy), rd=[r_S[h]], wr=[r_sbf])
                    pS, r_pS = psf.next()
                    k.pe(lambda e: e.matmul(pS[:, 0:128], lhsT=rkT[:, h, n * 128:(n + 1) * 128],
                                            rhs=rqT[:, h, n * 128:(n + 1) * 128], start=True, stop=True),
                         rd=[r_ld], wr=[r_pS])
                    sT_, r_sT_ = sTr.next()
                    k.dve(lambda e: e.tensor_tensor(out=sT_[:], in0=pS[:, 0:128], in1=dmt[:, h, :], op=ALU.mult),
                          rd=[r_pS, r_ld], wr=[r_sT_])
                    pY, r_pY = psf.next()
                    k.pe(lambda e: e.matmul(pY[:, 0:256], lhsT=sT_[:], rhs=rv[:, n, h * 256:(h + 1) * 256],
                                            start=True, stop=False), rd=[r_sT_, r_rv], wr=[r_pY])
                    k.pe(lambda e: e.matmul(pY[:, 0:256], lhsT=rqTd[:, h, n * 128:(n + 1) * 128], rhs=sbf[:],
                                            start=False, stop=True), rd=[r_rqTd, r_sbf], wr=[r_pY])
                    bn, r_bn = bnr.next()
                    k.dve(lambda e: e.bn_stats(out=bn[:, 0:6], in_=pY[:, 0:256]), rd=[r_pY], wr=[r_bn])
                    k.dve(lambda e: e.bn_aggr(out=bn[:, 8:10], in_=bn[:, 0:6]), rd=[r_bn], wr=[r_bn])
                    k.act(lambda e: e.activation(out=bn[:, 10:11], in_=bn[:, 9:10], func=AF.Sqrt, bias=EPS),
                          rd=[r_bn], wr=[r_bn])
                    k.dve(lambda e: e.reciprocal(out=bn[:, 11:12], in_=bn[:, 10:11]), rd=[r_bn], wr=[r_bn])
                    yn, r_yn = ynr.next()
                    k.dve(lambda e: e.tensor_scalar(out=yn[:], in0=pY[:, 0:256], scalar1=bn[:, 8:9], scalar2=bn[:, 11:12],
                                                    op0=ALU.subtract, op1=ALU.mult), rd=[r_pY, r_bn], wr=[r_yn])
                    sg, r_sg = sgr.next()
                    k.act(lambda e: e.activation(out=sg[:], in_=rg[:, h * 256:(h + 1) * 256], func=AF.Silu),
                          rd=[r_rg], wr=[r_sg])
                    k.dve(lambda e: e.tensor_tensor(out=ob[:, h * 256:(h + 1) * 256], in0=yn[:], in1=sg[:], op=ALU.mult),
                          rd=[r_yn, r_sg], wr=[r_ob])
                    if n < TT - 1:
                        kv_update(Sst, r_S, rks, r_rks, rv, r_rv, h, n, False)
                for half in range(2):
                    pb, r_pb = psb.next()
                    for j in range(8):
                        c = half * 8 + j
                        k.pe(lambda e: e.transpose(out=pb[:, j * 128:(j + 1) * 128], in_=ob[:, c * 128:(c + 1) * 128],
                                                   identity=ident[:]), rd=[r_ob, r_const], wr=[r_pb])
                    k.act(lambda e: e.activation(out=obT[:, half * 8:half * 8 + 8, n * 128:(n + 1) * 128],
                                                 in_=pb[:].rearrange("p (j q) -> p j q", q=128), func=AF.Copy),
                          rd=[r_pb], wr=[r_obT])
            k.dma("sync", OBT.rearrange("(c f) t -> f c t", f=128), obT[:], rd=[r_obT], wr=[R["OBT"]], acc=True)
            k.barrier()

        if upto == "p6":
            return nc
        with ExitStack() as ph:
            memT = sb(ph, "memT", [128, KC, 256], BF16)
            r_memT = Res()
            with ExitStack() as p1:
                rings = [Ring(p1, nc, "mxt", [128, D], F32, 2), Ring(p1, nc, "mxb", [128, D], BF16, 2),
                         Ring(p1, nc, "mst_", [128, 4], F32, 2), Ring(p1, nc, "mjk", [128, D], BF16, 1)]
                for t in range(2):
                    norm_T([r.next() for r in rings], mem_d[t * 128:(t + 1) * 128, :], [], 32, memT, r_memT, t * 128)
                k.barrier()
            wmr = Ring(ph, nc, "wm", [128, KC, 512], BF16, 2)
            mkT = sb(ph, "mkT", [128, 8, 256], BF16)
            mv = sb(ph, "mv", [128, 2, 1024], BF16)
            mqT = sb(ph, "mqT", [128, 8, NT], BF16)
            ocT = sb(ph, "ocT", [128, 8, NT], BF16)
            pmr = Ring(ph, nc, "pm", [128, 512], BF16, 4)
            rdn = Ring(ph, nc, "rdm", [128, 512], F32, 2)
            r_mk = Res()
            r_mv = Res()
            r_mq = Res()
            r_oc = Res()
            k.dma("sync", mqT[:], MQT.rearrange("(c f) t -> f c t", f=128), rd=[R["MQT"]], wr=[r_mq])
            for g in range(2):
                wt, r_w = load_w(wmr, wmk_d, KC, g * 512, 512)
                for j in range(4):
                    ps, r_ps = psf.next()
                    for c in range(KC):
                        k.pe(lambda e: e.matmul(ps[:, 0:256], lhsT=wt[:, c, j * 128:(j + 1) * 128], rhs=memT[:, c, :],
                                                start=(c == 0), stop=(c == KC - 1)), rd=[r_w, r_memT], wr=[r_ps])
                    k.act(lambda e: e.activation(out=mkT[:, g * 4 + j, :], in_=ps[:, 0:256], func=AF.Copy),
                          rd=[r_ps], wr=[r_mk])
            for g in range(2):
                wt, r_w = load_w(wmr, wmk_d, KC, 1024 + g * 512, 512)
                for mt in range(2):
                    ps, r_ps = psf.next()
                    for c in range(KC):
                        k.pe(lambda e: e.matmul(ps[:], lhsT=memT[:, c, mt * 128:(mt + 1) * 128], rhs=wt[:, c, :],
                                                start=(c == 0), stop=(c == KC - 1)), rd=[r_w, r_memT], wr=[r_ps])
                    k.act(lambda e: e.activation(out=mv[:, mt, g * 512:(g + 1) * 512], in_=ps[:], func=AF.Copy),
                          rd=[r_ps], wr=[r_mv])
            for hm in range(4):
                for qc in range(2):
                    pms = []
                    for mt in range(2):
                        ps, r_ps = psf.next()
                        for dc in range(2):
                            k.pe(lambda e: e.matmul(ps[:], lhsT=mkT[:, hm * 2 + dc, mt * 128:(mt + 1) * 128],
                                                    rhs=mqT[:, hm * 2 + dc, qc * 512:(qc + 1) * 512],
                                                    start=(dc == 0), stop=(dc == 1)), rd=[r_mk, r_mq], wr=[r_ps])
                        pm, r_pm = pmr.next()
                        k.act(lambda e: e.activation(out=pm[:], in_=ps[:], func=AF.Exp), rd=[r_ps], wr=[r_pm])
                        pms.append((pm, r_pm))
                    pd, r_pd = psf.next()
                    for mt in range(2):
                        k.pe(lambda e: e.matmul(pd[:], lhsT=ones[:], rhs=pms[mt][0][:], start=(mt == 0), stop=(mt == 1)),
                             rd=[r_cs, pms[mt][1]], wr=[r_pd])
                    rd_, r_rd = rdn.next()
                    k.dve(lambda e: e.reciprocal(out=rd_[:], in_=pd[:]), rd=[r_pd], wr=[r_rd])
                    for ec in range(2):
                        po, r_po = psf.next()
                        for mt in range(2):
                            k.pe(lambda e: e.matmul(po[:], lhsT=mv[:, mt, hm * 256 + ec * 128:hm * 256 + (ec + 1) * 128],
                                                    rhs=pms[mt][0][:], start=(mt == 0), stop=(mt == 1)),
                                 rd=[r_mv, pms[mt][1]], wr=[r_po])
                        k.dve(lambda e: e.tensor_tensor(out=ocT[:, hm * 2 + ec, qc * 512:(qc + 1) * 512], in0=po[:],
                                                        in1=rd_[:], op=ALU.mult), rd=[r_po, r_rd], wr=[r_oc])
            k.dma("sync", OCT.rearrange("(c f) t -> f c t", f=128), ocT[:], rd=[r_oc], wr=[R["OCT"]], acc=True)
            k.barrier()

        if upto == "p7":
            return nc
        with ExitStack() as ph:
            oT = sb(ph, "oT", [128, 32, NT], BF16)
            r_oT = Res()
            k.dma("sync", oT[:, 0:8, :], OAT.rearrange("(c f) t -> f c t", f=128), rd=[R["OAT"]], wr=[r_oT])
            k.dma("sync", oT[:, 8:24, :], OBT.rearrange("(c f) t -> f c t", f=128), rd=[R["OBT"]], wr=[r_oT])
            k.dma("sync", oT[:, 24:32, :], OCT.rearrange("(c f) t -> f c t", f=128), rd=[R["OCT"]], wr=[r_oT])
            wbr = Ring(ph, nc, "wbr", [128, 32, 512], BF16, 2)
            gtr = Ring(ph, nc, "gt", [128, TT, 3, 512], BF16, 1)
            tr = Ring(ph, nc, "tm3", [128, 512], F32, 4)
            mxr = Ring(ph, nc, "mx", [128, TT, 512], BF16, 2)
            branches = ((0, 8, wa_d), (8, 16, wb_d), (24, 8, wc_d))
            for cg in range(4):
                wt, r_w = wbr.next()
                for (c0, nk, wsrc) in branches:
                    for a in range(0, nk, 8):
                        k.dma("gpsimd", wt[:, c0 + a:c0 + a + 8, :],
                              wsrc[a * 128:(a + 8) * 128, cg * 512:(cg + 1) * 512].rearrange("(c p) n -> p c n", p=128),
                              wr=[r_w], acc=not (c0 == 0 and a == 0))
                gt, r_gt = gtr.next()
                for b in range(3):
                    k.dma("sync", gt[:, :, b, :],
                          GATES[:, b * D + cg * 512:b * D + (cg + 1) * 512].rearrange("(t p) n -> p t n", p=128),
                          rd=[R["GATES"]], wr=[r_gt], acc=(b > 0))
                mx, r_mx = mxr.next()
                for t in range(TT):
                    tmps = []
                    for b, (c0, nk, wsrc) in enumerate(branches):
                        ps, r_ps = psf.next()
                        for c in range(nk):
                            k.pe(lambda e: e.matmul(ps[:], lhsT=oT[:, c0 + c, t * 128:(t + 1) * 128], rhs=wt[:, c0 + c, :],
                                                    start=(c == 0), stop=(c == nk - 1)), rd=[r_oT, r_w], wr=[r_ps])
                        tm_, r_tm = tr.next()
                        k.dve(lambda e: e.tensor_tensor(out=tm_[:], in0=ps[:], in1=gt[:, t, b, :], op=ALU.mult),
                              rd=[r_ps, r_gt], wr=[r_tm])
                        tmps.append((tm_, r_tm))
                    k.dve(lambda e: e.tensor_tensor(out=tmps[0][0][:], in0=tmps[0][0][:], in1=tmps[1][0][:], op=ALU.add),
                          rd=[tmps[0][1], tmps[1][1]], wr=[tmps[0][1]])
                    k.dve(lambda e: e.tensor_tensor(out=mx[:, t, :], in0=tmps[0][0][:], in1=tmps[2][0][:], op=ALU.add),
                          rd=[tmps[0][1], tmps[2][1]], wr=[r_mx])
                k.dma("sync", MIX[:, cg * 512:(cg + 1) * 512].rearrange("(t p) n -> p t n", p=128), mx[:],
                      rd=[r_mx], wr=[R["MIX"]], acc=True)
            k.barrier()

        if upto == "p8a":
            return nc
        with ExitStack() as ph0:
            h2T = sb(ph0, "h2T", [128, KC, NT], BF16)
            r_h2T = Res()
            gpm = sb(ph0, "gpm", [128, D], F32)
            r_g = Res()
            k.dma("sync", gpm[:], gpm_d[:, :], wr=[r_g])

            def post_norm_residual(z, r_z, gp, resid, r_resid, outt, r_out, stt, r_stt, jk, r_jk):
                k.act(lambda e: e.activation(out=jk[:], in_=z[:], func=AF.Square, accum_out=stt[:, 0:1]),
                      rd=[r_z], wr=[r_jk, r_stt])
                k.act(lambda e: e.activation(out=stt[:, 1:2], in_=stt[:, 0:1], func=AF.Sqrt, scale=1.0 / D, bias=EPS),
                      rd=[r_stt], wr=[r_stt])
                k.dve(lambda e: e.reciprocal(out=stt[:, 2:3], in_=stt[:, 1:2]), rd=[r_stt], wr=[r_stt])
                k.dve(lambda e: e.scalar_tensor_tensor(out=z[:], in0=z[:], scalar=stt[:, 2:3], in1=gp[:],
                                                       op0=ALU.mult, op1=ALU.mult), rd=[r_z, r_stt, r_g], wr=[r_z])
                k.dve(lambda e: e.tensor_tensor(out=outt[:], in0=z[:], in1=resid[:], op=ALU.add),
                      rd=[r_z, r_resid], wr=[r_out])

            with ExitStack() as ph:
                wo = sb(ph, "wo", [128, KC, D], BF16)
                r_wo = Res()
                for a in range(0, KC, 2):
                    k.dma("gpsimd", wo[:, a:a + 2, :], wo_d[a * 128:(a + 2) * 128, :].rearrange("(c p) n -> p c n", p=128),
                          wr=[r_wo], acc=(a > 0))
                mixT = sb(ph, "mixT", [128, KC, NT], BF16)
                r_mixT = Res()
                mtr = Ring(ph, nc, "mtile", [128, D], BF16, 1)
                for t in range(TT):
                    mt_, r_mt = mtr.next()
                    k.dma("sync", mt_[:], MIX[t * 128:(t + 1) * 128, :], rd=[R["MIX"]], wr=[r_mt])
                    for half in range(2):
                        pb, r_pb = psb.next()
                        for j in range(8):
                            c = half * 8 + j
                            k.pe(lambda e: e.transpose(out=pb[:, j * 128:(j + 1) * 128], in_=mt_[:, c * 128:(c + 1) * 128],
                                                       identity=ident[:]), rd=[r_mt, r_const], wr=[r_pb])
                        k.act(lambda e: e.activation(out=mixT[:, half * 8:half * 8 + 8, t * 128:(t + 1) * 128],
                                                     in_=pb[:].rearrange("p (j q) -> p j q", q=128), func=AF.Copy),
                              rd=[r_pb], wr=[r_mixT])
                zr = Ring(ph, nc, "z", [128, D], F32, 2)
                xr_ = Ring(ph, nc, "xres", [128, D], F32, 1)
                x1r = Ring(ph, nc, "x1", [128, D], F32, 2)
                str_ = Ring(ph, nc, "pst", [128, 4], F32, 2)
                jkr = Ring(ph, nc, "pjk", [128, D], BF16, 1)
                hbr = Ring(ph, nc, "hb", [128, D], BF16, 1)
                for t in range(TT):
                    z, r_z = zr.next()
                    for cg in range(4):
                        ps, r_ps = psf.next()
                        for c in range(KC):
                            k.pe(lambda e: e.matmul(ps[:], lhsT=mixT[:, c, t * 128:(t + 1) * 128],
                                                    rhs=wo[:, c, cg * 512:(cg + 1) * 512],
                                                    start=(c == 0), stop=(c == KC - 1)), rd=[r_mixT, r_wo], wr=[r_ps])
                        k.act(lambda e: e.activation(out=z[:, cg * 512:(cg + 1) * 512], in_=ps[:], func=AF.Copy),
                              rd=[r_ps], wr=[r_z])
                    xres, r_xres = xr_.next()
                    k.dma("sync", xres[:], x_d[t * 128:(t + 1) * 128, :], wr=[r_xres])
                    x1, r_x1 = x1r.next()
                    stt, r_stt = str_.next()
                    jk, r_jk = jkr.next()
                    post_norm_residual(z, r_z, gpm, xres, r_xres, x1, r_x1, stt, r_stt, jk, r_jk)
                    k.dma("sync", X1[t * 128:(t + 1) * 128, :], x1[:], rd=[r_x1], wr=[R["X1"]], acc=True)
                    stt2, r_stt2 = str_.next()
                    hb, r_hb = hbr.next()
                    k.act(lambda e: e.activation(out=jk[:], in_=x1[:], func=AF.Square, accum_out=stt2[:, 0:1]),
                          rd=[r_x1], wr=[r_jk, r_stt2])
                    k.act(lambda e: e.activation(out=stt2[:, 1:2], in_=stt2[:, 0:1], func=AF.Sqrt, scale=1.0 / D, bias=EPS),
                          rd=[r_stt2], wr=[r_stt2])
                    k.dve(lambda e: e.reciprocal(out=stt2[:, 2:3], in_=stt2[:, 1:2]), rd=[r_stt2], wr=[r_stt2])
                    k.dve(lambda e: e.tensor_scalar(out=hb[:], in0=x1[:], scalar1=stt2[:, 2:3], scalar2=None, op0=ALU.mult),
                          rd=[r_x1, r_stt2], wr=[r_hb])
                    for half in range(2):
                        pb, r_pb = psb.next()
                        for j in range(8):
                            c = half * 8 + j
                            k.pe(lambda e: e.transpose(out=pb[:, j * 128:(j + 1) * 128], in_=hb[:, c * 128:(c + 1) * 128],
                                                       identity=ident[:]), rd=[r_hb, r_const], wr=[r_pb])
                        gb = gfm[:, 16 + half * 8:16 + half * 8 + 8].unsqueeze(2).to_broadcast([128, 8, 128])
                        k.dve(lambda e: e.tensor_tensor(out=h2T[:, half * 8:half * 8 + 8, t * 128:(t + 1) * 128],
                                                        in0=pb[:].rearrange("p (j q) -> p j q", q=128), in1=gb,
                                                        op=ALU.mult), rd=[r_pb, r_const], wr=[r_h2T])
                k.barrier()

            if upto == "p8b":
                return nc
            with ExitStack() as ph:
                actT = sb(ph, "actT", [128, 44, NT], BF16)
                r_act = Res()
                with ExitStack() as p9:
                    wgr = Ring(p9, nc, "wg", [128, KC, 256], BF16, 2)
                    wur = Ring(p9, nc, "wu", [128, KC, 256], BF16, 2)
                    sgr = Ring(p9, nc, "fsg", [128, 512], F32, 3)
                    pend = []
                    nxt = 0

                    def pre9():
                        nonlocal nxt
                        while nxt < 22 and len(pend) < 1:
                            a = load_w(wgr, wfi_d, KC, nxt * 256, 256, nsplit=2)
                            b = load_w(wur, wfi_d, KC, DFF + nxt * 256, 256, nsplit=2)
                            pend.append((a, b))
                            nxt += 1
                    pre9()
                    for jj in range(22):
                        (wg, r_wg), (wu, r_wu) = pend.pop(0)
                        pre9()
                        for a in range(2):
                            j = jj * 2 + a
                            for b in range(2):
                                pg, r_pg = psf.next()
                                pu, r_pu = psf.next()
                                for c in range(KC):
                                    k.pe(lambda e: e.matmul(pg[:], lhsT=wg[:, c, a * 128:(a + 1) * 128],
                                                            rhs=h2T[:, c, b * 512:(b + 1) * 512],
                                                            start=(c == 0), stop=(c == KC - 1)), rd=[r_wg, r_h2T], wr=[r_pg])
                                for c in range(KC):
                                    k.pe(lambda e: e.matmul(pu[:], lhsT=wu[:, c, a * 128:(a + 1) * 128],
                                                            rhs=h2T[:, c, b * 512:(b + 1) * 512],
                                                            start=(c == 0), stop=(c == KC - 1)), rd=[r_wu, r_h2T], wr=[r_pu])
                                sg, r_sg = sgr.next()
                                k.act(lambda e: e.activation(out=sg[:], in_=pg[:], func=AF.Silu), rd=[r_pg], wr=[r_sg])
                                k.dve(lambda e: e.tensor_tensor(out=actT[:, j, b * 512:(b + 1) * 512], in0=sg[:], in1=pu[:],
                                                                op=ALU.mult), rd=[r_sg, r_pu], wr=[r_act])
                    k.barrier()
                with ExitStack() as p10:
                    wfr = Ring(p10, nc, "wf", [128, 44, 256], BF16, 2)
                    yst = Ring(p10, nc, "yst", [128, TT, 256], F32, 2)
                    pend = []
                    nxt = 0

                    def pre10():
                        nonlocal nxt
                        while nxt < 8 and len(pend) < 1:
                            pend.append(load_w(wfr, wfo_d, 44, nxt * 256, 256, nsplit=4))
                            nxt += 1
                    pre10()
                    for cg in range(8):
                        wf, r_wf = pend.pop(0)
                        pre10()
                        ys, r_ys = yst.next()
                        for t in range(TT):
                            ps, r_ps = psf.next()
                            for j in range(44):
                                k.pe(lambda e: e.matmul(ps[:, 0:256], lhsT=actT[:, j, t * 128:(t + 1) * 128], rhs=wf[:, j, :],
                                                        start=(j == 0), stop=(j == 43)), rd=[r_act, r_wf], wr=[r_ps])
                            k.act(lambda e: e.activation(out=ys[:, t, :], in_=ps[:, 0:256], func=AF.Copy),
                                  rd=[r_ps], wr=[r_ys])
                        k.dma("sync", Y2[:, cg * 256:(cg + 1) * 256].rearrange("(t p) n -> p t n", p=128), ys[:],
                              rd=[r_ys], wr=[R["Y2"]], acc=True)
                    k.barrier()

            with ExitStack() as ph:
                gpf = sb(ph, "gpf", [128, D], F32)
                k.dma("sync", gpf[:], gpf_d[:, :], wr=[r_g])
                zr = Ring(ph, nc, "fz", [128, D], F32, 2)
                xr_ = Ring(ph, nc, "fx1", [128, D], F32, 2)
                orr = Ring(ph, nc, "fo", [128, D], F32, 2)
                str_ = Ring(ph, nc, "fst", [128, 4], F32, 2)
                jkr = Ring(ph, nc, "fjk", [128, D], BF16, 1)
                for t in range(TT):
                    z, r_z = zr.next()
                    x1, r_x1 = xr_.next()
                    o, r_o = orr.next()
                    stt, r_stt = str_.next()
                    jk, r_jk = jkr.next()
                    k.dma("sync", z[:], Y2[t * 128:(t + 1) * 128, :], rd=[R["Y2"]], wr=[r_z])
                    k.dma("sync", x1[:], X1[t * 128:(t + 1) * 128, :], rd=[R["X1"]], wr=[r_x1])
                    post_norm_residual(z, r_z, gpf, x1, r_x1, o, r_o, stt, r_stt, jk, r_jk)
                    k.dma("sync", out_d[t * 128:(t + 1) * 128, :], o[:], rd=[r_o], wr=[R["out"]], acc=True)
                k.barrier()
    return nc


_NC_CACHE = {}


def _consts():
    ident = np.eye(128, dtype=np.float32).astype(ml_dtypes.bfloat16)
    fr = (10000.0 ** (-np.arange(0, 128, 2, dtype=np.float32) / np.float32(128))).astype(np.float32)
    freqs = np.broadcast_to(fr[None, :], (128, 64)).copy()
    q = np.arange(128)[:, None]
    sidx = np.arange(512)[None, :]
    d4 = (sidx - q).astype(np.float32)
    H = 8
    log_gamma = np.log1p(-np.exp2(-5.0 - np.arange(H, dtype=np.float64)))
    i = np.arange(128, dtype=np.float64)
    diff = i[None, :] - i[:, None]
    dmt = np.where(diff[:, None, :] >= 0, np.exp(np.maximum(diff, 0)[:, None, :] * log_gamma[None, :, None]), 0.0) * 128 ** -0.5
    qdec = np.exp((i + 1)[None, None, :] * log_gamma[None, :, None]) * np.ones((128, 1, 1))
    kdec = np.exp((127 - i)[:, None] * log_gamma[None, :]) * 128 ** -0.5
    dch = np.exp(128 * log_gamma)[None, :] * np.ones((128, 1))
    return dict(ident=ident, freqs=freqs, d4=d4, dmt=dmt.reshape(128, 1024).astype(np.float32),
                qdec=qdec.reshape(128, 1024).astype(np.float32), kdec=kdec.astype(np.float32),
                dch=dch.astype(np.float32)), log_gamma


def make_in_maps(inputs):
    c, log_gamma = _consts()
    x = np.asarray(inputs["x"], dtype=np.float32)[0]
    mem = np.ascontiguousarray(np.asarray(inputs["mem"], dtype=np.float32)[0])
    pos = np.asarray(inputs["positions"]).astype(np.int32)[0]

    def fm(g):
        return np.asarray(g, dtype=np.float32)[0].reshape(16, 128).T
    gfm = np.ascontiguousarray(np.concatenate([fm(inputs["g_pre_mix"]), fm(inputs["g_pre_ffn"]), fm(inputs["g_mem"])], axis=1))
    gpm = np.ascontiguousarray(np.broadcast_to(np.asarray(inputs["g_post_mix"], dtype=np.float32)[0][None, :], (128, D)))
    gpf = np.ascontiguousarray(np.broadcast_to(np.asarray(inputs["g_post_ffn"], dtype=np.float32)[0][None, :], (128, D)))
    shared = dict(mem=mem, gfm=gfm, gpm=gpm, gpf=gpf, x_all=np.ascontiguousarray(x),
                  pos_all=np.ascontiguousarray(pos.reshape(NCORES * TT, 128).T),
                  w_in=np.ascontiguousarray(np.asarray(inputs["w_in"], dtype=np.float32)[0]),
                  w_mem_kv=np.ascontiguousarray(np.asarray(inputs["w_mem_kv"], dtype=np.float32)[0]),
                  w_a=np.ascontiguousarray(np.asarray(inputs["w_branch_a"], dtype=np.float32)[0]),
                  w_b=np.ascontiguousarray(np.asarray(inputs["w_branch_b"], dtype=np.float32)[0]),
                  w_c=np.ascontiguousarray(np.asarray(inputs["w_branch_c"], dtype=np.float32)[0]),
                  w_out=np.ascontiguousarray(np.asarray(inputs["w_out"], dtype=np.float32)[0]),
                  w_fi=np.ascontiguousarray(np.asarray(inputs["w_ffn_in"], dtype=np.float32)[0]),
                  w_fo=np.ascontiguousarray(np.asarray(inputs["w_ffn_out"], dtype=np.float32)[0]),
                  **c)
    maps = []
    for core in range(NCORES):
        m = dict(shared)
        m["x"] = np.ascontiguousarray(x[core * NT:(core + 1) * NT])
        m["pos"] = np.ascontiguousarray(pos[core * NT:(core + 1) * NT].reshape(TT, 128).T)
        cbg = np.zeros((TT, 16), np.float32)
        for i in range(TT):
            for g in range(16):
                cbg[i, g] = 128.0 * ((core * TT + i) - 4 * g)
        m["cbg"] = np.ascontiguousarray(np.broadcast_to(cbg.reshape(1, 128), (128, 128)))
        coef = np.zeros((NCORES, 8), np.float64)
        for c2 in range(NCORES):
            if c2 < core:
                coef[c2] = np.exp(128.0 * TT * (core - 1 - c2) * log_gamma)
        m["coef"] = np.ascontiguousarray(np.broadcast_to(coef.reshape(1, 64).astype(np.float32), (128, 64)))
        maps.append(m)
    return maps


def kernel(**inputs):
    if "nc" not in _NC_CACHE:
        _NC_CACHE["nc"] = build(False)
    nc = _NC_CACHE["nc"]
    maps = make_in_maps(inputs)
    res = run_bass_kernel_spmd(nc, maps, core_ids=list(range(NCORES)))
    out = np.concatenate([np.asarray(r["out"], dtype=np.float32) for r in res.results], axis=0)
    return out.reshape(1, S, D)
```
